# Optimizing a Trainium2 kernel written in Bass

```python
import jax
import jax.numpy as jnp
from jax import lax
import numpy as np

D_MODEL = 1024
BATCH = 4
SEQ = 8192
DEPTH = 1

CTX_LEN = 256
GRID_W = 64
HEAD_DIM = 64
ATTN_Q_HEADS = 8
ATTN_KV_HEADS = 2
ATTN_GROUP = ATTN_Q_HEADS // ATTN_KV_HEADS
WINDOW = 128
BLOCK = 128
ROPE_BASE = 10000.0
ROPE_FREQS = HEAD_DIM // 4
GM_GROUPS = 8
GM_HEAD = 64
GM_CHUNK = 128
GM_WIDTH = GM_GROUPS * GM_HEAD
ATTN_WIDTH = ATTN_Q_HEADS * HEAD_DIM
KV_WIDTH = ATTN_KV_HEADS * HEAD_DIM
N_BRANCH = 2
Q0 = 0
K0 = Q0 + ATTN_WIDTH
V0 = K0 + KV_WIDTH
U0 = V0 + KV_WIDTH
VG0 = U0 + GM_WIDTH
GATE0 = VG0 + GM_WIDTH
IN_WIDTH = GATE0 + N_BRANCH * D_MODEL
N_GROUPS = 4
EXPERTS_PER_GROUP = 8
N_EXPERTS = N_GROUPS * EXPERTS_PER_GROUP
TOP_K_IN_GROUP = 2
D_EXPERT = D_MODEL // 2
EXPERT_BLOCK = 128
LN_EPS = 1e-6
NEG_INF = -1e30
ALPHA = (2.0 * DEPTH) ** 0.25
BETA = (8.0 * DEPTH) ** -0.25

kernel_name = 'hybrid_dit_window_gqa_gmlp_hmoe'


def _layernorm(x, g=None, b=None):
    xf = x.astype(jnp.float32)
    mu = jnp.mean(xf, -1, keepdims=True)
    var = jnp.mean(jnp.square(xf - mu), -1, keepdims=True)
    y = ((xf - mu) * lax.rsqrt(var + LN_EPS)).astype(x.dtype)
    if g is not None:
        y = y * g + b
    return y


def _modulate(h, shift, scale):
    return h * (1 + scale) + shift


def _rope_tables(pos, dtype):
    inv = 1.0 / (ROPE_BASE ** (jnp.arange(ROPE_FREQS, dtype=jnp.float32) / ROPE_FREQS))
    ang = pos.astype(jnp.float32)[:, None] * inv[None, :]
    return (jnp.cos(ang).astype(dtype)[:, None, :], jnp.sin(ang).astype(dtype)[:, None, :])


def _rotate(x, cos, sin):
    xa, xb = jnp.split(x, 2, axis=-1)
    return jnp.concatenate([xa * cos - xb * sin, xb * cos + xa * sin], -1)


def _rope_2d(x, rope):
    cos_r, sin_r, cos_c, sin_c = rope
    xr, xc = jnp.split(x, 2, axis=-1)
    return jnp.concatenate([_rotate(xr, cos_r, sin_r), _rotate(xc, cos_c, sin_c)], -1)


def _window_mask(n):
    nb = n // BLOCK
    blk = jnp.arange(nb, dtype=jnp.int32)[:, None, None]
    qpos = blk * BLOCK + jnp.arange(BLOCK, dtype=jnp.int32)[None, :, None]
    kpos = (blk - 1) * BLOCK + jnp.arange(3 * BLOCK, dtype=jnp.int32)[None, None, :]
    return (jnp.abs(qpos - kpos) <= WINDOW) & (kpos >= 0) & (kpos < n)


def _split_proj(p):
    b, n, _ = p.shape
    q = p[..., Q0:K0].reshape(b, n, ATTN_Q_HEADS, HEAD_DIM)
    k = p[..., K0:V0].reshape(b, n, ATTN_KV_HEADS, HEAD_DIM)
    v = p[..., V0:U0].reshape(b, n, ATTN_KV_HEADS, HEAD_DIM)
    u = p[..., U0:VG0]
    vg = p[..., VG0:GATE0]
    ga = p[..., GATE0:GATE0 + D_MODEL]
    gb = p[..., GATE0 + D_MODEL:IN_WIDTH]
    return q, k, v, u, vg, ga, gb


def _kv_proj(h, w_in):
    b, n, _ = h.shape
    kv = h @ w_in[:, K0:U0]
    k = kv[..., :KV_WIDTH].reshape(b, n, ATTN_KV_HEADS, HEAD_DIM)
    v = kv[..., KV_WIDTH:].reshape(b, n, ATTN_KV_HEADS, HEAD_DIM)
    return k, v


def _sink_column(sink, shape):
    s = sink.astype(jnp.float32).reshape(ATTN_KV_HEADS, ATTN_GROUP, 1, 1)
    return jnp.broadcast_to(s, shape[:-1] + (1,))


def _latent_attention(q, k, v, k_ctx, v_ctx, sink, mask):
    b, n = q.shape[:2]
    nb = n // BLOCK
    qb = q.reshape(b, nb, BLOCK, ATTN_KV_HEADS, ATTN_GROUP, HEAD_DIM)

    def band(t):
        tp = jnp.pad(t, ((0, 0), (BLOCK, BLOCK), (0, 0), (0, 0)))
        tp = tp.reshape(b, nb + 2, BLOCK, ATTN_KV_HEADS, HEAD_DIM)
        return jnp.concatenate([tp[:, :-2], tp[:, 1:-1], tp[:, 2:]], axis=2)

    kw, vw = band(k), band(v)
    s_loc = jnp.einsum('bnqhgd,bnkhd->bnhgqk', qb, kw).astype(jnp.float32)
    s_loc = jnp.where(mask[None, :, None, None], s_loc, NEG_INF)
    s_ctx = jnp.einsum('bnqhgd,bkhd->bnhgqk', qb, k_ctx).astype(jnp.float32)
    logits = jnp.concatenate([_sink_column(sink, s_ctx.shape), s_ctx, s_loc], -1)
    p = jax.nn.softmax(logits, -1).astype(q.dtype)
    c_len = k_ctx.shape[1]
    o = (jnp.einsum('bnhgqk,bkhd->bnqhgd', p[..., 1:1 + c_len], v_ctx)
         + jnp.einsum('bnhgqk,bnkhd->bnqhgd', p[..., 1 + c_len:], vw))
    return o.reshape(b, n, ATTN_WIDTH)


def _context_attention(q, k, v, sink):
    b, n = q.shape[:2]
    qg = q.reshape(b, n, ATTN_KV_HEADS, ATTN_GROUP, HEAD_DIM)
    s = jnp.einsum('bqhgd,bkhd->bhgqk', qg, k).astype(jnp.float32)
    p = jax.nn.softmax(jnp.concatenate([_sink_column(sink, s.shape), s], -1), -1)[..., 1:]
    o = jnp.einsum('bhgqk,bkhd->bqhgd', p.astype(q.dtype), v)
    return o.reshape(b, n, ATTN_WIDTH)


def _chunk_gmlp(u, v, lp):
    b, n, _ = u.shape
    u = jax.nn.gelu(u)
    v = _layernorm(jax.nn.gelu(v), lp['gm_ln_g'], lp['gm_ln_b'])
    vc = v.reshape(b, n // GM_CHUNK, GM_CHUNK, GM_GROUPS, GM_HEAD)
    sp = jnp.einsum('gpq,bnqgc->bnpgc', lp['gm_ws'], vc) + lp['gm_bs'].T[None, None, :, :, None]
    return u * sp.reshape(b, n, GM_WIDTH)


def _merge(y_attn, y_gm, ga, gb, lp):
    y = jax.nn.sigmoid(ga) * (y_attn @ lp['w_pa']) + jax.nn.sigmoid(gb) * (y_gm @ lp['w_pb'])
    return y @ lp['w_o']


def _hier_moe(h, lp):
    b, n, d = h.shape
    t = h.reshape(b * n, d)
    n_tok = b * n
    g_prob = jax.nn.softmax((t @ lp['router_g_w'] + lp['router_g_b']).astype(jnp.float32), -1)
    g_w, g_idx = lax.top_k(g_prob, 1)
    e_logit = (jnp.einsum('nd,gde->nge', t, lp['router_e_w']) + lp['router_e_b']).astype(jnp.float32)
    sel = jnp.broadcast_to(g_idx[:, :, None], (n_tok, 1, EXPERTS_PER_GROUP))
    e_logit = jnp.take_along_axis(e_logit, sel, axis=1)[:, 0]
    e_val, e_idx = lax.top_k(e_logit, TOP_K_IN_GROUP)
    e_w = jax.nn.softmax(e_val, -1)
    expert = g_idx * EXPERTS_PER_GROUP + e_idx
    weight = (g_w * e_w).astype(h.dtype)
    n_assign = n_tok * TOP_K_IN_GROUP
    e_flat = expert.reshape(-1)
    w_flat = weight.reshape(-1)
    tok_flat = jnp.repeat(jnp.arange(n_tok, dtype=jnp.int32), TOP_K_IN_GROUP)
    order = jnp.argsort(e_flat)
    e_sorted = e_flat[order]
    counts = jnp.bincount(e_flat, length=N_EXPERTS)
    padded = (counts + EXPERT_BLOCK - 1) // EXPERT_BLOCK * EXPERT_BLOCK
    pad_end = jnp.cumsum(padded)
    pad_start = pad_end - padded
    start = jnp.cumsum(counts) - counts
    pos = pad_start[e_sorted] + jnp.arange(n_assign, dtype=jnp.int32) - start[e_sorted]
    cap = -(-n_assign // EXPERT_BLOCK) * EXPERT_BLOCK + N_EXPERTS * EXPERT_BLOCK
    buf_tok = jnp.zeros((cap,), jnp.int32).at[pos].set(tok_flat[order])
    buf_w = jnp.zeros((cap,), h.dtype).at[pos].set(w_flat[order])
    n_blk = cap // EXPERT_BLOCK
    blk_e = jnp.searchsorted(pad_end, jnp.arange(n_blk, dtype=jnp.int32) * EXPERT_BLOCK, side='right')
    blk_e = jnp.minimum(blk_e, N_EXPERTS - 1)
    xb = t[buf_tok].reshape(n_blk, EXPERT_BLOCK, d)
    w1, w3, w2 = lp['moe_w1'], lp['moe_w3'], lp['moe_w2']

    def run_block(args):
        xt, e = args
        hid = jax.nn.silu(xt @ w1[e]) * (xt @ w3[e])
        return hid @ w2[e]

    y = lax.map(run_block, (xb, blk_e)).reshape(cap, d)
    out = jnp.zeros_like(t).at[buf_tok].add(y * buf_w[:, None])
    return out.reshape(b, n, d)


def setup_inputs(seed: int = 0) -> dict:
    key = jax.random.key(seed)
    ks = jax.random.split(key, 32)
    L, D = DEPTH, D_MODEL

    def nrm(k, shape, scale):
        return jax.random.normal(k, shape, jnp.float32) * scale

    return {
        'x': nrm(ks[0], (BATCH, SEQ, D), 1.0),
        'c': nrm(ks[1], (BATCH, D), 1.0),
        'ctx': nrm(ks[2], (BATCH, CTX_LEN, D), 1.0),
        'c_ctx': nrm(ks[3], (D,), 1.0),
        'w_ada': nrm(ks[4], (L, D, 6 * D), 0.5 * D ** -0.5),
        'b_ada': nrm(ks[5], (L, 6 * D), 0.02),
        'w_in': nrm(ks[6], (L, D, IN_WIDTH), D ** -0.5),
        'attn_sink': nrm(ks[7], (L, ATTN_Q_HEADS), 0.5),
        'gm_ln_g': 1.0 + nrm(ks[8], (L, GM_WIDTH), 0.02),
        'gm_ln_b': nrm(ks[9], (L, GM_WIDTH), 0.02),
        'gm_ws': nrm(ks[10], (L, GM_GROUPS, GM_CHUNK, GM_CHUNK), GM_CHUNK ** -0.5),
        'gm_bs': 1.0 + nrm(ks[11], (L, GM_GROUPS, GM_CHUNK), 0.02),
        'w_pa': nrm(ks[12], (L, ATTN_WIDTH, D), BETA * ATTN_WIDTH ** -0.5),
        'w_pb': nrm(ks[13], (L, GM_WIDTH, D), BETA * GM_WIDTH ** -0.5),
        'w_o': nrm(ks[14], (L, D, D), BETA * D ** -0.5),
        'ln1_g': 1.0 + nrm(ks[15], (L, D), 0.02),
        'ln1_b': nrm(ks[16], (L, D), 0.02),
        'router_g_w': nrm(ks[17], (L, D, N_GROUPS), D ** -0.5),
        'router_g_b': nrm(ks[18], (L, N_GROUPS), 0.01),
        'router_e_w': nrm(ks[19], (L, N_GROUPS, D, EXPERTS_PER_GROUP), D ** -0.5),
        'router_e_b': nrm(ks[20], (L, N_GROUPS, EXPERTS_PER_GROUP), 0.01),
        'moe_w1': nrm(ks[21], (L, N_EXPERTS, D, D_EXPERT), D ** -0.5),
        'moe_w3': nrm(ks[22], (L, N_EXPERTS, D, D_EXPERT), D ** -0.5),
        'moe_w2': nrm(ks[23], (L, N_EXPERTS, D_EXPERT, D), BETA * D_EXPERT ** -0.5),
        'ln2_g': 1.0 + nrm(ks[24], (L, D), 0.02),
        'ln2_b': nrm(ks[25], (L, D), 0.02),
    }


def reference(x, c, ctx, c_ctx, w_ada, b_ada, w_in, attn_sink, gm_ln_g, gm_ln_b, gm_ws, gm_bs,
              w_pa, w_pb, w_o, ln1_g, ln1_b, router_g_w, router_g_b, router_e_w, router_e_b,
              moe_w1, moe_w3, moe_w2, ln2_g, ln2_b):
    n_lat = x.shape[1]
    rows = n_lat // GRID_W
    row = jnp.repeat(jnp.arange(rows, dtype=jnp.int32), GRID_W)
    col = jnp.tile(jnp.arange(GRID_W, dtype=jnp.int32), rows)
    rope = _rope_tables(row, x.dtype) + _rope_tables(col, x.dtype)
    mask = _window_mask(n_lat)
    q_scale = HEAD_DIM ** -0.5
    for i in range(DEPTH):
        lp = {'gm_ln_g': gm_ln_g[i], 'gm_ln_b': gm_ln_b[i], 'gm_ws': gm_ws[i], 'gm_bs': gm_bs[i],
              'w_pa': w_pa[i], 'w_pb': w_pb[i], 'w_o': w_o[i],
              'router_g_w': router_g_w[i], 'router_g_b': router_g_b[i],
              'router_e_w': router_e_w[i], 'router_e_b': router_e_b[i],
              'moe_w1': moe_w1[i], 'moe_w3': moe_w3[i], 'moe_w2': moe_w2[i]}
        mod = jax.nn.silu(c) @ w_ada[i] + b_ada[i]
        sh1, sc1, g1, sh2, sc2, g2 = jnp.split(mod[:, None, :], 6, axis=-1)
        cmod = jax.nn.silu(c_ctx) @ w_ada[i] + b_ada[i]
        csh1, csc1, cg1, csh2, csc2, cg2 = jnp.split(cmod, 6, axis=-1)
        hc = _modulate(_layernorm(ctx), csh1, csc1)
        if i < DEPTH - 1:
            qc, k_ctx, v_ctx, uc, vgc, gac, gbc = _split_proj(hc @ w_in[i])
            mix_c = _merge(_context_attention(qc * q_scale, k_ctx, v_ctx, attn_sink[i]),
                           _chunk_gmlp(uc, vgc, lp), gac, gbc, lp)
            ctx = _layernorm(ALPHA * ctx + cg1 * mix_c, ln1_g[i], ln1_b[i])
            hc2 = _modulate(_layernorm(ctx), csh2, csc2)
            ctx = _layernorm(ALPHA * ctx + cg2 * _hier_moe(hc2, lp), ln2_g[i], ln2_b[i])
        else:
            k_ctx, v_ctx = _kv_proj(hc, w_in[i])
        h = _modulate(_layernorm(x), sh1, sc1)
        q, k, v, u, vg, ga, gb = _split_proj(h @ w_in[i])
        q = _rope_2d(q, rope) * q_scale
        k = _rope_2d(k, rope)
        y_attn = _latent_attention(q, k, v, k_ctx, v_ctx, attn_sink[i], mask)
        y_gm = _chunk_gmlp(u, vg, lp)
        mix = _merge(y_attn, y_gm, ga, gb, lp)
        x = _layernorm(ALPHA * x + g1 * mix, ln1_g[i], ln1_b[i])
        h2 = _modulate(_layernorm(x), sh2, sc2)
        x = _layernorm(ALPHA * x + g2 * _hier_moe(h2, lp), ln2_g[i], ln2_b[i])
    return x
```

```python
import numpy as np
from contextlib import ExitStack
import concourse.bass as bass
import concourse.mybir as mybir
from concourse.bass_utils import run_bass_kernel_spmd

F32 = mybir.dt.float32
BF16 = mybir.dt.bfloat16
I32 = mybir.dt.int32
AF = mybir.ActivationFunctionType
ALU = mybir.AluOpType
AX = mybir.AxisListType

import os
KDBG = os.environ.get('KDBG', '')
NT = 32
NSLOT = 128
NPAIR = 64
ALPHA = 2.0 ** 0.25
LN_EPS = 1e-6
GC = 0.7978845608028654


class Buf:
    def __init__(self, name):
        self.name = name
        self.w = None
        self.r = {}
        self.dsem = None
        self.dcount = 0


class EngState:
    def __init__(self, name, eng, sem, selfwait):
        self.name = name
        self.eng = eng
        self.sem = sem
        self.count = 0
        self.seen = {}
        self.selfwait = selfwait


class KB:
    def __init__(self, nc, stack):
        self.nc = nc
        self.stack = stack
        self.E = {}
        for name, eng, sw in (("pe", nc.tensor, False), ("act", nc.scalar, True),
                              ("dve", nc.vector, True), ("pool", nc.gpsimd, True),
                              ("sp", nc.sync, True)):
            sem = stack.enter_context(nc.semaphore("s_" + name))
            self.E[name] = EngState(name, eng, sem, sw)
        self.dma_toks = []

    def new_sem(self, name):
        return self.stack.enter_context(self.nc.semaphore(name))

    def sb(self, name, shape, dt):
        return self.stack.enter_context(self.nc.sbuf_tensor(name, shape, dt))

    def ps(self, name, shape, dt):
        return self.stack.enter_context(self.nc.psum_tensor(name, shape, dt))

    def _wait(self, E, toks):
        need = {}
        for t in toks:
            if t is None:
                continue
            sem, val = t
            if sem is E.sem and not E.selfwait:
                continue
            k = id(sem)
            if val > E.seen.get(k, 0):
                if k not in need or need[k][1] < val:
                    need[k] = (sem, val)
        for k, (sem, val) in need.items():
            E.eng.wait_ge(sem, val)
            E.seen[k] = val

    def _deps(self, reads, writes):
        deps = []
        for b in reads:
            deps.append(b.w)
        for b in writes:
            deps.extend(b.r.values())
            deps.append(b.w)
        return deps

    def _record(self, tok, reads, writes):
        for b in reads:
            k = id(tok[0])
            if k not in b.r or b.r[k][1] < tok[1]:
                b.r[k] = tok
        for b in writes:
            b.w = tok
            b.r = {}

    def op(self, engname, fn, reads=(), writes=(), extra=()):
        E = self.E[engname]
        self._wait(E, self._deps(reads, writes) + list(extra))
        ins = fn(E.eng)
        E.count += 1
        ins.then_inc(E.sem, 1)
        tok = (E.sem, E.count)
        self._record(tok, reads, writes)
        return tok

    def group(self, engname, fns, reads=(), writes=(), extra=()):
        E = self.E[engname]
        self._wait(E, self._deps(reads, writes) + list(extra))
        ins = None
        for fn in fns:
            ins = fn(E.eng)
        E.count += 1
        ins.then_inc(E.sem, 1)
        tok = (E.sem, E.count)
        self._record(tok, reads, writes)
        return tok

    def dma(self, engname, fn, reads=(), writes=(), owner=None, extra=()):
        E = self.E[engname]
        self._wait(E, self._deps(reads, writes) + list(extra))
        if owner is None:
            owner = (list(writes) + list(reads))[0]
        if owner.dsem is None:
            owner.dsem = self.new_sem("d_" + owner.name)
        ins = fn(E.eng)
        owner.dcount += 16
        ins.then_inc(owner.dsem, 16)
        tok = (owner.dsem, owner.dcount)
        self.dma_toks.append(tok)
        self._record(tok, reads, writes)
        return tok

    def wait_tok(self, engname, toks):
        self._wait(self.E[engname], toks)

    def barrier_all(self):
        toks = [(e.sem, e.count) for e in self.E.values() if e.count > 0] + maxtoks(self.dma_toks)
        for e in self.E.values():
            self._wait(e, [t for t in toks if t[0] is not e.sem])
        self.dma_toks = maxtoks(self.dma_toks)


def maxtoks(toks):
    best = {}
    for t in toks:
        if t is None:
            continue
        k = id(t[0])
        if k not in best or best[k][1] < t[1]:
            best[k] = t
    return list(best.values())


def build_nc(stop=None, nta=NT, dbg=False):
    nc = bass.Bass("TRN2", target_bir_lowering=False)

    def din(name, shape, dt=F32):
        return nc.dram_tensor(name, shape, dt, kind="ExternalInput").ap()

    x_d = din("x", [4096, 1024])
    xh_d = din("xh", [256, 1024])
    ctx_d = din("ctx", [256, 1024])
    cT_d = din("cT", [128, 16])
    wada_d = din("w_ada", [1024, 6144])
    bada_d = din("b_ada2", [2, 6144])
    win_d = din("w_in", [1024, 3840])
    sink_d = din("sinkb", [64, 1024])
    gmln_d = din("gmln", [128, 1024])
    wsT_d = din("wsT", [128, 1024])
    bsT_d = din("bsT", [128, 8])
    wpa_d = din("w_pa", [512, 1024])
    wpb_d = din("w_pb", [512, 1024])
    wo_d = din("w_o", [1024, 1024])
    lnp_d = din("lnp", [128, 4096])
    wr_d = din("wr", [1024, 36])
    br_d = din("br", [128, 36])
    w1_d = din("w1", [8192, 2048])
    w3_d = din("w3", [8192, 2048])
    w2_d = din("w2", [8192, 2048])
    rope_d = din("rope", [34 * 128, 128])
    masks_d = din("masks", [128, 512])
    NCST = 512 + 64 + 1
    consts_d = din("consts", [128, NCST])
    out_d = nc.dram_tensor("out", [4096, 1024], F32, kind="ExternalOutput").ap()

    dk = dict(kind="ExternalOutput") if dbg else {}
    x1_d = nc.dram_tensor("x1_scr", [4096, 1024], F32, **dk).ap()
    h2_d = nc.dram_tensor("h2_scr", [4096, 1024], BF16, **dk).ap()
    y_d = nc.dram_tensor("y_scr", [4096, 1024], BF16, **dk).ap()
    dbgA_d = nc.dram_tensor("dbgA", [4096, 1024], F32, **dk).ap() if dbg else None
    mod_d = nc.dram_tensor("mod_scr", [128, 8 * 1024], F32).ap()
    wq_d = [nc.dram_tensor("wq%d_scr" % i, [8192, 2048], BF16).ap() for i in range(3)]
    xs_d = nc.dram_tensor("xs_scr", [NSLOT * 128, 1024], BF16).ap()
    ys_d = nc.dram_tensor("ys_scr", [NSLOT * 128, 1024], F32).ap()

    with ExitStack() as st:
        kb = KB(nc, st)

        def mkp(ph):
            def mk(name, shape, dt):
                return ph.enter_context(nc.sbuf_tensor(name, shape, dt)), Buf(name)
            return mk

        mkg = mkp(st)

        PS = []
        for i in range(4):
            t = kb.ps("ps%d" % i, [128, 1024], F32)
            PS.append((t, Buf("ps%d" % i)))
        psi = [0]

        def nextps():
            r = PS[psi[0] % 4]
            psi[0] += 1
            return r

        def bfv(t):
            return t[:].bitcast(BF16)

        cst_f, b_cst_f = mkg("cst_f", [128, NCST], F32)
        kb.dma("sp", lambda e: e.dma_start(out=cst_f[:], in_=consts_d), writes=[b_cst_f])
        ident_f = cst_f[:, 0:128]
        S128 = 512
        PIDX = 512 + 64
        cst_b, b_cst_b = mkg("cst_b", [128, 384], BF16)
        kb.op("dve", lambda e: e.tensor_copy(out=cst_b[:], in_=cst_f[:, 0:384]), reads=[b_cst_f], writes=[b_cst_b])
        ident_b = cst_b[:, 0:128]
        tri_b = cst_b[:, 128:256]
        ones_b = cst_b[:, 256:384]

        st12, b_st12 = mkg("st12", [128, 12], F32)
        mv, b_mv = mkg("mv", [128, 2], F32)
        nw, b_nw = mkg("nw", [128, 4], F32)
        lnt, b_lnt = mkg("lnt", [128, 1024], F32)
        Cc, b_Cc = mkg("Cc", [128, 32], F32)
        A12all, b_A12all = mkg("A12all", [128, NT, 64], BF16)
        RK, b_RK = mkg("RK", [128, 4, NT], F32)
        posi, b_posi = mkg("posi", [128, 2, NT], I32)

        def rstd_newton(eps):
            kb.op("dve", lambda e: e.tensor_scalar(out=nw[:, 0:1], in0=mv[:, 1:2], scalar1=eps, scalar2=None, op0=ALU.add),
                  reads=[b_mv], writes=[b_nw])
            kb.op("dve", lambda e: e.tensor_scalar(out=nw[:, 2:3], in0=nw[:, 0:1], scalar1=0.5, scalar2=0.5, op0=ALU.mult, op1=ALU.add),
                  reads=[b_nw], writes=[b_nw])
            kb.op("dve", lambda e: e.reciprocal(out=nw[:, 1:2], in_=nw[:, 2:3]), reads=[b_nw], writes=[b_nw])
            for _ in range(4):
                kb.op("dve", lambda e: e.tensor_tensor(out=nw[:, 2:3], in0=nw[:, 1:2], in1=nw[:, 1:2], op=ALU.mult),
                      reads=[b_nw], writes=[b_nw])
                kb.op("dve", lambda e: e.scalar_tensor_tensor(out=nw[:, 2:3], in0=nw[:, 2:3], scalar=-0.5, in1=nw[:, 0:1],
                                                              op0=ALU.mult, op1=ALU.mult), reads=[b_nw], writes=[b_nw])
                kb.op("dve", lambda e: e.scalar_tensor_tensor(out=nw[:, 1:2], in0=nw[:, 2:3], scalar=1.5, in1=nw[:, 1:2],
                                                              op0=ALU.add, op1=ALU.mult), reads=[b_nw], writes=[b_nw])

        def layernorm(src, b_src, n, A, B, rA, dst, b_dst, eps=LN_EPS):
            for i in range(n // 512):
                kb.op("dve", lambda e, i=i: e.bn_stats(out=st12[:, 6 * i:6 * i + 6], in_=src[:, i * 512:(i + 1) * 512]),
                      reads=[b_src], writes=[b_st12])
            kb.op("dve", lambda e: e.bn_aggr(out=mv[:], in_=st12[:, 0:6 * (n // 512)]), reads=[b_st12], writes=[b_mv])
            rstd_newton(eps)
            kb.op("dve", lambda e: e.scalar_tensor_tensor(out=lnt[:, 0:n], in0=src[:, 0:n], scalar=mv[:, 0:1], in1=A,
                                                          op0=ALU.subtract, op1=ALU.mult),
                  reads=[b_src, b_mv] + rA, writes=[b_lnt])
            kb.op("dve", lambda e: e.scalar_tensor_tensor(out=dst, in0=lnt[:, 0:n], scalar=nw[:, 1:2], in1=B,
                                                          op0=ALU.mult, op1=ALU.add),
                  reads=[b_lnt, b_nw] + rA, writes=[b_dst])

        def transposes(src_aps, rsrc, identity, dst, b_dst, dt_bf=True, pst=None):
            n = len(src_aps)
            pt, b_pt = nextps() if pst is None else pst
            pv = bfv(pt) if dt_bf else pt
            kb.group("pe", [lambda e, i=i, a=a: e.transpose(pv[:, i * 128:(i + 1) * 128], a, identity)
                            for i, a in enumerate(src_aps)], reads=rsrc + [b_cst_b, b_cst_f], writes=[b_pt])
            kb.op("act", lambda e: e.copy(out=dst, in_=pv[:, 0:n * 128]), reads=[b_pt], writes=[b_dst])

        mod_toks = []
        with ExitStack() as ph:
            mk = mkp(ph)
            zt, b_zt = mk("zt", [128, 8192], BF16)
            kb.op("pool", lambda e: e.memset(zt[:], 0.0), writes=[b_zt])
            xs_v = xs_d.rearrange("(p a) n -> p (a n)", p=128)
            b_zfill = Buf("zfill")
            zf_tok = None
            for i in range(16):
                zf_tok = kb.dma("sp", lambda e, i=i: e.dma_start(out=xs_v[:, i * 8192:(i + 1) * 8192], in_=zt[:]),
                                reads=[b_zt], owner=b_zfill)
            cT_f, b_cT = mk("cT_f", [128, 16], F32)
            kb.dma("sp", lambda e: e.dma_start(out=cT_f[:], in_=cT_d), writes=[b_cT])
            cth, b_cth = mk("cth", [128, 16], F32)
            kb.op("act", lambda e: e.activation(out=cth[:], in_=cT_f[:], func=AF.Tanh, scale=0.5), reads=[b_cT], writes=[b_cth])
            kb.op("dve", lambda e: e.scalar_tensor_tensor(out=cth[:], in0=cth[:], scalar=1.0, in1=cT_f[:], op0=ALU.add, op1=ALU.mult),
                  reads=[b_cth, b_cT], writes=[b_cth])
            sT_b, b_sT = mk("sT_b", [128, 16], F32)
            kb.op("dve", lambda e: e.tensor_scalar(out=sT_b[:], in0=cth[:], scalar1=0.5, scalar2=None, op0=ALU.mult),
                  reads=[b_cth], writes=[b_sT])
            sel, b_sel = mk("sel", [2, 256], F32)
            kb.op("pool", lambda e: e.memset(sel[:], 0.0), writes=[b_sel])
            kb.op("pool", lambda e: e.memset(sel[0:1, 0:128], 1.0), reads=[b_sel], writes=[b_sel])
            kb.dma("sp", lambda e: e.dma_start(out=sel[1:2, 128:256], in_=consts_d[0:1, 256:384]), reads=[b_sel], writes=[b_sel])
            wada_v = wada_d.rearrange("(k p) n -> p k n", p=128)
            wab = [mk("wab%d" % i, [128, 8, 512], F32) for i in range(2)]
            bad = [mk("bad%d" % i, [2, 512], F32) for i in range(2)]
            mrow = [mk("modrow%d" % i, [2, 512], F32) for i in range(2)]
            mts = [mk("mt%d" % i, [128, 1024], F32) for i in range(2)]
            for ch in range(12):
                wa, b_wa = wab[ch % 2]
                bd, b_bd = bad[ch % 2]
                modrow, b_modrow = mrow[ch % 2]
                mt, b_mt = mts[ch % 2]
                kb.dma("sp", lambda e, ch=ch, wa=wa: e.dma_start(out=wa[:], in_=wada_v[:, :, ch * 512:(ch + 1) * 512]), writes=[b_wa])
                kb.dma("sp", lambda e, ch=ch, bd=bd: e.dma_start(out=bd[:], in_=bada_d[:, ch * 512:(ch + 1) * 512]), writes=[b_bd])
                pt, b_pt = nextps()
                kb.group("pe", [lambda e, k=k, wa=wa, pt=pt: e.matmul(pt[0:2, 0:512], lhsT=sT_b[:, 2 * k:2 * k + 2], rhs=wa[:, k, :],
                                                                       start=(k == 0), stop=(k == 7)) for k in range(8)],
                         reads=[b_sT, b_wa], writes=[b_pt])
                vec = ch // 2
                addc = 1.0 if vec in (1, 4) else 0.0
                kb.op("dve", lambda e, pt=pt, bd=bd, modrow=modrow, addc=addc: e.scalar_tensor_tensor(
                    out=modrow[:], in0=pt[0:2, 0:512], scalar=addc, in1=bd[:], op0=ALU.add, op1=ALU.add),
                    reads=[b_pt, b_bd], writes=[b_modrow])
                pt2, b_pt2 = nextps()
                fns = [lambda e, pt2=pt2, modrow=modrow: e.matmul(pt2[:, 0:512], lhsT=sel[:, 0:128], rhs=modrow[:], start=True, stop=True)]
                if vec < 2:
                    fns.append(lambda e, pt2=pt2, modrow=modrow: e.matmul(pt2[:, 512:1024], lhsT=sel[:, 128:256], rhs=modrow[:],
                                                                          start=True, stop=True))
                kb.group("pe", fns, reads=[b_sel, b_modrow], writes=[b_pt2])
                sc = 0.5 if vec == 2 else 1.0
                kb.op("act", lambda e, pt2=pt2, mt=mt, sc=sc: e.activation(out=mt[:], in_=pt2[:], func=AF.Copy, scale=sc),
                      reads=[b_pt2], writes=[b_mt])
                col = vec * 1024 + (ch % 2) * 512
                mod_toks.append(kb.dma("sp", lambda e, mt=mt, col=col: e.dma_start(out=mod_d[:, col:col + 512], in_=mt[:, 0:512]), reads=[b_mt]))
                if vec < 2:
                    col2 = (6 + vec) * 1024 + (ch % 2) * 512
                    mod_toks.append(kb.dma("sp", lambda e, mt=mt, col2=col2: e.dma_start(out=mod_d[:, col2:col2 + 512], in_=mt[:, 512:1024]),
                                           reads=[b_mt]))
            kb.barrier_all()
        modt = maxtoks(mod_toks)
        if stop == "S":
            return nc

        y_toks = []
        conv_toks = []
        with ExitStack() as ph:
            mk = mkp(ph)
            msk_b, b_msk = mk("msk_b", [128, 512], BF16)
            kb.dma("pool", lambda e: e.dma_start(out=msk_b[:], in_=masks_d), writes=[b_msk])
            gmln_t, b_gmln = mk("gmln_t", [128, 1024], F32)
            kb.dma("sp", lambda e: e.dma_start(out=gmln_t[:], in_=gmln_d), writes=[b_gmln])
            bsT_t, b_bsT = mk("bsT_t", [128, 8], F32)
            kb.dma("sp", lambda e: e.dma_start(out=bsT_t[:], in_=bsT_d), writes=[b_bsT])
            esink, b_esink = mk("esink", [64, 1024], F32)
            kb.dma("sp", lambda e: e.dma_start(out=esink[:], in_=sink_d), writes=[b_esink])
            kb.op("act", lambda e: e.activation(out=esink[:], in_=esink[:], func=AF.Exp), reads=[b_esink], writes=[b_esink])
            wsT_b, b_wsT = mk("wsT_b", [128, 1024], BF16)
            kb.dma("pool", lambda e: e.dma_start(out=wsT_b[:], in_=wsT_d), writes=[b_wsT])
            win_b, b_win = mk("win_b", [128, 8, 3840], BF16)
            win_v = win_d.rearrange("(k p) n -> p k n", p=128)
            for k in range(8):
                for hf in range(2):
                    kb.dma("pool", lambda e, k=k, hf=hf: e.dma_start(out=win_b[:, k, hf * 1920:(hf + 1) * 1920],
                                                                      in_=win_v[:, k, hf * 1920:(hf + 1) * 1920]), writes=[b_win])
            wpa_b, b_wpa = mk("wpa_b", [64, 8, 1024], BF16)
            kb.dma("pool", lambda e: e.dma_start(out=wpa_b[:], in_=wpa_d.rearrange("(h p) n -> p h n", p=64)), writes=[b_wpa])
            wpb_b, b_wpb = mk("wpb_b", [128, 4, 1024], BF16)
            kb.dma("pool", lambda e: e.dma_start(out=wpb_b[:], in_=wpb_d.rearrange("(k p) n -> p k n", p=128)), writes=[b_wpb])
            modA, b_modA = mk("modA", [128, 2, 1024], F32)
            kb.dma("sp", lambda e: e.dma_start(out=modA[:].rearrange("p a n -> p (a n)"), in_=mod_d[:, 0:2048]), writes=[b_modA], extra=modt)
            KT, b_KT = mk("KT", [128, 34 * 128], BF16)
            VV, b_VV = mk("VV", [128, 34, 128], BF16)
            KTc, b_KTc = mk("KTc", [128, 256], BF16)
            VVc, b_VVc = mk("VVc", [128, 2, 128], BF16)
            xts = [mk("xt%d" % i, [128, 1024], F32) for i in range(2)]
            hb, b_hb = mk("hb", [128, 1024], BF16)
            hTs = [mk("hT%d" % i, [128, 1024], BF16) for i in range(2)]
            ropes = [mk("rope%d" % i, [128, 128], F32) for i in range(2)]
            rt1, b_rt1 = mk("rt1", [128, 512], F32)
            rt2, b_rt2 = mk("rt2", [128, 512], F32)
            rq, b_rq = mk("rq", [128, 512], BF16)
            rk, b_rk = mk("rk", [128, 128], BF16)
            QT, b_QT = mk("QT", [128, 512], BF16)
            gg, b_gg = mk("gg", [128, 1024], F32)
            gsq, b_gsq = mk("gsq", [128, 1024], F32)
            tgs = [mk("tg%d" % i, [128, 1024], F32) for i in range(2)]
            ETs = [mk("ET%d" % i, [128, 512], BF16) for i in range(4)]
            eti = [0]
            dens, b_dens = rt2[0:64, :], b_rt2
            oT, b_oT = mk("oT", [64, 8, 128], BF16)
            vgm, b_vgm = mk("vgm", [128, 512], BF16)
            spb, b_spb = rt1, b_rt1
            ygm, b_ygm = mk("ygm", [128, 512], BF16)
            ygT, b_ygT = mk("ygT", [128, 512], BF16)
            yp1, b_yp1 = tgs[0]
            y2bs = [mk("y2b%d" % i, [128, 1024], BF16) for i in range(2)]

            rsrc, b_rsrc = mk("rsrc", [128, 512], F32)

            def rope_apply(src_ps, b_srcs_ps, nh, tab, b_tab, dst, b_dst, view=None):
                n = nh * 64
                kb.op("act", lambda e: e.copy(out=rsrc[:, 0:n], in_=src_ps), reads=b_srcs_ps, writes=[b_rsrc])
                src = rsrc[:, 0:n]
                b_srcs = [b_rsrc]
                cosb = tab[:, 0:64].unsqueeze(1).to_broadcast([128, nh, 64])
                kb.op("dve", lambda e: e.tensor_tensor(out=rt1[:, 0:n].rearrange("p (h d) -> p h d", h=nh),
                                                       in0=src.rearrange("p (h d) -> p h d", h=nh), in1=cosb, op=ALU.mult),
                      reads=b_srcs + [b_tab], writes=[b_rt1])
                sv = src.rearrange("p (g a d) -> p g a d", a=2, d=16)
                tv = rt2[:, 0:n].rearrange("p (g a d) -> p g a d", a=2, d=16)
                sn = tab[:, 64:128].rearrange("p (x a d) -> p x a d", a=2, d=16)
                for a in range(2):
                    o = tv[:, :, a, :].rearrange("p (h x) d -> p h x d", x=2)
                    i0 = sv[:, :, 1 - a, :].rearrange("p (h x) d -> p h x d", x=2)
                    i1 = sn[:, :, a, :].unsqueeze(1).to_broadcast([128, nh, 2, 16])
                    kb.op("dve", lambda e, o=o, i0=i0, i1=i1: e.tensor_tensor(out=o, in0=i0, in1=i1, op=ALU.mult),
                          reads=b_srcs + [b_tab], writes=[b_rt2])
                a1, a2 = rt1[:, 0:n], rt2[:, 0:n]
                if view is not None:
                    a1, a2 = view(a1), view(a2)
                kb.op("dve", lambda e: e.tensor_tensor(out=dst, in0=a1, in1=a2, op=ALU.add),
                      reads=[b_rt1, b_rt2], writes=[b_dst])

            def ln_mod_T(xt, b_xt, A, B, rA, i2):
                hT, b_hT = hTs[i2]
                layernorm(xt, b_xt, 1024, A, B, rA, hb[:], b_hb)
                transposes([hb[:, k * 128:(k + 1) * 128] for k in range(8)], [b_hb], ident_b, hT[:], b_hT)
                return hT, b_hT

            def proj_kv(hT, b_hT):
                pt, b_pt = nextps()
                kb.group("pe", [lambda e, k=k: e.matmul(pt[:, 0:256], lhsT=hT[:, k * 128:(k + 1) * 128], rhs=win_b[:, k, 512:768],
                                                        start=(k == 0), stop=(k == 7)) for k in range(8)],
                         reads=[b_hT, b_win], writes=[b_pt])
                return pt, b_pt

            cm, b_cm = gg, b_gg
            cs_, b_cs_ = gsq, b_gsq
            kb.dma("sp", lambda e: e.dma_start(out=cm[:], in_=mod_d[:, 7 * 1024:8 * 1024]), writes=[b_cm], extra=modt)
            kb.dma("sp", lambda e: e.dma_start(out=cs_[:], in_=mod_d[:, 6 * 1024:7 * 1024]), writes=[b_cs_], extra=modt)
            for ci in range(2):
                xt, b_xt = xts[ci % 2]
                kb.dma("sp", lambda e, ci=ci, xt=xt: e.dma_start(out=xt[:], in_=ctx_d[ci * 128:(ci + 1) * 128, :]), writes=[b_xt])
                hT, b_hT = ln_mod_T(xt, b_xt, cm[:], cs_[:], [b_cm, b_cs_], ci % 2)
                pt, b_pt = proj_kv(hT, b_hT)
                kb.op("act", lambda e, pt=pt: e.copy(out=rk[:], in_=pt[:, 0:128]), reads=[b_pt], writes=[b_rk])
                kb.op("act", lambda e, pt=pt, ci=ci: e.copy(out=VVc[:, ci, :], in_=pt[:, 128:256]), reads=[b_pt], writes=[b_VVc])
                transposes([rk[:]], [b_rk], ident_b, KTc[:, ci * 128:(ci + 1) * 128], b_KTc)

            class TS:
                pass

            def stage_kv(t):
                S = TS()
                S.t = t
                slot = t + 1
                xt, b_xt = xts[slot % 2]
                if t < 0:
                    src = xh_d[0:128, :]
                elif t >= NT:
                    src = xh_d[128:256, :]
                else:
                    src = x_d[t * 128:(t + 1) * 128, :]
                kb.dma("pool", lambda e: e.dma_start(out=xt[:], in_=src), writes=[b_xt])
                tab, b_tab = ropes[slot % 2]
                kb.dma("pool", lambda e: e.dma_start(out=tab[:], in_=rope_d[slot * 128:(slot + 1) * 128, :]), writes=[b_tab])
                S.tab, S.b_tab = tab, b_tab
                S.hT, S.b_hT = ln_mod_T(xt, b_xt, modA[:, 1, :], modA[:, 0, :], [b_modA], slot % 2)
                kvp, b_kvp = proj_kv(S.hT, S.b_hT)
                if KDBG == "norope":
                    kb.op("act", lambda e: e.copy(out=rk[:], in_=kvp[:, 0:128]), reads=[b_kvp], writes=[b_rk])
                else:
                    rope_apply(kvp[:, 0:128], [b_kvp], 2, tab, b_tab, rk[:], b_rk)
                kb.op("act", lambda e: e.copy(out=VV[:, slot, :], in_=kvp[:, 128:256]), reads=[b_kvp], writes=[b_VV])
                transposes([rk[:]], [b_rk], ident_b, KT[:, slot * 128:(slot + 1) * 128], b_KT)
                return S

            def stage_pre(S):
                t = S.t
                slot = t + 1
                hT, b_hT = S.hT, S.b_hT
                for gi in range(2):
                    pg, b_pg = PS[gi]
                    tg, b_tg = tgs[gi]
                    kb.group("pe", [lambda e, k=k, g=g, gi=gi, pg=pg: e.matmul(
                        pg[:, g * 512:(g + 1) * 512], lhsT=hT[:, k * 128:(k + 1) * 128],
                        rhs=win_b[:, k, 1792 + gi * 1024 + g * 512:1792 + gi * 1024 + (g + 1) * 512],
                        start=(k == 0), stop=(k == 7)) for g in range(2) for k in range(8)],
                        reads=[b_hT, b_win], writes=[b_pg])
                    kb.op("act", lambda e, pg=pg, tg=tg: e.activation(out=tg[:], in_=pg[:], func=AF.Tanh, scale=0.5), reads=[b_pg], writes=[b_tg])
                pq, b_pq = PS[2]
                kb.group("pe", [lambda e, k=k: e.matmul(pq[:, 0:512], lhsT=hT[:, k * 128:(k + 1) * 128], rhs=win_b[:, k, 0:512],
                                                        start=(k == 0), stop=(k == 7)) for k in range(8)],
                         reads=[b_hT, b_win], writes=[b_pq])
                rope_apply(pq[:, 0:512], [b_pq], 8, S.tab, S.b_tab, rq[:].rearrange("p (c a d) -> p a c d", a=2, d=64), b_rq,
                           view=lambda ap: ap.rearrange("p (a c d) -> p a c d", a=2, d=64))
                transposes([rq[:, c * 128:(c + 1) * 128] for c in range(4)], [b_rq], ident_b, QT[:], b_QT, pst=PS[2])
                puv, b_puv = PS[3]
                kb.group("pe", [lambda e, k=k, g=g: e.matmul(puv[:, g * 512:(g + 1) * 512], lhsT=hT[:, k * 128:(k + 1) * 128],
                                                             rhs=win_b[:, k, 768 + g * 512:768 + (g + 1) * 512],
                                                             start=(k == 0), stop=(k == 7)) for g in range(2) for k in range(8)],
                         reads=[b_hT, b_win], writes=[b_puv])
                kb.op("act", lambda e: e.activation(out=gsq[:], in_=puv[:], func=AF.Square), reads=[b_puv], writes=[b_gsq])
                kb.op("dve", lambda e: e.tensor_scalar(out=gsq[:], in0=gsq[:], scalar1=0.044715, scalar2=1.0, op0=ALU.mult, op1=ALU.add),
                      reads=[b_gsq], writes=[b_gsq])
                kb.op("dve", lambda e: e.tensor_tensor(out=gsq[:], in0=gsq[:], in1=puv[:], op=ALU.mult),
                      reads=[b_gsq, b_puv], writes=[b_gsq])
                kb.op("act", lambda e: e.activation(out=gsq[:], in_=gsq[:], func=AF.Tanh, scale=GC), reads=[b_gsq], writes=[b_gsq])
                kb.op("dve", lambda e: e.scalar_tensor_tensor(out=gg[:], in0=gsq[:], scalar=1.0, in1=puv[:], op0=ALU.add, op1=ALU.mult),
                      reads=[b_gsq, b_puv], writes=[b_gg])
                layernorm(gg[:, 512:1024], b_gg, 512, gmln_t[:, 0:512], gmln_t[:, 512:1024], [b_gmln], vgm[:], b_vgm, eps=4.0 * LN_EPS)

            def stage_main(S):
                t = S.t
                slot = t + 1
                hT, b_hT = S.hT, S.b_hT
                po = [PS[0], PS[1]]
                blocks = [("c", 0), ("c", 1), ("l", slot - 1), ("l", slot), ("l", slot + 1)]
                steps = [(g, bi) for g in range(2) for bi in range(5)]

                def kv_of(g, bi):
                    kind, idx = blocks[bi]
                    if kind == "c":
                        return (KTc[g * 64:(g + 1) * 64, idx * 128:(idx + 1) * 128], VVc[:, idx, g * 64:(g + 1) * 64], [b_KTc, b_VVc])
                    return (KT[g * 64:(g + 1) * 64, idx * 128:(idx + 1) * 128], VV[:, idx, g * 64:(g + 1) * 64], [b_KT, b_VV])

                def issue_S(si):
                    g, bi = steps[si]
                    kt, vt, rkv = kv_of(g, bi)
                    pS, b_pS = PS[2 + (si % 2)]
                    kb.op("pe", lambda e: e.matmul(pS[:, 0:512], lhsT=kt, rhs=QT[g * 64:(g + 1) * 64, :], start=True, stop=True),
                          reads=rkv + [b_QT], writes=[b_pS])

                issue_S(0)
                for si, (g, bi) in enumerate(steps):
                    if si + 1 < len(steps):
                        issue_S(si + 1)
                    pog, b_pog = po[g]
                    pS, b_pS = PS[2 + (si % 2)]
                    kt, vt, rkv = kv_of(g, bi)
                    ET, b_ET = ETs[eti[0] % 4]
                    eti[0] += 1
                    kb.op("act", lambda e, ET=ET, pS=pS: e.activation(out=ET[:], in_=pS[:, 0:512], func=AF.Exp, scale=0.125),
                          reads=[b_pS], writes=[b_ET])
                    mi = None
                    if bi == 2:
                        mi = 2 if t == 0 else 0
                    if bi == 4:
                        mi = 3 if t == NT - 1 else 1
                    if mi is not None:
                        mb = msk_b[:, mi * 128:(mi + 1) * 128].unsqueeze(1).to_broadcast([128, 4, 128])
                        kb.op("dve", lambda e, ET=ET, mb=mb: e.tensor_tensor(out=ET[:].rearrange("p (h q) -> p h q", h=4),
                                                                           in0=ET[:].rearrange("p (h q) -> p h q", h=4), in1=mb, op=ALU.mult),
                              reads=[b_ET, b_msk], writes=[b_ET])
                    fns = [lambda e, h=h, ET=ET, vt=vt, pog=pog, bi=bi: e.matmul(pog[0:64, h * 128:(h + 1) * 128], lhsT=vt,
                                                                              rhs=ET[:, h * 128:(h + 1) * 128],
                                                                              start=(bi == 0 and h == 0), stop=(bi == 4 and h == 3),
                                                                              skip_group_check=True) for h in range(4)]
                    fns.append(lambda e, ET=ET, pog=pog, bi=bi: e.matmul(pog[0:64, 512:1024], lhsT=ones_b[:, 0:64], rhs=ET[:],
                                                                       start=(bi == 0), stop=(bi == 4), skip_group_check=True))
                    kb.group("pe", fns, reads=rkv + [b_ET, b_cst_b], writes=[b_pog])
                    if bi == 4:
                        kb.op("dve", lambda e, pog=pog, g=g: e.tensor_tensor(out=dens[:], in0=pog[0:64, 512:1024],
                                                                           in1=esink[:, g * 512:(g + 1) * 512], op=ALU.add),
                              reads=[b_pog, b_esink], writes=[b_dens])
                        kb.op("dve", lambda e: e.reciprocal(out=dens[:], in_=dens[:]), reads=[b_dens], writes=[b_dens])
                        kb.op("dve", lambda e, pog=pog, g=g: e.tensor_tensor(out=oT[:, g * 4:(g + 1) * 4, :].rearrange("p h q -> p (h q)"),
                                                                           in0=pog[0:64, 0:512], in1=dens[:], op=ALU.mult),
                              reads=[b_pog, b_dens], writes=[b_oT])
                pya, b_pya = nextps()
                kb.group("pe", [lambda e, h=h, hf=hf: e.matmul(pya[:, hf * 512:(hf + 1) * 512], lhsT=oT[:, h, :],
                                                               rhs=wpa_b[:, h, hf * 512:(hf + 1) * 512], start=(h == 0), stop=(h == 7))
                                for hf in range(2) for h in range(8)], reads=[b_oT, b_wpa], writes=[b_pya])

                kb.op("dve", lambda e: e.scalar_tensor_tensor(out=yp1[:], in0=yp1[:], scalar=1.0, in1=pya[:], op0=ALU.add, op1=ALU.mult),
                      reads=[b_yp1, b_pya], writes=[b_yp1])
                if dbg:
                    y_toks.append(kb.dma("sp", lambda e: e.dma_start(out=dbgA_d[t * 128:(t + 1) * 128, :], in_=yp1[:]), reads=[b_yp1]))
                psp, b_psp = nextps()
                kb.group("pe", [lambda e, g=g: e.matmul(psp[:, g * 64:(g + 1) * 64], lhsT=wsT_b[:, g * 128:(g + 1) * 128],
                                                        rhs=vgm[:, g * 64:(g + 1) * 64], start=True, stop=True) for g in range(8)],
                         reads=[b_wsT, b_vgm], writes=[b_psp])
                bsb = bsT_t[:, 0:8].unsqueeze(2).to_broadcast([128, 8, 64])
                kb.op("dve", lambda e: e.tensor_tensor(out=spb[:].rearrange("p (g c) -> p g c", g=8),
                                                       in0=psp[:, 0:512].rearrange("p (g c) -> p g c", g=8), in1=bsb, op=ALU.add),
                      reads=[b_psp, b_bsT], writes=[b_spb])
                kb.op("dve", lambda e: e.scalar_tensor_tensor(out=ygm[:], in0=spb[:], scalar=0.5, in1=gg[:, 0:512], op0=ALU.mult, op1=ALU.mult),
                      reads=[b_spb, b_gg], writes=[b_ygm])
                transposes([ygm[:, k * 128:(k + 1) * 128] for k in range(4)], [b_ygm], ident_b, ygT[:], b_ygT)
                pyb, b_pyb = nextps()
                kb.group("pe", [lambda e, k=k, hf=hf: e.matmul(pyb[:, hf * 512:(hf + 1) * 512], lhsT=ygT[:, k * 128:(k + 1) * 128],
                                                               rhs=wpb_b[:, k, hf * 512:(hf + 1) * 512], start=(k == 0), stop=(k == 3))
                                for hf in range(2) for k in range(4)], reads=[b_ygT, b_wpb], writes=[b_pyb])
                kb.op("dve", lambda e: e.scalar_tensor_tensor(out=tgs[1][0][:], in0=tgs[1][0][:], scalar=1.0, in1=pyb[:], op0=ALU.add, op1=ALU.mult),
                      reads=[tgs[1][1], b_pyb], writes=[tgs[1][1]])
                y2b, b_y2b = y2bs[t % 2]
                kb.op("dve", lambda e: e.tensor_tensor(out=y2b[:], in0=yp1[:], in1=tgs[1][0][:], op=ALU.add),
                      reads=[b_yp1, tgs[1][1]], writes=[b_y2b])
                y_toks.append(kb.dma("sp", lambda e: e.dma_start(out=y_d[t * 128:(t + 1) * 128, :], in_=y2b[:]), reads=[b_y2b]))

            if stop == "A0":
                kb.barrier_all()
                return nc
            stage_kv(-1)
            Scur = stage_kv(0)
            if stop == "A1":
                kb.barrier_all()
                return nc
            b_conv = Buf("wconv")
            conv_jobs = [(mi, j) for mi in range(3) for j in range(16)]
            wsrc = [w1_d, w3_d, w2_d]
            for t in range(nta):
                stage_pre(Scur)
                Snext = stage_kv(t + 1)
                for _ in range(2):
                    if conv_jobs:
                        mi, j = conv_jobs.pop(0)
                        conv_toks.append(kb.dma("pool", lambda e, mi=mi, j=j: e.dma_start(
                            out=wq_d[mi][j * 512:(j + 1) * 512, :], in_=wsrc[mi][j * 512:(j + 1) * 512, :]), owner=b_conv, reads=[b_conv]))
                stage_main(Scur)
                Scur = Snext
            kb.barrier_all()
            if stop == "A":
                return nc

        x1_toks = []
        h2_toks = []
        with ExitStack() as ph:
            mk = mkp(ph)
            wo_b, b_wo = mk("wo_b", [128, 8, 1024], BF16)
            kb.dma("pool", lambda e: e.dma_start(out=wo_b[:], in_=wo_d.rearrange("(k p) n -> p k n", p=128)), writes=[b_wo])
            modB, b_modB = mk("modB", [128, 3, 1024], F32)
            kb.dma("sp", lambda e: e.dma_start(out=modB[:].rearrange("p a n -> p (a n)"), in_=mod_d[:, 2048:5120]), writes=[b_modB])
            lnB, b_lnB = mk("lnB", [128, 2, 1024], F32)
            kb.dma("sp", lambda e: e.dma_start(out=lnB[:].rearrange("p a n -> p (a n)"), in_=lnp_d[:, 0:2048]), writes=[b_lnB])
            br_t, b_br = mk("br_t", [128, 36], F32)
            kb.dma("sp", lambda e: e.dma_start(out=br_t[:], in_=br_d), writes=[b_br])
            wr_t, b_wr = mk("wr_t", [128, 8, 36], F32)
            kb.dma("sp", lambda e: e.dma_start(out=wr_t[:], in_=wr_d.rearrange("(k p) n -> p k n", p=128)), writes=[b_wr])
            kb.op("pool", lambda e: e.memset(Cc[:], 0.0), writes=[b_Cc])
            xts = [mk("xtB%d" % i, [128, 1024], F32) for i in range(3)]
            ybs = [mk("ybB%d" % i, [128, 1024], BF16) for i in range(3)]
            yT, b_yT = mk("yT", [128, 1024], BF16)
            z1, b_z1 = mk("z1", [128, 1024], F32)
            x1s = [mk("x1_%d" % i, [128, 1024], F32) for i in range(2)]
            h2f, b_h2f = mk("h2f", [128, 1024], F32)
            h2bs = [mk("h2b%d" % i, [128, 1024], BF16) for i in range(2)]
            h2T, b_h2T = mk("h2T", [128, 1024], F32)
            lg, b_lg = mk("lg", [128, 36], F32)
            rs, b_rs = mk("rs", [128, 96], F32)
            A1f, b_A1f = mk("A1f", [128, 64], F32)
            cs, b_cs = mk("cs", [128, 96], F32)
            yt_ = maxtoks(y_toks)
            def loadB(t):
                xt, b_xt = xts[t % 3]
                kb.dma("pool", lambda e: e.dma_start(out=xt[:], in_=x_d[t * 128:(t + 1) * 128, :]), writes=[b_xt])
                yb, b_yb = ybs[t % 3]
                kb.dma("pool", lambda e: e.dma_start(out=yb[:], in_=y_d[t * 128:(t + 1) * 128, :]), writes=[b_yb], extra=yt_)

            def frontB(t):
                yb, b_yb = ybs[t % 3]
                transposes([yb[:, k * 128:(k + 1) * 128] for k in range(8)], [b_yb], ident_b, yT[:], b_yT, pst=PS[2])
                pmx, b_pmx = PS[t % 2]
                kb.group("pe", [lambda e, k=k, hf=hf: e.matmul(pmx[:, hf * 512:(hf + 1) * 512], lhsT=yT[:, k * 128:(k + 1) * 128],
                                                               rhs=wo_b[:, k, hf * 512:(hf + 1) * 512], start=(k == 0), stop=(k == 7))
                                for hf in range(2) for k in range(8)], reads=[b_yT, b_wo], writes=[b_pmx])

            def backB(t):
                xt, b_xt = xts[t % 3]
                pmx, b_pmx = PS[t % 2]
                kb.op("dve", lambda e: e.tensor_tensor(out=z1[:], in0=pmx[:], in1=modB[:, 0, :], op=ALU.mult),
                      reads=[b_pmx, b_modB], writes=[b_z1])
                kb.op("dve", lambda e: e.scalar_tensor_tensor(out=z1[:], in0=xt[:], scalar=ALPHA, in1=z1[:], op0=ALU.mult, op1=ALU.add),
                      reads=[b_xt, b_z1], writes=[b_z1])
                x1, b_x1 = x1s[t % 2]
                layernorm(z1, b_z1, 1024, lnB[:, 0, :], lnB[:, 1, :], [b_lnB], x1[:], b_x1)
                x1_toks.append(kb.dma("sp", lambda e: e.dma_start(out=x1_d[t * 128:(t + 1) * 128, :], in_=x1[:]), reads=[b_x1]))
                layernorm(x1, b_x1, 1024, modB[:, 2, :], modB[:, 1, :], [b_modB], h2f[:], b_h2f)
                h2b, b_h2b = h2bs[t % 2]
                kb.op("act", lambda e: e.copy(out=h2b[:].rearrange("t (j p) -> t p j", j=8),
                                              in_=h2f[:].rearrange("t (p j) -> t p j", j=8)), reads=[b_h2f], writes=[b_h2b])
                h2_toks.append(kb.dma("sp", lambda e: e.dma_start(out=h2_d[t * 128:(t + 1) * 128, :], in_=h2b[:]), reads=[b_h2b]))
                for hf in range(2):
                    transposes([h2f[:, (hf * 4 + k) * 128:(hf * 4 + k + 1) * 128] for k in range(4)], [b_h2f], ident_f,
                               h2T[:, hf * 512:(hf + 1) * 512], b_h2T, dt_bf=False, pst=PS[3])
                plg, b_plg = PS[3]
                kb.group("pe", [lambda e, k=k: e.matmul(plg[:, 0:36], lhsT=h2T[:, k * 128:(k + 1) * 128], rhs=wr_t[:, k, :],
                                                        start=(k == 0), stop=(k == 7)) for k in range(8)],
                         reads=[b_h2T, b_wr], writes=[b_plg])
                kb.op("dve", lambda e: e.tensor_tensor(out=lg[:], in0=plg[:, 0:36], in1=br_t[:], op=ALU.add),
                      reads=[b_plg, b_br], writes=[b_lg])

                def dv(fn, reads=(), writes=()):
                    kb.op("dve", fn, reads=[b_rs, b_lg] + list(reads), writes=[b_rs] + list(writes))
                dv(lambda e: e.reduce_max(out=rs[:, 0:1], in_=lg[:, 0:4], axis=AX.X))
                dv(lambda e: e.tensor_scalar(out=rs[:, 1:2], in0=rs[:, 0:1], scalar1=-1.0, scalar2=None, op0=ALU.mult))
                kb.op("act", lambda e: e.activation(out=rs[:, 4:8], in_=lg[:, 0:4], func=AF.Exp, bias=rs[:, 1:2], scale=1.0),
                      reads=[b_rs, b_lg], writes=[b_rs])
                dv(lambda e: e.reduce_sum(out=rs[:, 2:3], in_=rs[:, 4:8], axis=AX.X))
                dv(lambda e: e.reciprocal(out=rs[:, 3:4], in_=rs[:, 2:3]))
                dv(lambda e: e.tensor_scalar(out=rs[:, 8:12], in0=lg[:, 0:4], scalar1=rs[:, 0:1], scalar2=None, op0=ALU.is_equal))
                dv(lambda e: e.tensor_tensor(out=rs[:, 12:44].rearrange("p (g x) -> p g x", g=4),
                                             in0=lg[:, 4:36].rearrange("p (g x) -> p g x", g=4),
                                             in1=rs[:, 8:12].unsqueeze(2).to_broadcast([128, 4, 8]), op=ALU.mult))
                dv(lambda e: e.tensor_reduce(out=rs[:, 44:52], in_=rs[:, 12:44].rearrange("p (g x) -> p x g", g=4), axis=AX.X, op=ALU.add))
                dv(lambda e: e.max(out=rs[:, 52:60], in_=rs[:, 44:52]))
                dv(lambda e: e.tensor_scalar(out=rs[:, 60:68], in0=rs[:, 44:52], scalar1=rs[:, 52:53], scalar2=None, op0=ALU.is_equal))
                dv(lambda e: e.tensor_scalar(out=rs[:, 68:76], in0=rs[:, 44:52], scalar1=rs[:, 53:54], scalar2=None, op0=ALU.is_equal))
                dv(lambda e: e.tensor_tensor(out=rs[:, 76:77], in0=rs[:, 53:54], in1=rs[:, 52:53], op=ALU.subtract))
                kb.op("act", lambda e: e.activation(out=rs[:, 77:78], in_=rs[:, 76:77], func=AF.Exp), reads=[b_rs], writes=[b_rs])
                dv(lambda e: e.tensor_scalar(out=rs[:, 78:79], in0=rs[:, 77:78], scalar1=1.0, scalar2=None, op0=ALU.add))
                dv(lambda e: e.reciprocal(out=rs[:, 79:80], in_=rs[:, 78:79]))
                dv(lambda e: e.tensor_tensor(out=RK[:, 2, t:t + 1], in0=rs[:, 3:4], in1=rs[:, 79:80], op=ALU.mult), writes=[b_RK])
                dv(lambda e: e.tensor_tensor(out=RK[:, 3, t:t + 1], in0=RK[:, 2, t:t + 1], in1=rs[:, 77:78], op=ALU.mult),
                   reads=[b_RK], writes=[b_RK])
                for k2 in range(2):
                    dv(lambda e, k2=k2: e.tensor_tensor(out=A1f[:, k2 * 32:(k2 + 1) * 32].rearrange("p (g x) -> p g x", g=4),
                                                      in0=rs[:, 8:12].unsqueeze(2).to_broadcast([128, 4, 8]),
                                                      in1=rs[:, 60 + 8 * k2:68 + 8 * k2].unsqueeze(1).to_broadcast([128, 4, 8]), op=ALU.mult),
                       reads=[b_A1f], writes=[b_A1f])
                kb.op("dve", lambda e: e.tensor_copy(out=A12all[:, t, :], in_=A1f[:]), reads=[b_A1f], writes=[b_A12all])
                pc, b_pc = PS[3]
                kb.group("pe", [lambda e: e.matmul(pc[:, 0:64], lhsT=tri_b, rhs=A12all[:, t, :], start=True, stop=True),
                                lambda e: e.matmul(pc[:, 64:128], lhsT=ones_b, rhs=A12all[:, t, :], start=True, stop=True)],
                         reads=[b_cst_b, b_A12all], writes=[b_pc])
                kb.op("dve", lambda e: e.tensor_tensor(out=cs[:, 0:32], in0=pc[:, 0:32], in1=Cc[:], op=ALU.add),
                      reads=[b_pc, b_Cc], writes=[b_cs])
                kb.op("dve", lambda e: e.tensor_tensor(out=cs[:, 64:96], in0=pc[:, 64:96], in1=Cc[:], op=ALU.add),
                      reads=[b_pc, b_Cc, b_cs], writes=[b_cs])
                kb.op("dve", lambda e: e.tensor_tensor(out=cs[:, 32:64], in0=pc[:, 32:64], in1=cs[:, 64:96], op=ALU.add),
                      reads=[b_pc, b_cs], writes=[b_cs])
                kb.op("dve", lambda e: e.tensor_tensor(out=cs[:, 0:64], in0=cs[:, 0:64], in1=A1f[:], op=ALU.mult),
                      reads=[b_cs, b_A1f], writes=[b_cs])
                kb.op("dve", lambda e: e.tensor_reduce(out=RK[:, 0:2, t:t + 1].rearrange("p k o -> p (k o)"),
                                                       in_=cs[:, 0:64].rearrange("p (k x) -> p k x", k=2), axis=AX.X, op=ALU.add),
                      reads=[b_cs, b_RK], writes=[b_RK])
                kb.op("dve", lambda e: e.tensor_tensor(out=Cc[:], in0=cs[:, 64:96], in1=pc[:, 96:128], op=ALU.add),
                      reads=[b_cs, b_pc, b_Cc], writes=[b_Cc])

            loadB(0)
            loadB(1)
            frontB(0)
            for t in range(NT):
                if t + 2 < NT:
                    loadB(t + 2)
                if t + 1 < NT:
                    frontB(t + 1)
                backB(t)
            kb.barrier_all()
            if stop == "B":
                return nc

        ys_toks = []
        with ExitStack() as ph:
            mk = mkp(ph)
            big, b_big = mk("big", [128, 3072], F32)
            s256 = cst_f[:, S128:S128 + 64]
            kb.op("dve", lambda e: e.tensor_tensor(out=big[:, 0:1056].rearrange("p (x j) -> p x j", j=33),
                                                   in0=Cc[:].unsqueeze(2).to_broadcast([128, 32, 33]),
                                                   in1=s256[:, 0:33].unsqueeze(1).to_broadcast([128, 32, 33]), op=ALU.is_gt),
                  reads=[b_Cc, b_cst_f], writes=[b_big])
            pe_, b_pe_ = mk("pend", [128, 4, 32], F32)
            kb.op("dve", lambda e: e.tensor_reduce(out=pe_[:, 0, :], in_=big[:, 0:1056].rearrange("p (x j) -> p x j", j=33), axis=AX.X, op=ALU.add),
                  reads=[b_big], writes=[b_pe_])
            kb.op("dve", lambda e: e.tensor_scalar(out=pe_[:, 0, :], in0=pe_[:, 0, :], scalar1=256.0, scalar2=None, op0=ALU.mult),
                  reads=[b_pe_], writes=[b_pe_])
            kb.op("dve", lambda e: e.tensor_copy(out=pe_[:, 1, :], in_=pe_[:, 0, :]), reads=[b_pe_], writes=[b_pe_])
            cur = 1
            for k in (1, 2, 4, 8, 16):
                nxt = 3 - cur
                kb.op("dve", lambda e, cur=cur, nxt=nxt, k=k: e.tensor_copy(out=pe_[:, nxt, 0:k], in_=pe_[:, cur, 0:k]),
                      reads=[b_pe_], writes=[b_pe_])
                kb.op("dve", lambda e, cur=cur, nxt=nxt, k=k: e.tensor_tensor(out=pe_[:, nxt, k:32], in0=pe_[:, cur, k:32],
                                                                           in1=pe_[:, cur, 0:32 - k], op=ALU.add),
                      reads=[b_pe_], writes=[b_pe_])
                cur = nxt
            pend = pe_[:, cur, :]
            kb.op("dve", lambda e: e.tensor_tensor(out=pe_[:, 3, :], in0=pend, in1=pe_[:, 0, :], op=ALU.subtract),
                  reads=[b_pe_], writes=[b_pe_])
            posf, b_posf = mk("posf", [128, 2, NT], F32)
            for k2 in range(2):
                kb.op("dve", lambda e, k2=k2: e.tensor_tensor(out=big[:, 0:1024].rearrange("p (t x) -> p t x", x=32),
                                                            in0=A12all[:, :, k2 * 32:(k2 + 1) * 32],
                                                            in1=pe_[:, 3, :].unsqueeze(1).to_broadcast([128, NT, 32]), op=ALU.mult),
                      reads=[b_A12all, b_pe_, b_big], writes=[b_big])
                kb.op("dve", lambda e, k2=k2: e.tensor_reduce(out=posf[:, k2, :], in_=big[:, 0:1024].rearrange("p (t x) -> p t x", x=32),
                                                            axis=AX.X, op=ALU.add), reads=[b_big, b_posf], writes=[b_posf])
                kb.op("dve", lambda e, k2=k2: e.tensor_tensor(out=posf[:, k2, :], in0=posf[:, k2, :], in1=RK[:, k2, :], op=ALU.add),
                      reads=[b_posf, b_RK], writes=[b_posf])
            kb.op("dve", lambda e: e.tensor_copy(out=posi[:], in_=posf[:]), reads=[b_posf], writes=[b_posi])
            kb.op("dve", lambda e: e.tensor_tensor(out=big[:, 0:2048].rearrange("p (s x) -> p s x", x=32),
                                                   in0=pend.unsqueeze(1).to_broadcast([128, NPAIR, 32]),
                                                   in1=s256.unsqueeze(2).to_broadcast([128, NPAIR, 32]), op=ALU.is_le),
                  reads=[b_pe_, b_cst_f, b_big], writes=[b_big])
            wif, b_wif = mk("wif", [128, NPAIR], F32)
            widx, b_widx = mk("widx", [128, 2, NPAIR], I32)
            wif2, b_wif2 = mk("wif2", [128, 2, NPAIR], F32)
            kb.op("dve", lambda e: e.tensor_reduce(out=wif[:], in_=big[:, 0:2048].rearrange("p (s x) -> p s x", x=32), axis=AX.X, op=ALU.add),
                  reads=[b_big], writes=[b_wif])
            kb.op("dve", lambda e: e.tensor_scalar(out=wif[:], in0=wif[:], scalar1=31.0, scalar2=128.0, op0=ALU.min, op1=ALU.mult),
                  reads=[b_wif], writes=[b_wif])
            kb.op("dve", lambda e: e.tensor_scalar(out=wif[:], in0=wif[:], scalar1=cst_f[:, PIDX:PIDX + 1], scalar2=None, op0=ALU.add),
                  reads=[b_wif, b_cst_f], writes=[b_wif])
            for hf in range(2):
                kb.op("dve", lambda e, hf=hf: e.tensor_scalar(out=wif2[:, hf, :], in0=wif[:], scalar1=2.0, scalar2=float(hf),
                                                            op0=ALU.mult, op1=ALU.add), reads=[b_wif, b_wif2], writes=[b_wif2])
            kb.op("dve", lambda e: e.tensor_copy(out=widx[:], in_=wif2[:]), reads=[b_wif2], writes=[b_widx])
            widx1, _b = mk("widx1", [128, NPAIR], I32)
            kb.op("dve", lambda e: e.tensor_copy(out=widx1[:], in_=wif[:]), reads=[b_wif, b_widx], writes=[b_widx])

            h2bs = [mk("h2c%d" % i, [128, 1024], BF16) for i in range(2)]
            sc_toks = []
            h2t_ = maxtoks(h2_toks)
            for t in range(NT):
                h2b, b_h2b = h2bs[t % 2]
                kb.dma("sp", lambda e: e.dma_start(out=h2b[:], in_=h2_d[t * 128:(t + 1) * 128, :]), writes=[b_h2b], extra=h2t_)
                for k2 in range(2):
                    sc_toks.append(kb.dma("pool", lambda e, k2=k2: e.indirect_dma_start(
                        out=xs_d, out_offset=bass.IndirectOffsetOnAxis(ap=posi[:, k2, t:t + 1], axis=0), in_=h2b[:], in_offset=None),
                        reads=[b_h2b, b_posi], owner=b_h2b, extra=[zf_tok]))

            wbufs = [(mk("w1b%d" % i, [128, 4096], BF16), mk("w3b%d" % i, [128, 4096], BF16), mk("w2b%d" % i, [128, 4096], BF16))
                     for i in range(2)]
            xbs = [mk("xb%d" % i, [128, 1024], BF16) for i in range(4)]
            xbT, b_xbT = mk("xbT", [128, 1024], BF16)
            hid, b_hid = mk("hid", [128, 512], BF16)
            hidT, b_hidT = mk("hidT", [128, 512], BF16)
            tht, b_tht = mk("tht", [128, 512], F32)
            ysbs = [mk("ysb%d" % i, [128, 1024], F32) for i in range(2)]
            sct = maxtoks(sc_toks)
            if stop == "C":
                kb.barrier_all()
                return nc
            convt = maxtoks(conv_toks)
            wq_v = [w.rearrange("(r h) c -> r (h c)", h=2) for w in wq_d]

            def loadW(pr):
                (w1b, b_w1b), (w3b, b_w3b), (w2b, b_w2b) = wbufs[pr % 2]
                for (wb_, bw_, wd_) in ((w1b, b_w1b, wq_v[0]), (w3b, b_w3b, wq_v[1]), (w2b, b_w2b, wq_v[2])):
                    kb.dma("pool", lambda e, wb_=wb_, wd_=wd_: e.indirect_dma_start(
                        out=wb_[:], out_offset=None, in_=wd_,
                        in_offset=bass.IndirectOffsetOnAxis(ap=widx1[:, pr:pr + 1], axis=0)),
                        reads=[b_widx], writes=[bw_], extra=convt)

            def loadX(s):
                xb, b_xb = xbs[s % 4]
                kb.dma("pool", lambda e: e.dma_start(out=xb[:], in_=xs_d[s * 128:(s + 1) * 128, :]), writes=[b_xb], extra=sct)

            def frontD_a(s):
                xb, b_xb = xbs[s % 4]
                pt, b_pt = PS[2]
                pv = bfv(pt)
                kb.group("pe", [lambda e, i=i: e.transpose(pv[:, i * 128:(i + 1) * 128], xb[:, i * 128:(i + 1) * 128], ident_b)
                                for i in range(8)], reads=[b_xb, b_cst_b], writes=[b_pt])

            def frontD_b(s):
                pr = s // 2
                (w1b, b_w1b), (w3b, b_w3b), (w2b, b_w2b) = wbufs[pr % 2]
                pt, b_pt = PS[2]
                pv = bfv(pt)
                kb.op("act", lambda e: e.copy(out=xbT[:], in_=pv[:, 0:1024]), reads=[b_pt], writes=[b_xbT])
                pab, b_pab = PS[s % 2]
                kb.group("pe", [lambda e, k=k, wq=wq, o=o: e.matmul(pab[:, o * 512:(o + 1) * 512], lhsT=xbT[:, k * 128:(k + 1) * 128],
                                                                    rhs=wq[:, k * 512:(k + 1) * 512], start=(k == 0), stop=(k == 7))
                                for o, wq in ((0, w1b), (1, w3b)) for k in range(8)],
                         reads=[b_xbT, b_w1b, b_w3b], writes=[b_pab])

            def backD_a(s):
                pab, b_pab = PS[s % 2]
                kb.op("act", lambda e: e.activation(out=tht[:], in_=pab[:, 0:512], func=AF.Tanh, scale=0.5), reads=[b_pab], writes=[b_tht])
                kb.op("dve", lambda e: e.scalar_tensor_tensor(out=tht[:], in0=tht[:], scalar=1.0, in1=pab[:, 0:512], op0=ALU.add, op1=ALU.mult),
                      reads=[b_tht, b_pab], writes=[b_tht])
                kb.op("dve", lambda e: e.tensor_tensor(out=hid[:].rearrange("t (j p) -> t p j", j=4),
                                                       in0=tht[:].rearrange("t (p j) -> t p j", j=4),
                                                       in1=pab[:, 512:1024].rearrange("t (p j) -> t p j", j=4), op=ALU.mult),
                      reads=[b_tht, b_pab], writes=[b_hid])

            def backD_b(s):
                pr = s // 2
                (w1b, b_w1b), (w3b, b_w3b), (w2b, b_w2b) = wbufs[pr % 2]
                transposes([hid[:, k * 128:(k + 1) * 128] for k in range(4)], [b_hid], ident_b, hidT[:], b_hidT, pst=PS[3])
                py, b_py = PS[3]
                kb.group("pe", [lambda e, k=k, hf=hf: e.matmul(py[:, hf * 512:(hf + 1) * 512], lhsT=hidT[:, k * 128:(k + 1) * 128],
                                                               rhs=w2b[:, k * 1024 + hf * 512:k * 1024 + (hf + 1) * 512],
                                                               start=(k == 0), stop=(k == 3)) for hf in range(2) for k in range(4)],
                         reads=[b_hidT, b_w2b], writes=[b_py])
                ysb, b_ysb = ysbs[s % 2]
                kb.op("act", lambda e: e.activation(out=ysb[:], in_=py[:], func=AF.Copy, scale=0.5), reads=[b_py], writes=[b_ysb])
                ys_toks.append(kb.dma("sp", lambda e: e.dma_start(out=ys_d[s * 128:(s + 1) * 128, :], in_=ysb[:]), reads=[b_ysb]))

            loadW(0)
            for s0 in range(3):
                loadX(s0)
            frontD_a(0)
            frontD_b(0)
            for s in range(NSLOT):
                if s + 3 < NSLOT:
                    loadX(s + 3)
                if s + 1 < NSLOT:
                    if (s + 1) % 2 == 0:
                        loadW((s + 1) // 2)
                    frontD_a(s + 1)
                backD_a(s)
                if s + 1 < NSLOT:
                    frontD_b(s + 1)
                backD_b(s)
            kb.barrier_all()
            if stop == "D":
                return nc

        with ExitStack() as ph:
            mk = mkp(ph)
            g2t, b_g2t = mk("g2t", [128, 1024], F32)
            kb.dma("sp", lambda e: e.dma_start(out=g2t[:], in_=mod_d[:, 5120:6144]), writes=[b_g2t])
            lnE, b_lnE = mk("lnE", [128, 2, 1024], F32)
            kb.dma("sp", lambda e: e.dma_start(out=lnE[:].rearrange("p a n -> p (a n)"), in_=lnp_d[:, 2048:4096]), writes=[b_lnE])
            xts = [mk("xtE%d" % i, [128, 1024], F32) for i in range(2)]
            yg = [[mk("yg%d_%d" % (k2, i), [128, 1024], F32) for i in range(2)] for k2 in range(2)]
            z1, b_z1 = mk("z1E", [128, 1024], F32)
            ots = [mk("ot%d" % i, [128, 1024], F32) for i in range(2)]
            yst = maxtoks(ys_toks)
            x1t = maxtoks(x1_toks)
            out_toks = []
            for t in range(NT):
                xt, b_xt = xts[t % 2]
                kb.dma("pool", lambda e: e.dma_start(out=xt[:], in_=x1_d[t * 128:(t + 1) * 128, :]), writes=[b_xt], extra=x1t)
                yk = []
                for k2 in range(2):
                    ykt, b_yk = yg[k2][t % 2]
                    yk.append((ykt, b_yk))
                    kb.dma("pool", lambda e, k2=k2, ykt=ykt: e.indirect_dma_start(
                        out=ykt[:], out_offset=None, in_=ys_d, in_offset=bass.IndirectOffsetOnAxis(ap=posi[:, k2, t:t + 1], axis=0)),
                        reads=[b_posi], writes=[b_yk], extra=yst)
                kb.op("dve", lambda e: e.tensor_scalar(out=z1[:], in0=yk[0][0][:], scalar1=RK[:, 2, t:t + 1], scalar2=None, op0=ALU.mult),
                      reads=[yk[0][1], b_RK], writes=[b_z1])
                kb.op("dve", lambda e: e.scalar_tensor_tensor(out=z1[:], in0=yk[1][0][:], scalar=RK[:, 3, t:t + 1], in1=z1[:],
                                                              op0=ALU.mult, op1=ALU.add), reads=[yk[1][1], b_RK, b_z1], writes=[b_z1])
                kb.op("dve", lambda e: e.tensor_tensor(out=z1[:], in0=z1[:], in1=g2t[:], op=ALU.mult),
                      reads=[b_z1, b_g2t], writes=[b_z1])
                kb.op("dve", lambda e: e.scalar_tensor_tensor(out=z1[:], in0=xt[:], scalar=ALPHA, in1=z1[:], op0=ALU.mult, op1=ALU.add),
                      reads=[b_xt, b_z1], writes=[b_z1])
                ot, b_ot = ots[t % 2]
                layernorm(z1, b_z1, 1024, lnE[:, 0, :], lnE[:, 1, :], [b_lnE], ot[:], b_ot)
                out_toks.append(kb.dma("sp", lambda e: e.dma_start(out=out_d[t * 128:(t + 1) * 128, :], in_=ot[:]), reads=[b_ot]))
            kb.wait_tok("sp", maxtoks(out_toks))
    return nc


def _rope_table():
    n = 8192
    pos = np.arange(n)
    row = pos // 64
    col = pos % 64
    inv = (1.0 / (10000.0 ** (np.arange(16, dtype=np.float32) / 16.0))).astype(np.float32)
    tabs = []
    for p in (row, col):
        ang = p.astype(np.float32)[:, None] * inv[None, :]
        tabs.append((np.cos(ang).astype(np.float32), np.sin(ang).astype(np.float32)))
    cosr, sinr = tabs[0]
    cosc, sinc = tabs[1]
    cos64 = np.concatenate([cosr, cosr, cosc, cosc], 1)
    sin64 = np.concatenate([-sinr, sinr, -sinc, sinc], 1)
    return np.concatenate([cos64, sin64], 1).astype(np.float32)


def _prep_inputs(inputs):
    f = lambda a: np.ascontiguousarray(np.asarray(a, dtype=np.float32))
    x = f(inputs["x"]); c = f(inputs["c"]); ctx = f(inputs["ctx"]); c_ctx = f(inputs["c_ctx"])
    rope = _rope_table()
    kk = np.arange(128)[:, None]
    qq = np.arange(128)[None, :]
    mP = (kk >= qq).astype(np.float32)
    mN = (kk <= qq).astype(np.float32)
    ident = np.eye(128, dtype=np.float32)
    tri = (kk < qq).astype(np.float32)
    ones = np.ones((128, 128), np.float32)
    thr = np.tile((np.arange(65, dtype=np.float32) * 128.0)[None, :], (32, 1)).reshape(1, 2080)
    s128 = (np.arange(64, dtype=np.float32) * 256.0)[None, :]
    consts = np.concatenate([ident, tri, ones, np.zeros((128, 128), np.float32),
                             np.tile(s128, (128, 1)),
                             np.arange(128, dtype=np.float32)[:, None]], 1)
    consts = np.ascontiguousarray(consts, dtype=np.float32)
    shared = {
        "w_ada": f(inputs["w_ada"][0]),
        "b_ada2": np.ascontiguousarray(np.tile(f(inputs["b_ada"][0])[None, :], (2, 1))),
        "w_in": f(inputs["w_in"][0]),
        "sinkb": np.ascontiguousarray(np.tile(np.repeat(f(inputs["attn_sink"][0]), 128)[None, :], (64, 1))),
        "gmln": np.ascontiguousarray(np.tile(np.concatenate([f(inputs["gm_ln_g"][0]), f(inputs["gm_ln_b"][0])])[None, :], (128, 1))),
        "wsT": np.ascontiguousarray(f(inputs["gm_ws"][0]).transpose(2, 0, 1).reshape(128, 1024)),
        "bsT": np.ascontiguousarray(f(inputs["gm_bs"][0]).T),
        "w_pa": f(inputs["w_pa"][0]), "w_pb": f(inputs["w_pb"][0]), "w_o": f(inputs["w_o"][0]),
        "lnp": np.ascontiguousarray(np.tile(np.concatenate([f(inputs["ln1_g"][0]), f(inputs["ln1_b"][0]),
                                                            f(inputs["ln2_g"][0]), f(inputs["ln2_b"][0])])[None, :], (128, 1))),
        "wr": np.ascontiguousarray(np.concatenate([f(inputs["router_g_w"][0]),
                                                   f(inputs["router_e_w"][0]).transpose(1, 0, 2).reshape(1024, 32)], 1)),
        "br": np.ascontiguousarray(np.tile(np.concatenate([f(inputs["router_g_b"][0]),
                                                           f(inputs["router_e_b"][0]).reshape(32)])[None, :], (128, 1))),
        "w1": f(inputs["moe_w1"][0]).reshape(8192, 2048),
        "w3": f(inputs["moe_w3"][0]).reshape(8192, 2048),
        "w2": f(inputs["moe_w2"][0]).reshape(8192, 2048),
        "consts": consts,
    }
    in_maps = []
    for k in range(8):
        b, half = k // 2, k % 2
        lo = half * 4096
        xh = np.zeros((256, 1024), np.float32)
        rp = np.zeros((34 * 128, 128), np.float32)
        rp[128:128 + 4096] = rope[lo:lo + 4096]
        if half == 1:
            xh[0:128] = x[b, lo - 128:lo]
            rp[0:128] = rope[lo - 128:lo]
        if half == 0:
            xh[128:256] = x[b, lo + 4096:lo + 4096 + 128]
            rp[33 * 128:34 * 128] = rope[lo + 4096:lo + 4096 + 128]
        zero = np.zeros((128, 128), np.float32)
        masks = np.concatenate([mP, mN, mP if half == 1 else zero, mN if half == 0 else zero], 1)
        cT = np.stack([c[b].reshape(8, 128).T, c_ctx.reshape(8, 128).T], 2).reshape(128, 16)
        m = dict(shared)
        m.update({
            "x": np.ascontiguousarray(x[b, lo:lo + 4096]),
            "xh": xh,
            "ctx": np.ascontiguousarray(ctx[b]),
            "cT": np.ascontiguousarray(cT, dtype=np.float32),
            "rope": rp,
            "masks": np.ascontiguousarray(masks, dtype=np.float32),
        })
        in_maps.append(m)
    return in_maps


def kernel(**inputs):
    in_maps = _prep_inputs(inputs)
    nc = build_nc()
    res = run_bass_kernel_spmd(nc, in_maps, core_ids=list(range(8)))
    out = np.zeros((4, 8192, 1024), np.float32)
    for k in range(8):
        b, half = k // 2, k % 2
        out[b, half * 4096:(half + 1) * 4096] = np.asarray(res.results[k]["out"], dtype=np.float32)
    return out
```

```python
import numpy as np
from contextlib import ExitStack
import concourse.bass as bass
import concourse.mybir as mybir
from concourse.bass_utils import run_bass_kernel_spmd

F32 = mybir.dt.float32
BF16 = mybir.dt.bfloat16
I32 = mybir.dt.int32
AF = mybir.ActivationFunctionType
ALU = mybir.AluOpType
AX = mybir.AxisListType

import os
KDBG = os.environ.get('KDBG', '')
NT = 32
NSLOT = 128
NPAIR = 64
ALPHA = 2.0 ** 0.25
LN_EPS = 1e-6
GC = 0.7978845608028654


class Buf:
    def __init__(self, name):
        self.name = name
        self.w = None
        self.r = {}
        self.dsem = None
        self.dcount = 0


class EngState:
    def __init__(self, name, eng, sem, selfwait):
        self.name = name
        self.eng = eng
        self.sem = sem
        self.count = 0
        self.seen = {}
        self.selfwait = selfwait


class KB:
    def __init__(self, nc, stack):
        self.nc = nc
        self.stack = stack
        self.E = {}
        for name, eng, sw in (("pe", nc.tensor, False), ("act", nc.scalar, True),
                              ("dve", nc.vector, True), ("pool", nc.gpsimd, True),
                              ("sp", nc.sync, True)):
            sem = stack.enter_context(nc.semaphore("s_" + name))
            self.E[name] = EngState(name, eng, sem, sw)
        self.dma_toks = []

    def new_sem(self, name):
        return self.stack.enter_context(self.nc.semaphore(name))

    def sb(self, name, shape, dt):
        return self.stack.enter_context(self.nc.sbuf_tensor(name, shape, dt))

    def ps(self, name, shape, dt):
        return self.stack.enter_context(self.nc.psum_tensor(name, shape, dt))

    def _wait(self, E, toks):
        need = {}
        for t in toks:
            if t is None:
                continue
            sem, val = t
            if sem is E.sem and not E.selfwait:
                continue
            k = id(sem)
            if val > E.seen.get(k, 0):
                if k not in need or need[k][1] < val:
                    need[k] = (sem, val)
        for k, (sem, val) in need.items():
            E.eng.wait_ge(sem, val)
            E.seen[k] = val

    def _deps(self, reads, writes):
        deps = []
        for b in reads:
            deps.append(b.w)
        for b in writes:
            deps.extend(b.r.values())
            deps.append(b.w)
        return deps

    def _record(self, tok, reads, writes):
        for b in reads:
            k = id(tok[0])
            if k not in b.r or b.r[k][1] < tok[1]:
                b.r[k] = tok
        for b in writes:
            b.w = tok
            b.r = {}

    def op(self, engname, fn, reads=(), writes=(), extra=()):
        E = self.E[engname]
        self._wait(E, self._deps(reads, writes) + list(extra))
        ins = fn(E.eng)
        E.count += 1
        ins.then_inc(E.sem, 1)
        tok = (E.sem, E.count)
        self._record(tok, reads, writes)
        return tok

    def group(self, engname, fns, reads=(), writes=(), extra=()):
        E = self.E[engname]
        self._wait(E, self._deps(reads, writes) + list(extra))
        ins = None
        for fn in fns:
            ins = fn(E.eng)
        E.count += 1
        ins.then_inc(E.sem, 1)
        tok = (E.sem, E.count)
        self._record(tok, reads, writes)
        return tok

    def dma(self, engname, fn, reads=(), writes=(), owner=None, extra=()):
        E = self.E[engname]
        self._wait(E, self._deps(reads, writes) + list(extra))
        if owner is None:
            owner = (list(writes) + list(reads))[0]
        if owner.dsem is None:
            owner.dsem = self.new_sem("d_" + owner.name)
        ins = fn(E.eng)
        owner.dcount += 16
        ins.then_inc(owner.dsem, 16)
        tok = (owner.dsem, owner.dcount)
        self.dma_toks.append(tok)
        self._record(tok, reads, writes)
        return tok

    def wait_tok(self, engname, toks):
        self._wait(self.E[engname], toks)

    def barrier_all(self):
        toks = [(e.sem, e.count) for e in self.E.values() if e.count > 0] + maxtoks(self.dma_toks)
        for e in self.E.values():
            self._wait(e, [t for t in toks if t[0] is not e.sem])
        self.dma_toks = maxtoks(self.dma_toks)


def maxtoks(toks):
    best = {}
    for t in toks:
        if t is None:
            continue
        k = id(t[0])
        if k not in best or best[k][1] < t[1]:
            best[k] = t
    return list(best.values())


def build_nc(stop=None, nta=NT, dbg=False):
    nc = bass.Bass("TRN2", target_bir_lowering=False)

    def din(name, shape, dt=F32):
        return nc.dram_tensor(name, shape, dt, kind="ExternalInput").ap()

    x_d = din("x", [4096, 1024])
    xh_d = din("xh", [256, 1024])
    ctx_d = din("ctx", [256, 1024])
    cT_d = din("cT", [128, 16])
    wada_d = din("w_ada", [1024, 6144])
    bada_d = din("b_ada2", [2, 6144])
    win_d = din("w_in", [1024, 3840])
    sink_d = din("sinkb", [64, 1024])
    gmln_d = din("gmln", [128, 1024])
    wsT_d = din("wsT", [128, 1024])
    bsT_d = din("bsT", [128, 8])
    wpa_d = din("w_pa", [512, 1024])
    wpb_d = din("w_pb", [512, 1024])
    wo_d = din("w_o", [1024, 1024])
    lnp_d = din("lnp", [128, 4096])
    wr_d = din("wr", [1024, 36])
    br_d = din("br", [128, 36])
    w1_d = din("w1", [8192, 2048])
    w3_d = din("w3", [8192, 2048])
    w2_d = din("w2", [8192, 2048])
    rope_d = din("rope", [34 * 128, 128])
    masks_d = din("masks", [128, 512])
    NCST = 512 + 64 + 1
    consts_d = din("consts", [128, NCST])
    out_d = nc.dram_tensor("out", [4096, 1024], F32, kind="ExternalOutput").ap()

    dk = dict(kind="ExternalOutput") if dbg else {}
    x1_d = nc.dram_tensor("x1_scr", [4096, 1024], F32, **dk).ap()
    h2_d = nc.dram_tensor("h2_scr", [4096, 1024], BF16, **dk).ap()
    y_d = nc.dram_tensor("y_scr", [4096, 1024], BF16, **dk).ap()
    dbgA_d = nc.dram_tensor("dbgA", [4096, 1024], F32, **dk).ap() if dbg else None
    mod_d = nc.dram_tensor("mod_scr", [128, 8 * 1024], F32).ap()
    wq_d = [nc.dram_tensor("wq%d_scr" % i, [8192, 2048], BF16).ap() for i in range(3)]
    xs_d = nc.dram_tensor("xs_scr", [NSLOT * 128, 1024], BF16).ap()
    ys_d = nc.dram_tensor("ys_scr", [NSLOT * 128, 1024], F32).ap()

    with ExitStack() as st:
        kb = KB(nc, st)

        def mkp(ph):
            def mk(name, shape, dt):
                return ph.enter_context(nc.sbuf_tensor(name, shape, dt)), Buf(name)
            return mk

        mkg = mkp(st)

        PS = []
        for i in range(4):
            t = kb.ps("ps%d" % i, [128, 1024], F32)
            PS.append((t, Buf("ps%d" % i)))
        psi = [0]

        def nextps():
            r = PS[psi[0] % 4]
            psi[0] += 1
            return r

        def bfv(t):
            return t[:].bitcast(BF16)

        cst_f, b_cst_f = mkg("cst_f", [128, NCST], F32)
        kb.dma("sp", lambda e: e.dma_start(out=cst_f[:], in_=consts_d), writes=[b_cst_f])
        ident_f = cst_f[:, 0:128]
        S128 = 512
        PIDX = 512 + 64
        cst_b, b_cst_b = mkg("cst_b", [128, 384], BF16)
        kb.op("dve", lambda e: e.tensor_copy(out=cst_b[:], in_=cst_f[:, 0:384]), reads=[b_cst_f], writes=[b_cst_b])
        ident_b = cst_b[:, 0:128]
        tri_b = cst_b[:, 128:256]
        ones_b = cst_b[:, 256:384]

        st12, b_st12 = mkg("st12", [128, 12], F32)
        mv, b_mv = mkg("mv", [128, 2], F32)
        nw, b_nw = mkg("nw", [128, 4], F32)
        lnt, b_lnt = mkg("lnt", [128, 1024], F32)
        Cc, b_Cc = mkg("Cc", [128, 32], F32)
        A12all, b_A12all = mkg("A12all", [128, NT, 64], BF16)
        RK, b_RK = mkg("RK", [128, 4, NT], F32)
        posi, b_posi = mkg("posi", [128, 2, NT], I32)

        def rstd_newton(eps):
            kb.op("dve", lambda e: e.tensor_scalar(out=nw[:, 0:1], in0=mv[:, 1:2], scalar1=eps, scalar2=None, op0=ALU.add),
                  reads=[b_mv], writes=[b_nw])
            kb.op("dve", lambda e: e.tensor_scalar(out=nw[:, 2:3], in0=nw[:, 0:1], scalar1=0.5, scalar2=0.5, op0=ALU.mult, op1=ALU.add),
                  reads=[b_nw], writes=[b_nw])
            kb.op("dve", lambda e: e.reciprocal(out=nw[:, 1:2], in_=nw[:, 2:3]), reads=[b_nw], writes=[b_nw])
            for _ in range(4):
                kb.op("dve", lambda e: e.tensor_tensor(out=nw[:, 2:3], in0=nw[:, 1:2], in1=nw[:, 1:2], op=ALU.mult),
                      reads=[b_nw], writes=[b_nw])
                kb.op("dve", lambda e: e.scalar_tensor_tensor(out=nw[:, 2:3], in0=nw[:, 2:3], scalar=-0.5, in1=nw[:, 0:1],
                                                              op0=ALU.mult, op1=ALU.mult), reads=[b_nw], writes=[b_nw])
                kb.op("dve", lambda e: e.scalar_tensor_tensor(out=nw[:, 1:2], in0=nw[:, 2:3], scalar=1.5, in1=nw[:, 1:2],
                                                              op0=ALU.add, op1=ALU.mult), reads=[b_nw], writes=[b_nw])

        def layernorm(src, b_src, n, A, B, rA, dst, b_dst, eps=LN_EPS):
            for i in range(n // 512):
                kb.op("dve", lambda e, i=i: e.bn_stats(out=st12[:, 6 * i:6 * i + 6], in_=src[:, i * 512:(i + 1) * 512]),
                      reads=[b_src], writes=[b_st12])
            kb.op("dve", lambda e: e.bn_aggr(out=mv[:], in_=st12[:, 0:6 * (n // 512)]), reads=[b_st12], writes=[b_mv])
            rstd_newton(eps)
            kb.op("dve", lambda e: e.scalar_tensor_tensor(out=lnt[:, 0:n], in0=src[:, 0:n], scalar=mv[:, 0:1], in1=A,
                                                          op0=ALU.subtract, op1=ALU.mult),
                  reads=[b_src, b_mv] + rA, writes=[b_lnt])
            kb.op("dve", lambda e: e.scalar_tensor_tensor(out=dst, in0=lnt[:, 0:n], scalar=nw[:, 1:2], in1=B,
                                                          op0=ALU.mult, op1=ALU.add),
                  reads=[b_lnt, b_nw] + rA, writes=[b_dst])

        def transposes(src_aps, rsrc, identity, dst, b_dst, dt_bf=True, pst=None):
            n = len(src_aps)
            pt, b_pt = nextps() if pst is None else pst
            pv = bfv(pt) if dt_bf else pt
            kb.group("pe", [lambda e, i=i, a=a: e.transpose(pv[:, i * 128:(i + 1) * 128], a, identity)
                            for i, a in enumerate(src_aps)], reads=rsrc + [b_cst_b, b_cst_f], writes=[b_pt])
            kb.op("act", lambda e: e.copy(out=dst, in_=pv[:, 0:n * 128]), reads=[b_pt], writes=[b_dst])

        mod_toks = []
        with ExitStack() as ph:
            mk = mkp(ph)
            cT_f, b_cT = mk("cT_f", [128, 16], F32)
            kb.dma("sp", lambda e: e.dma_start(out=cT_f[:], in_=cT_d), writes=[b_cT])
            cth, b_cth = mk("cth", [128, 16], F32)
            kb.op("act", lambda e: e.activation(out=cth[:], in_=cT_f[:], func=AF.Tanh, scale=0.5), reads=[b_cT], writes=[b_cth])
            kb.op("dve", lambda e: e.scalar_tensor_tensor(out=cth[:], in0=cth[:], scalar=1.0, in1=cT_f[:], op0=ALU.add, op1=ALU.mult),
                  reads=[b_cth, b_cT], writes=[b_cth])
            sT_b, b_sT = mk("sT_b", [128, 16], F32)
            kb.op("dve", lambda e: e.tensor_scalar(out=sT_b[:], in0=cth[:], scalar1=0.5, scalar2=None, op0=ALU.mult),
                  reads=[b_cth], writes=[b_sT])
            sel, b_sel = mk("sel", [2, 256], F32)
            kb.op("pool", lambda e: e.memset(sel[:], 0.0), writes=[b_sel])
            kb.op("pool", lambda e: e.memset(sel[0:1, 0:128], 1.0), reads=[b_sel], writes=[b_sel])
            kb.dma("sp", lambda e: e.dma_start(out=sel[1:2, 128:256], in_=consts_d[0:1, 256:384]), reads=[b_sel], writes=[b_sel])
            wada_v = wada_d.rearrange("(k p) n -> p k n", p=128)
            wab = [mk("wab%d" % i, [128, 8, 512], F32) for i in range(2)]
            bad = [mk("bad%d" % i, [2, 512], F32) for i in range(2)]
            mrow = [mk("modrow%d" % i, [2, 512], F32) for i in range(2)]
            mts = [mk("mt%d" % i, [128, 1024], F32) for i in range(2)]
            for ch in range(12):
                wa, b_wa = wab[ch % 2]
                bd, b_bd = bad[ch % 2]
                modrow, b_modrow = mrow[ch % 2]
                mt, b_mt = mts[ch % 2]
                kb.dma("sp", lambda e, ch=ch, wa=wa: e.dma_start(out=wa[:], in_=wada_v[:, :, ch * 512:(ch + 1) * 512]), writes=[b_wa])
                kb.dma("sp", lambda e, ch=ch, bd=bd: e.dma_start(out=bd[:], in_=bada_d[:, ch * 512:(ch + 1) * 512]), writes=[b_bd])
                pt, b_pt = nextps()
                kb.group("pe", [lambda e, k=k, wa=wa, pt=pt: e.matmul(pt[0:2, 0:512], lhsT=sT_b[:, 2 * k:2 * k + 2], rhs=wa[:, k, :],
                                                                       start=(k == 0), stop=(k == 7)) for k in range(8)],
                         reads=[b_sT, b_wa], writes=[b_pt])
                vec = ch // 2
                addc = 1.0 if vec in (1, 4) else 0.0
                kb.op("dve", lambda e, pt=pt, bd=bd, modrow=modrow, addc=addc: e.scalar_tensor_tensor(
                    out=modrow[:], in0=pt[0:2, 0:512], scalar=addc, in1=bd[:], op0=ALU.add, op1=ALU.add),
                    reads=[b_pt, b_bd], writes=[b_modrow])
                pt2, b_pt2 = nextps()
                fns = [lambda e, pt2=pt2, modrow=modrow: e.matmul(pt2[:, 0:512], lhsT=sel[:, 0:128], rhs=modrow[:], start=True, stop=True)]
                if vec < 2:
                    fns.append(lambda e, pt2=pt2, modrow=modrow: e.matmul(pt2[:, 512:1024], lhsT=sel[:, 128:256], rhs=modrow[:],
                                                                          start=True, stop=True))
                kb.group("pe", fns, reads=[b_sel, b_modrow], writes=[b_pt2])
                sc = 0.5 if vec == 2 else 1.0
                kb.op("act", lambda e, pt2=pt2, mt=mt, sc=sc: e.activation(out=mt[:], in_=pt2[:], func=AF.Copy, scale=sc),
                      reads=[b_pt2], writes=[b_mt])
                col = vec * 1024 + (ch % 2) * 512
                mod_toks.append(kb.dma("sp", lambda e, mt=mt, col=col: e.dma_start(out=mod_d[:, col:col + 512], in_=mt[:, 0:512]), reads=[b_mt]))
                if vec < 2:
                    col2 = (6 + vec) * 1024 + (ch % 2) * 512
                    mod_toks.append(kb.dma("sp", lambda e, mt=mt, col2=col2: e.dma_start(out=mod_d[:, col2:col2 + 512], in_=mt[:, 512:1024]),
                                           reads=[b_mt]))
            kb.barrier_all()
        modt = maxtoks(mod_toks)
        if stop == "S":
            return nc

        y_toks = []
        conv_toks = []
        with ExitStack() as ph:
            mk = mkp(ph)
            msk_b, b_msk = mk("msk_b", [128, 512], BF16)
            kb.dma("pool", lambda e: e.dma_start(out=msk_b[:], in_=masks_d), writes=[b_msk])
            gmln_t, b_gmln = mk("gmln_t", [128, 1024], F32)
            kb.dma("sp", lambda e: e.dma_start(out=gmln_t[:], in_=gmln_d), writes=[b_gmln])
            bsT_t, b_bsT = mk("bsT_t", [128, 8], F32)
            kb.dma("sp", lambda e: e.dma_start(out=bsT_t[:], in_=bsT_d), writes=[b_bsT])
            esink, b_esink = mk("esink", [64, 1024], F32)
            kb.dma("sp", lambda e: e.dma_start(out=esink[:], in_=sink_d), writes=[b_esink])
            kb.op("act", lambda e: e.activation(out=esink[:], in_=esink[:], func=AF.Exp), reads=[b_esink], writes=[b_esink])
            wsT_b, b_wsT = mk("wsT_b", [128, 1024], BF16)
            kb.dma("pool", lambda e: e.dma_start(out=wsT_b[:], in_=wsT_d), writes=[b_wsT])
            win_b, b_win = mk("win_b", [128, 8, 3840], BF16)
            win_v = win_d.rearrange("(k p) n -> p k n", p=128)
            for k in range(8):
                for hf in range(2):
                    kb.dma("pool", lambda e, k=k, hf=hf: e.dma_start(out=win_b[:, k, hf * 1920:(hf + 1) * 1920],
                                                                      in_=win_v[:, k, hf * 1920:(hf + 1) * 1920]), writes=[b_win])
            wpa_b, b_wpa = mk("wpa_b", [64, 8, 1024], BF16)
            kb.dma("pool", lambda e: e.dma_start(out=wpa_b[:], in_=wpa_d.rearrange("(h p) n -> p h n", p=64)), writes=[b_wpa])
            wpb_b, b_wpb = mk("wpb_b", [128, 4, 1024], BF16)
            kb.dma("pool", lambda e: e.dma_start(out=wpb_b[:], in_=wpb_d.rearrange("(k p) n -> p k n", p=128)), writes=[b_wpb])
            modA, b_modA = mk("modA", [128, 2, 1024], F32)
            kb.dma("sp", lambda e: e.dma_start(out=modA[:].rearrange("p a n -> p (a n)"), in_=mod_d[:, 0:2048]), writes=[b_modA], extra=modt)
            KT, b_KT = mk("KT", [128, 34 * 128], BF16)
            VV, b_VV = mk("VV", [128, 34, 128], BF16)
            KTc, b_KTc = mk("KTc", [128, 256], BF16)
            VVc, b_VVc = mk("VVc", [128, 2, 128], BF16)
            xts = [mk("xt%d" % i, [128, 1024], F32) for i in range(2)]
            hb, b_hb = mk("hb", [128, 1024], BF16)
            hTs = [mk("hT%d" % i, [128, 1024], BF16) for i in range(2)]
            ropes = [mk("rope%d" % i, [128, 128], F32) for i in range(2)]
            rt1, b_rt1 = mk("rt1", [128, 512], F32)
            rt2, b_rt2 = mk("rt2", [128, 512], F32)
            rq, b_rq = mk("rq", [128, 512], BF16)
            rk, b_rk = mk("rk", [128, 128], BF16)
            QT, b_QT = mk("QT", [128, 512], BF16)
            gg, b_gg = mk("gg", [128, 1024], F32)
            gsq, b_gsq = mk("gsq", [128, 1024], F32)
            tgs = [mk("tg%d" % i, [128, 1024], F32) for i in range(2)]
            ETs = [mk("ET%d" % i, [128, 512], BF16) for i in range(4)]
            eti = [0]
            dens, b_dens = rt2[0:64, :], b_rt2
            oT, b_oT = mk("oT", [64, 8, 128], BF16)
            vgm, b_vgm = mk("vgm", [128, 512], BF16)
            spb, b_spb = rt1, b_rt1
            ygm, b_ygm = mk("ygm", [128, 512], BF16)
            ygT, b_ygT = mk("ygT", [128, 512], BF16)
            yp1, b_yp1 = tgs[0]
            y2bs = [mk("y2b%d" % i, [128, 1024], BF16) for i in range(2)]

            rsrc, b_rsrc = mk("rsrc", [128, 512], F32)

            def rope_apply(src_ps, b_srcs_ps, nh, tab, b_tab, dst, b_dst, view=None):
                n = nh * 64
                kb.op("act", lambda e: e.copy(out=rsrc[:, 0:n], in_=src_ps), reads=b_srcs_ps, writes=[b_rsrc])
                src = rsrc[:, 0:n]
                b_srcs = [b_rsrc]
                cosb = tab[:, 0:64].unsqueeze(1).to_broadcast([128, nh, 64])
                kb.op("dve", lambda e: e.tensor_tensor(out=rt1[:, 0:n].rearrange("p (h d) -> p h d", h=nh),
                                                       in0=src.rearrange("p (h d) -> p h d", h=nh), in1=cosb, op=ALU.mult),
                      reads=b_srcs + [b_tab], writes=[b_rt1])
                sv = src.rearrange("p (g a d) -> p g a d", a=2, d=16)
                tv = rt2[:, 0:n].rearrange("p (g a d) -> p g a d", a=2, d=16)
                sn = tab[:, 64:128].rearrange("p (x a d) -> p x a d", a=2, d=16)
                for a in range(2):
                    o = tv[:, :, a, :].rearrange("p (h x) d -> p h x d", x=2)
                    i0 = sv[:, :, 1 - a, :].rearrange("p (h x) d -> p h x d", x=2)
                    i1 = sn[:, :, a, :].unsqueeze(1).to_broadcast([128, nh, 2, 16])
                    kb.op("dve", lambda e, o=o, i0=i0, i1=i1: e.tensor_tensor(out=o, in0=i0, in1=i1, op=ALU.mult),
                          reads=b_srcs + [b_tab], writes=[b_rt2])
                a1, a2 = rt1[:, 0:n], rt2[:, 0:n]
                if view is not None:
                    a1, a2 = view(a1), view(a2)
                kb.op("dve", lambda e: e.tensor_tensor(out=dst, in0=a1, in1=a2, op=ALU.add),
                      reads=[b_rt1, b_rt2], writes=[b_dst])

            def ln_mod_T(xt, b_xt, A, B, rA, i2):
                hT, b_hT = hTs[i2]
                layernorm(xt, b_xt, 1024, A, B, rA, hb[:], b_hb)
                transposes([hb[:, k * 128:(k + 1) * 128] for k in range(8)], [b_hb], ident_b, hT[:], b_hT)
                return hT, b_hT

            def proj_kv(hT, b_hT):
                pt, b_pt = nextps()
                kb.group("pe", [lambda e, k=k: e.matmul(pt[:, 0:256], lhsT=hT[:, k * 128:(k + 1) * 128], rhs=win_b[:, k, 512:768],
                                                        start=(k == 0), stop=(k == 7)) for k in range(8)],
                         reads=[b_hT, b_win], writes=[b_pt])
                return pt, b_pt

            cm, b_cm = gg, b_gg
            cs_, b_cs_ = gsq, b_gsq
            kb.dma("sp", lambda e: e.dma_start(out=cm[:], in_=mod_d[:, 7 * 1024:8 * 1024]), writes=[b_cm], extra=modt)
            kb.dma("sp", lambda e: e.dma_start(out=cs_[:], in_=mod_d[:, 6 * 1024:7 * 1024]), writes=[b_cs_], extra=modt)
            for ci in range(2):
                xt, b_xt = xts[ci % 2]
                kb.dma("sp", lambda e, ci=ci, xt=xt: e.dma_start(out=xt[:], in_=ctx_d[ci * 128:(ci + 1) * 128, :]), writes=[b_xt])
                hT, b_hT = ln_mod_T(xt, b_xt, cm[:], cs_[:], [b_cm, b_cs_], ci % 2)
                pt, b_pt = proj_kv(hT, b_hT)
                kb.op("act", lambda e, pt=pt: e.copy(out=rk[:], in_=pt[:, 0:128]), reads=[b_pt], writes=[b_rk])
                kb.op("act", lambda e, pt=pt, ci=ci: e.copy(out=VVc[:, ci, :], in_=pt[:, 128:256]), reads=[b_pt], writes=[b_VVc])
                transposes([rk[:]], [b_rk], ident_b, KTc[:, ci * 128:(ci + 1) * 128], b_KTc)

            class TS:
                pass

            def stage_kv(t):
                S = TS()
                S.t = t
                slot = t + 1
                xt, b_xt = xts[slot % 2]
                if t < 0:
                    src = xh_d[0:128, :]
                elif t >= NT:
                    src = xh_d[128:256, :]
                else:
                    src = x_d[t * 128:(t + 1) * 128, :]
                kb.dma("pool", lambda e: e.dma_start(out=xt[:], in_=src), writes=[b_xt])
                tab, b_tab = ropes[slot % 2]
                kb.dma("pool", lambda e: e.dma_start(out=tab[:], in_=rope_d[slot * 128:(slot + 1) * 128, :]), writes=[b_tab])
                S.tab, S.b_tab = tab, b_tab
                S.hT, S.b_hT = ln_mod_T(xt, b_xt, modA[:, 1, :], modA[:, 0, :], [b_modA], slot % 2)
                kvp, b_kvp = proj_kv(S.hT, S.b_hT)
                if KDBG == "norope":
                    kb.op("act", lambda e: e.copy(out=rk[:], in_=kvp[:, 0:128]), reads=[b_kvp], writes=[b_rk])
                else:
                    rope_apply(kvp[:, 0:128], [b_kvp], 2, tab, b_tab, rk[:], b_rk)
                kb.op("act", lambda e: e.copy(out=VV[:, slot, :], in_=kvp[:, 128:256]), reads=[b_kvp], writes=[b_VV])
                transposes([rk[:]], [b_rk], ident_b, KT[:, slot * 128:(slot + 1) * 128], b_KT)
                return S

            def stage_pre(S):
                hT, b_hT = S.hT, S.b_hT
                for gi in range(2):
                    pg, b_pg = nextps()
                    tg, b_tg = tgs[gi]
                    kb.group("pe", [lambda e, k=k, g=g, gi=gi, pg=pg: e.matmul(
                        pg[:, g * 512:(g + 1) * 512], lhsT=hT[:, k * 128:(k + 1) * 128],
                        rhs=win_b[:, k, 1792 + gi * 1024 + g * 512:1792 + gi * 1024 + (g + 1) * 512],
                        start=(k == 0), stop=(k == 7)) for g in range(2) for k in range(8)],
                        reads=[b_hT, b_win], writes=[b_pg])
                    kb.op("act", lambda e, pg=pg, tg=tg: e.activation(out=tg[:], in_=pg[:], func=AF.Tanh, scale=0.5), reads=[b_pg], writes=[b_tg])

            def stage_main(S):
                t = S.t
                slot = t + 1
                hT, b_hT = S.hT, S.b_hT
                pq, b_pq = PS[2]
                kb.group("pe", [lambda e, k=k: e.matmul(pq[:, 0:512], lhsT=hT[:, k * 128:(k + 1) * 128], rhs=win_b[:, k, 0:512],
                                                        start=(k == 0), stop=(k == 7)) for k in range(8)],
                         reads=[b_hT, b_win], writes=[b_pq])
                rope_apply(pq[:, 0:512], [b_pq], 8, S.tab, S.b_tab, rq[:].rearrange("p (c a d) -> p a c d", a=2, d=64), b_rq,
                           view=lambda ap: ap.rearrange("p (a c d) -> p a c d", a=2, d=64))
                transposes([rq[:, c * 128:(c + 1) * 128] for c in range(4)], [b_rq], ident_b, QT[:], b_QT, pst=PS[2])
                puv, b_puv = PS[3]
                kb.group("pe", [lambda e, k=k, g=g: e.matmul(puv[:, g * 512:(g + 1) * 512], lhsT=hT[:, k * 128:(k + 1) * 128],
                                                             rhs=win_b[:, k, 768 + g * 512:768 + (g + 1) * 512],
                                                             start=(k == 0), stop=(k == 7)) for g in range(2) for k in range(8)],
                         reads=[b_hT, b_win], writes=[b_puv])
                kb.op("act", lambda e: e.activation(out=gsq[:], in_=puv[:], func=AF.Square), reads=[b_puv], writes=[b_gsq])
                kb.op("dve", lambda e: e.tensor_scalar(out=gsq[:], in0=gsq[:], scalar1=0.044715, scalar2=1.0, op0=ALU.mult, op1=ALU.add),
                      reads=[b_gsq], writes=[b_gsq])
                kb.op("dve", lambda e: e.tensor_tensor(out=gsq[:], in0=gsq[:], in1=puv[:], op=ALU.mult),
                      reads=[b_gsq, b_puv], writes=[b_gsq])
                kb.op("act", lambda e: e.activation(out=gsq[:], in_=gsq[:], func=AF.Tanh, scale=GC), reads=[b_gsq], writes=[b_gsq])
                kb.op("dve", lambda e: e.scalar_tensor_tensor(out=gg[:], in0=gsq[:], scalar=1.0, in1=puv[:], op0=ALU.add, op1=ALU.mult),
                      reads=[b_gsq, b_puv], writes=[b_gg])
                layernorm(gg[:, 512:1024], b_gg, 512, gmln_t[:, 0:512], gmln_t[:, 512:1024], [b_gmln], vgm[:], b_vgm, eps=4.0 * LN_EPS)
                po = [PS[0], PS[1]]
                blocks = [("c", 0), ("c", 1), ("l", slot - 1), ("l", slot), ("l", slot + 1)]
                steps = [(g, bi) for g in range(2) for bi in range(5)]

                def kv_of(g, bi):
                    kind, idx = blocks[bi]
                    if kind == "c":
                        return (KTc[g * 64:(g + 1) * 64, idx * 128:(idx + 1) * 128], VVc[:, idx, g * 64:(g + 1) * 64], [b_KTc, b_VVc])
                    return (KT[g * 64:(g + 1) * 64, idx * 128:(idx + 1) * 128], VV[:, idx, g * 64:(g + 1) * 64], [b_KT, b_VV])

                def issue_S(si):
                    g, bi = steps[si]
                    kt, vt, rkv = kv_of(g, bi)
                    pS, b_pS = PS[2 + (si % 2)]
                    kb.op("pe", lambda e: e.matmul(pS[:, 0:512], lhsT=kt, rhs=QT[g * 64:(g + 1) * 64, :], start=True, stop=True),
                          reads=rkv + [b_QT], writes=[b_pS])

                issue_S(0)
                for si, (g, bi) in enumerate(steps):
                    if si + 1 < len(steps):
                        issue_S(si + 1)
                    pog, b_pog = po[g]
                    pS, b_pS = PS[2 + (si % 2)]
                    kt, vt, rkv = kv_of(g, bi)
                    ET, b_ET = ETs[eti[0] % 4]
                    eti[0] += 1
                    kb.op("act", lambda e, ET=ET, pS=pS: e.activation(out=ET[:], in_=pS[:, 0:512], func=AF.Exp, scale=0.125),
                          reads=[b_pS], writes=[b_ET])
                    mi = None
                    if bi == 2:
                        mi = 2 if t == 0 else 0
                    if bi == 4:
                        mi = 3 if t == NT - 1 else 1
                    if mi is not None:
                        mb = msk_b[:, mi * 128:(mi + 1) * 128].unsqueeze(1).to_broadcast([128, 4, 128])
                        kb.op("dve", lambda e, ET=ET, mb=mb: e.tensor_tensor(out=ET[:].rearrange("p (h q) -> p h q", h=4),
                                                                           in0=ET[:].rearrange("p (h q) -> p h q", h=4), in1=mb, op=ALU.mult),
                              reads=[b_ET, b_msk], writes=[b_ET])
                    fns = [lambda e, h=h, ET=ET, vt=vt, pog=pog, bi=bi: e.matmul(pog[0:64, h * 128:(h + 1) * 128], lhsT=vt,
                                                                              rhs=ET[:, h * 128:(h + 1) * 128],
                                                                              start=(bi == 0 and h == 0), stop=(bi == 4 and h == 3),
                                                                              skip_group_check=True) for h in range(4)]
                    fns.append(lambda e, ET=ET, pog=pog, bi=bi: e.matmul(pog[0:64, 512:1024], lhsT=ones_b[:, 0:64], rhs=ET[:],
                                                                       start=(bi == 0), stop=(bi == 4), skip_group_check=True))
                    kb.group("pe", fns, reads=rkv + [b_ET, b_cst_b], writes=[b_pog])
                    if bi == 4:
                        kb.op("dve", lambda e, pog=pog, g=g: e.tensor_tensor(out=dens[:], in0=pog[0:64, 512:1024],
                                                                           in1=esink[:, g * 512:(g + 1) * 512], op=ALU.add),
                              reads=[b_pog, b_esink], writes=[b_dens])
                        kb.op("dve", lambda e: e.reciprocal(out=dens[:], in_=dens[:]), reads=[b_dens], writes=[b_dens])
                        kb.op("dve", lambda e, pog=pog, g=g: e.tensor_tensor(out=oT[:, g * 4:(g + 1) * 4, :].rearrange("p h q -> p (h q)"),
                                                                           in0=pog[0:64, 0:512], in1=dens[:], op=ALU.mult),
                              reads=[b_pog, b_dens], writes=[b_oT])
                pya, b_pya = nextps()
                kb.group("pe", [lambda e, h=h, hf=hf: e.matmul(pya[:, hf * 512:(hf + 1) * 512], lhsT=oT[:, h, :],
                                                               rhs=wpa_b[:, h, hf * 512:(hf + 1) * 512], start=(h == 0), stop=(h == 7))
                                for hf in range(2) for h in range(8)], reads=[b_oT, b_wpa], writes=[b_pya])

                kb.op("dve", lambda e: e.scalar_tensor_tensor(out=yp1[:], in0=yp1[:], scalar=1.0, in1=pya[:], op0=ALU.add, op1=ALU.mult),
                      reads=[b_yp1, b_pya], writes=[b_yp1])
                if dbg:
                    y_toks.append(kb.dma("sp", lambda e: e.dma_start(out=dbgA_d[t * 128:(t + 1) * 128, :], in_=yp1[:]), reads=[b_yp1]))
                psp, b_psp = nextps()
                kb.group("pe", [lambda e, g=g: e.matmul(psp[:, g * 64:(g + 1) * 64], lhsT=wsT_b[:, g * 128:(g + 1) * 128],
                                                        rhs=vgm[:, g * 64:(g + 1) * 64], start=True, stop=True) for g in range(8)],
                         reads=[b_wsT, b_vgm], writes=[b_psp])
                bsb = bsT_t[:, 0:8].unsqueeze(2).to_broadcast([128, 8, 64])
                kb.op("dve", lambda e: e.tensor_tensor(out=spb[:].rearrange("p (g c) -> p g c", g=8),
                                                       in0=psp[:, 0:512].rearrange("p (g c) -> p g c", g=8), in1=bsb, op=ALU.add),
                      reads=[b_psp, b_bsT], writes=[b_spb])
                kb.op("dve", lambda e: e.scalar_tensor_tensor(out=ygm[:], in0=spb[:], scalar=0.5, in1=gg[:, 0:512], op0=ALU.mult, op1=ALU.mult),
                      reads=[b_spb, b_gg], writes=[b_ygm])
                transposes([ygm[:, k * 128:(k + 1) * 128] for k in range(4)], [b_ygm], ident_b, ygT[:], b_ygT)
                pyb, b_pyb = nextps()
                kb.group("pe", [lambda e, k=k, hf=hf: e.matmul(pyb[:, hf * 512:(hf + 1) * 512], lhsT=ygT[:, k * 128:(k + 1) * 128],
                                                               rhs=wpb_b[:, k, hf * 512:(hf + 1) * 512], start=(k == 0), stop=(k == 3))
                                for hf in range(2) for k in range(4)], reads=[b_ygT, b_wpb], writes=[b_pyb])
                kb.op("dve", lambda e: e.scalar_tensor_tensor(out=tgs[1][0][:], in0=tgs[1][0][:], scalar=1.0, in1=pyb[:], op0=ALU.add, op1=ALU.mult),
                      reads=[tgs[1][1], b_pyb], writes=[tgs[1][1]])
                y2b, b_y2b = y2bs[t % 2]
                kb.op("dve", lambda e: e.tensor_tensor(out=y2b[:], in0=yp1[:], in1=tgs[1][0][:], op=ALU.add),
                      reads=[b_yp1, tgs[1][1]], writes=[b_y2b])
                y_toks.append(kb.dma("sp", lambda e: e.dma_start(out=y_d[t * 128:(t + 1) * 128, :], in_=y2b[:]), reads=[b_y2b]))

            if stop == "A0":
                kb.barrier_all()
                return nc
            stage_kv(-1)
            Scur = stage_kv(0)
            if stop == "A1":
                kb.barrier_all()
                return nc
            b_conv = Buf("wconv")
            conv_jobs = [(mi, j) for mi in range(3) for j in range(16)]
            wsrc = [w1_d, w3_d, w2_d]
            for t in range(nta):
                stage_pre(Scur)
                Snext = stage_kv(t + 1)
                for _ in range(2):
                    if conv_jobs:
                        mi, j = conv_jobs.pop(0)
                        conv_toks.append(kb.dma("pool", lambda e, mi=mi, j=j: e.dma_start(
                            out=wq_d[mi][j * 512:(j + 1) * 512, :], in_=wsrc[mi][j * 512:(j + 1) * 512, :]), owner=b_conv, reads=[b_conv]))
                stage_main(Scur)
                Scur = Snext
            kb.barrier_all()
            if stop == "A":
                return nc

        x1_toks = []
        h2_toks = []
        with ExitStack() as ph:
            mk = mkp(ph)
            zt, b_zt = mk("zt", [128, 8192], BF16)
            kb.op("pool", lambda e: e.memset(zt[:], 0.0), writes=[b_zt])
            xs_v = xs_d.rearrange("(p a) n -> p (a n)", p=128)
            b_zfill = Buf("zfill")
            zf_tok = None
            for i in range(16):
                zf_tok = kb.dma("sp", lambda e, i=i: e.dma_start(out=xs_v[:, i * 8192:(i + 1) * 8192], in_=zt[:]),
                                reads=[b_zt], owner=b_zfill)
            wo_b, b_wo = mk("wo_b", [128, 8, 1024], BF16)
            kb.dma("pool", lambda e: e.dma_start(out=wo_b[:], in_=wo_d.rearrange("(k p) n -> p k n", p=128)), writes=[b_wo])
            modB, b_modB = mk("modB", [128, 3, 1024], F32)
            kb.dma("sp", lambda e: e.dma_start(out=modB[:].rearrange("p a n -> p (a n)"), in_=mod_d[:, 2048:5120]), writes=[b_modB])
            lnB, b_lnB = mk("lnB", [128, 2, 1024], F32)
            kb.dma("sp", lambda e: e.dma_start(out=lnB[:].rearrange("p a n -> p (a n)"), in_=lnp_d[:, 0:2048]), writes=[b_lnB])
            br_t, b_br = mk("br_t", [128, 36], F32)
            kb.dma("sp", lambda e: e.dma_start(out=br_t[:], in_=br_d), writes=[b_br])
            wr_t, b_wr = mk("wr_t", [128, 8, 36], F32)
            kb.dma("sp", lambda e: e.dma_start(out=wr_t[:], in_=wr_d.rearrange("(k p) n -> p k n", p=128)), writes=[b_wr])
            kb.op("pool", lambda e: e.memset(Cc[:], 0.0), writes=[b_Cc])
            xts = [mk("xtB%d" % i, [128, 1024], F32) for i in range(3)]
            ybs = [mk("ybB%d" % i, [128, 1024], BF16) for i in range(3)]
            yT, b_yT = mk("yT", [128, 1024], BF16)
            z1, b_z1 = mk("z1", [128, 1024], F32)
            x1s = [mk("x1_%d" % i, [128, 1024], F32) for i in range(2)]
            h2f, b_h2f = mk("h2f", [128, 1024], F32)
            h2bs = [mk("h2b%d" % i, [128, 1024], BF16) for i in range(2)]
            h2T, b_h2T = mk("h2T", [128, 1024], F32)
            lg, b_lg = mk("lg", [128, 36], F32)
            rs, b_rs = mk("rs", [128, 96], F32)
            A1f, b_A1f = mk("A1f", [128, 64], F32)
            cs, b_cs = mk("cs", [128, 96], F32)
            yt_ = maxtoks(y_toks)
            def loadB(t):
                xt, b_xt = xts[t % 3]
                kb.dma("pool", lambda e: e.dma_start(out=xt[:], in_=x_d[t * 128:(t + 1) * 128, :]), writes=[b_xt])
                yb, b_yb = ybs[t % 3]
                kb.dma("pool", lambda e: e.dma_start(out=yb[:], in_=y_d[t * 128:(t + 1) * 128, :]), writes=[b_yb], extra=yt_)

            def frontB(t):
                yb, b_yb = ybs[t % 3]
                transposes([yb[:, k * 128:(k + 1) * 128] for k in range(8)], [b_yb], ident_b, yT[:], b_yT, pst=PS[2])
                pmx, b_pmx = PS[t % 2]
                kb.group("pe", [lambda e, k=k, hf=hf: e.matmul(pmx[:, hf * 512:(hf + 1) * 512], lhsT=yT[:, k * 128:(k + 1) * 128],
                                                               rhs=wo_b[:, k, hf * 512:(hf + 1) * 512], start=(k == 0), stop=(k == 7))
                                for hf in range(2) for k in range(8)], reads=[b_yT, b_wo], writes=[b_pmx])

            def backB(t):
                xt, b_xt = xts[t % 3]
                pmx, b_pmx = PS[t % 2]
                kb.op("dve", lambda e: e.tensor_tensor(out=z1[:], in0=pmx[:], in1=modB[:, 0, :], op=ALU.mult),
                      reads=[b_pmx, b_modB], writes=[b_z1])
                kb.op("dve", lambda e: e.scalar_tensor_tensor(out=z1[:], in0=xt[:], scalar=ALPHA, in1=z1[:], op0=ALU.mult, op1=ALU.add),
                      reads=[b_xt, b_z1], writes=[b_z1])
                x1, b_x1 = x1s[t % 2]
                layernorm(z1, b_z1, 1024, lnB[:, 0, :], lnB[:, 1, :], [b_lnB], x1[:], b_x1)
                x1_toks.append(kb.dma("sp", lambda e: e.dma_start(out=x1_d[t * 128:(t + 1) * 128, :], in_=x1[:]), reads=[b_x1]))
                layernorm(x1, b_x1, 1024, modB[:, 2, :], modB[:, 1, :], [b_modB], h2f[:], b_h2f)
                h2b, b_h2b = h2bs[t % 2]
                kb.op("act", lambda e: e.copy(out=h2b[:].rearrange("t (j p) -> t p j", j=8),
                                              in_=h2f[:].rearrange("t (p j) -> t p j", j=8)), reads=[b_h2f], writes=[b_h2b])
                h2_toks.append(kb.dma("sp", lambda e: e.dma_start(out=h2_d[t * 128:(t + 1) * 128, :], in_=h2b[:]), reads=[b_h2b]))
                for hf in range(2):
                    transposes([h2f[:, (hf * 4 + k) * 128:(hf * 4 + k + 1) * 128] for k in range(4)], [b_h2f], ident_f,
                               h2T[:, hf * 512:(hf + 1) * 512], b_h2T, dt_bf=False, pst=PS[3])
                plg, b_plg = PS[3]
                kb.group("pe", [lambda e, k=k: e.matmul(plg[:, 0:36], lhsT=h2T[:, k * 128:(k + 1) * 128], rhs=wr_t[:, k, :],
                                                        start=(k == 0), stop=(k == 7)) for k in range(8)],
                         reads=[b_h2T, b_wr], writes=[b_plg])
                kb.op("dve", lambda e: e.tensor_tensor(out=lg[:], in0=plg[:, 0:36], in1=br_t[:], op=ALU.add),
                      reads=[b_plg, b_br], writes=[b_lg])

                def dv(fn, reads=(), writes=()):
                    kb.op("dve", fn, reads=[b_rs, b_lg] + list(reads), writes=[b_rs] + list(writes))
                dv(lambda e: e.reduce_max(out=rs[:, 0:1], in_=lg[:, 0:4], axis=AX.X))
                dv(lambda e: e.tensor_scalar(out=rs[:, 1:2], in0=rs[:, 0:1], scalar1=-1.0, scalar2=None, op0=ALU.mult))
                kb.op("act", lambda e: e.activation(out=rs[:, 4:8], in_=lg[:, 0:4], func=AF.Exp, bias=rs[:, 1:2], scale=1.0),
                      reads=[b_rs, b_lg], writes=[b_rs])
                dv(lambda e: e.reduce_sum(out=rs[:, 2:3], in_=rs[:, 4:8], axis=AX.X))
                dv(lambda e: e.reciprocal(out=rs[:, 3:4], in_=rs[:, 2:3]))
                dv(lambda e: e.tensor_scalar(out=rs[:, 8:12], in0=lg[:, 0:4], scalar1=rs[:, 0:1], scalar2=None, op0=ALU.is_equal))
                dv(lambda e: e.tensor_tensor(out=rs[:, 12:44].rearrange("p (g x) -> p g x", g=4),
                                             in0=lg[:, 4:36].rearrange("p (g x) -> p g x", g=4),
                                             in1=rs[:, 8:12].unsqueeze(2).to_broadcast([128, 4, 8]), op=ALU.mult))
                dv(lambda e: e.tensor_reduce(out=rs[:, 44:52], in_=rs[:, 12:44].rearrange("p (g x) -> p x g", g=4), axis=AX.X, op=ALU.add))
                dv(lambda e: e.max(out=rs[:, 52:60], in_=rs[:, 44:52]))
                dv(lambda e: e.tensor_scalar(out=rs[:, 60:68], in0=rs[:, 44:52], scalar1=rs[:, 52:53], scalar2=None, op0=ALU.is_equal))
                dv(lambda e: e.tensor_scalar(out=rs[:, 68:76], in0=rs[:, 44:52], scalar1=rs[:, 53:54], scalar2=None, op0=ALU.is_equal))
                dv(lambda e: e.tensor_tensor(out=rs[:, 76:77], in0=rs[:, 53:54], in1=rs[:, 52:53], op=ALU.subtract))
                kb.op("act", lambda e: e.activation(out=rs[:, 77:78], in_=rs[:, 76:77], func=AF.Exp), reads=[b_rs], writes=[b_rs])
                dv(lambda e: e.tensor_scalar(out=rs[:, 78:79], in0=rs[:, 77:78], scalar1=1.0, scalar2=None, op0=ALU.add))
                dv(lambda e: e.reciprocal(out=rs[:, 79:80], in_=rs[:, 78:79]))
                dv(lambda e: e.tensor_tensor(out=RK[:, 2, t:t + 1], in0=rs[:, 3:4], in1=rs[:, 79:80], op=ALU.mult), writes=[b_RK])
                dv(lambda e: e.tensor_tensor(out=RK[:, 3, t:t + 1], in0=RK[:, 2, t:t + 1], in1=rs[:, 77:78], op=ALU.mult),
                   reads=[b_RK], writes=[b_RK])
                for k2 in range(2):
                    dv(lambda e, k2=k2: e.tensor_tensor(out=A1f[:, k2 * 32:(k2 + 1) * 32].rearrange("p (g x) -> p g x", g=4),
                                                      in0=rs[:, 8:12].unsqueeze(2).to_broadcast([128, 4, 8]),
                                                      in1=rs[:, 60 + 8 * k2:68 + 8 * k2].unsqueeze(1).to_broadcast([128, 4, 8]), op=ALU.mult),
                       reads=[b_A1f], writes=[b_A1f])
                kb.op("dve", lambda e: e.tensor_copy(out=A12all[:, t, :], in_=A1f[:]), reads=[b_A1f], writes=[b_A12all])
                pc, b_pc = PS[3]
                kb.group("pe", [lambda e: e.matmul(pc[:, 0:64], lhsT=tri_b, rhs=A12all[:, t, :], start=True, stop=True),
                                lambda e: e.matmul(pc[:, 64:128], lhsT=ones_b, rhs=A12all[:, t, :], start=True, stop=True)],
                         reads=[b_cst_b, b_A12all], writes=[b_pc])
                kb.op("dve", lambda e: e.tensor_tensor(out=cs[:, 0:32], in0=pc[:, 0:32], in1=Cc[:], op=ALU.add),
                      reads=[b_pc, b_Cc], writes=[b_cs])
                kb.op("dve", lambda e: e.tensor_tensor(out=cs[:, 64:96], in0=pc[:, 64:96], in1=Cc[:], op=ALU.add),
                      reads=[b_pc, b_Cc, b_cs], writes=[b_cs])
                kb.op("dve", lambda e: e.tensor_tensor(out=cs[:, 32:64], in0=pc[:, 32:64], in1=cs[:, 64:96], op=ALU.add),
                      reads=[b_pc, b_cs], writes=[b_cs])
                kb.op("dve", lambda e: e.tensor_tensor(out=cs[:, 0:64], in0=cs[:, 0:64], in1=A1f[:], op=ALU.mult),
                      reads=[b_cs, b_A1f], writes=[b_cs])
                kb.op("dve", lambda e: e.tensor_reduce(out=RK[:, 0:2, t:t + 1].rearrange("p k o -> p (k o)"),
                                                       in_=cs[:, 0:64].rearrange("p (k x) -> p k x", k=2), axis=AX.X, op=ALU.add),
                      reads=[b_cs, b_RK], writes=[b_RK])
                kb.op("dve", lambda e: e.tensor_tensor(out=Cc[:], in0=cs[:, 64:96], in1=pc[:, 96:128], op=ALU.add),
                      reads=[b_cs, b_pc, b_Cc], writes=[b_Cc])

            loadB(0)
            loadB(1)
            frontB(0)
            for t in range(NT):
                if t + 2 < NT:
                    loadB(t + 2)
                if t + 1 < NT:
                    frontB(t + 1)
                backB(t)
            kb.barrier_all()
            if stop == "B":
                return nc

        ys_toks = []
        with ExitStack() as ph:
            mk = mkp(ph)
            big, b_big = mk("big", [128, 3072], F32)
            s256 = cst_f[:, S128:S128 + 64]
            kb.op("dve", lambda e: e.tensor_tensor(out=big[:, 0:1056].rearrange("p (x j) -> p x j", j=33),
                                                   in0=Cc[:].unsqueeze(2).to_broadcast([128, 32, 33]),
                                                   in1=s256[:, 0:33].unsqueeze(1).to_broadcast([128, 32, 33]), op=ALU.is_gt),
                  reads=[b_Cc, b_cst_f], writes=[b_big])
            pe_, b_pe_ = mk("pend", [128, 4, 32], F32)
            kb.op("dve", lambda e: e.tensor_reduce(out=pe_[:, 0, :], in_=big[:, 0:1056].rearrange("p (x j) -> p x j", j=33), axis=AX.X, op=ALU.add),
                  reads=[b_big], writes=[b_pe_])
            kb.op("dve", lambda e: e.tensor_scalar(out=pe_[:, 0, :], in0=pe_[:, 0, :], scalar1=256.0, scalar2=None, op0=ALU.mult),
                  reads=[b_pe_], writes=[b_pe_])
            kb.op("dve", lambda e: e.tensor_copy(out=pe_[:, 1, :], in_=pe_[:, 0, :]), reads=[b_pe_], writes=[b_pe_])
            cur = 1
            for k in (1, 2, 4, 8, 16):
                nxt = 3 - cur
                kb.op("dve", lambda e, cur=cur, nxt=nxt, k=k: e.tensor_copy(out=pe_[:, nxt, 0:k], in_=pe_[:, cur, 0:k]),
                      reads=[b_pe_], writes=[b_pe_])
                kb.op("dve", lambda e, cur=cur, nxt=nxt, k=k: e.tensor_tensor(out=pe_[:, nxt, k:32], in0=pe_[:, cur, k:32],
                                                                           in1=pe_[:, cur, 0:32 - k], op=ALU.add),
                      reads=[b_pe_], writes=[b_pe_])
                cur = nxt
            pend = pe_[:, cur, :]
            kb.op("dve", lambda e: e.tensor_tensor(out=pe_[:, 3, :], in0=pend, in1=pe_[:, 0, :], op=ALU.subtract),
                  reads=[b_pe_], writes=[b_pe_])
            posf, b_posf = mk("posf", [128, 2, NT], F32)
            for k2 in range(2):
                kb.op("dve", lambda e, k2=k2: e.tensor_tensor(out=big[:, 0:1024].rearrange("p (t x) -> p t x", x=32),
                                                            in0=A12all[:, :, k2 * 32:(k2 + 1) * 32],
                                                            in1=pe_[:, 3, :].unsqueeze(1).to_broadcast([128, NT, 32]), op=ALU.mult),
                      reads=[b_A12all, b_pe_, b_big], writes=[b_big])
                kb.op("dve", lambda e, k2=k2: e.tensor_reduce(out=posf[:, k2, :], in_=big[:, 0:1024].rearrange("p (t x) -> p t x", x=32),
                                                            axis=AX.X, op=ALU.add), reads=[b_big, b_posf], writes=[b_posf])
                kb.op("dve", lambda e, k2=k2: e.tensor_tensor(out=posf[:, k2, :], in0=posf[:, k2, :], in1=RK[:, k2, :], op=ALU.add),
                      reads=[b_posf, b_RK], writes=[b_posf])
            kb.op("dve", lambda e: e.tensor_copy(out=posi[:], in_=posf[:]), reads=[b_posf], writes=[b_posi])
            kb.op("dve", lambda e: e.tensor_tensor(out=big[:, 0:2048].rearrange("p (s x) -> p s x", x=32),
                                                   in0=pend.unsqueeze(1).to_broadcast([128, NPAIR, 32]),
                                                   in1=s256.unsqueeze(2).to_broadcast([128, NPAIR, 32]), op=ALU.is_le),
                  reads=[b_pe_, b_cst_f, b_big], writes=[b_big])
            wif, b_wif = mk("wif", [128, NPAIR], F32)
            widx, b_widx = mk("widx", [128, 2, NPAIR], I32)
            wif2, b_wif2 = mk("wif2", [128, 2, NPAIR], F32)
            kb.op("dve", lambda e: e.tensor_reduce(out=wif[:], in_=big[:, 0:2048].rearrange("p (s x) -> p s x", x=32), axis=AX.X, op=ALU.add),
                  reads=[b_big], writes=[b_wif])
            kb.op("dve", lambda e: e.tensor_scalar(out=wif[:], in0=wif[:], scalar1=31.0, scalar2=128.0, op0=ALU.min, op1=ALU.mult),
                  reads=[b_wif], writes=[b_wif])
            kb.op("dve", lambda e: e.tensor_scalar(out=wif[:], in0=wif[:], scalar1=cst_f[:, PIDX:PIDX + 1], scalar2=None, op0=ALU.add),
                  reads=[b_wif, b_cst_f], writes=[b_wif])
            for hf in range(2):
                kb.op("dve", lambda e, hf=hf: e.tensor_scalar(out=wif2[:, hf, :], in0=wif[:], scalar1=2.0, scalar2=float(hf),
                                                            op0=ALU.mult, op1=ALU.add), reads=[b_wif, b_wif2], writes=[b_wif2])
            kb.op("dve", lambda e: e.tensor_copy(out=widx[:], in_=wif2[:]), reads=[b_wif2], writes=[b_widx])
            widx1, _b = mk("widx1", [128, NPAIR], I32)
            kb.op("dve", lambda e: e.tensor_copy(out=widx1[:], in_=wif[:]), reads=[b_wif, b_widx], writes=[b_widx])

            h2bs = [mk("h2c%d" % i, [128, 1024], BF16) for i in range(2)]
            sc_toks = []
            h2t_ = maxtoks(h2_toks)
            for t in range(NT):
                h2b, b_h2b = h2bs[t % 2]
                kb.dma("sp", lambda e: e.dma_start(out=h2b[:], in_=h2_d[t * 128:(t + 1) * 128, :]), writes=[b_h2b], extra=h2t_)
                for k2 in range(2):
                    sc_toks.append(kb.dma("pool", lambda e, k2=k2: e.indirect_dma_start(
                        out=xs_d, out_offset=bass.IndirectOffsetOnAxis(ap=posi[:, k2, t:t + 1], axis=0), in_=h2b[:], in_offset=None),
                        reads=[b_h2b, b_posi], owner=b_h2b, extra=[zf_tok]))

            wbufs = [(mk("w1b%d" % i, [128, 4096], BF16), mk("w3b%d" % i, [128, 4096], BF16), mk("w2b%d" % i, [128, 4096], BF16))
                     for i in range(2)]
            xbs = [mk("xb%d" % i, [128, 1024], BF16) for i in range(4)]
            xbT, b_xbT = mk("xbT", [128, 1024], BF16)
            hid, b_hid = mk("hid", [128, 512], BF16)
            hidT, b_hidT = mk("hidT", [128, 512], BF16)
            tht, b_tht = mk("tht", [128, 512], F32)
            ysbs = [mk("ysb%d" % i, [128, 1024], F32) for i in range(2)]
            sct = maxtoks(sc_toks)
            if stop == "C":
                kb.barrier_all()
                return nc
            convt = maxtoks(conv_toks)
            wq_v = [w.rearrange("(r h) c -> r (h c)", h=2) for w in wq_d]

            def loadW(pr):
                (w1b, b_w1b), (w3b, b_w3b), (w2b, b_w2b) = wbufs[pr % 2]
                for (wb_, bw_, wd_) in ((w1b, b_w1b, wq_v[0]), (w3b, b_w3b, wq_v[1]), (w2b, b_w2b, wq_v[2])):
                    kb.dma("pool", lambda e, wb_=wb_, wd_=wd_: e.indirect_dma_start(
                        out=wb_[:], out_offset=None, in_=wd_,
                        in_offset=bass.IndirectOffsetOnAxis(ap=widx1[:, pr:pr + 1], axis=0)),
                        reads=[b_widx], writes=[bw_], extra=convt)

            def loadX(s):
                xb, b_xb = xbs[s % 4]
                kb.dma("pool", lambda e: e.dma_start(out=xb[:], in_=xs_d[s * 128:(s + 1) * 128, :]), writes=[b_xb], extra=sct)

            def frontD_a(s):
                xb, b_xb = xbs[s % 4]
                pt, b_pt = PS[2]
                pv = bfv(pt)
                kb.group("pe", [lambda e, i=i: e.transpose(pv[:, i * 128:(i + 1) * 128], xb[:, i * 128:(i + 1) * 128], ident_b)
                                for i in range(8)], reads=[b_xb, b_cst_b], writes=[b_pt])

            def frontD_b(s):
                pr = s // 2
                (w1b, b_w1b), (w3b, b_w3b), (w2b, b_w2b) = wbufs[pr % 2]
                pt, b_pt = PS[2]
                pv = bfv(pt)
                kb.op("act", lambda e: e.copy(out=xbT[:], in_=pv[:, 0:1024]), reads=[b_pt], writes=[b_xbT])
                pab, b_pab = PS[s % 2]
                kb.group("pe", [lambda e, k=k, wq=wq, o=o: e.matmul(pab[:, o * 512:(o + 1) * 512], lhsT=xbT[:, k * 128:(k + 1) * 128],
                                                                    rhs=wq[:, k * 512:(k + 1) * 512], start=(k == 0), stop=(k == 7))
                                for o, wq in ((0, w1b), (1, w3b)) for k in range(8)],
                         reads=[b_xbT, b_w1b, b_w3b], writes=[b_pab])

            def backD_a(s):
                pab, b_pab = PS[s % 2]
                kb.op("act", lambda e: e.activation(out=tht[:], in_=pab[:, 0:512], func=AF.Tanh, scale=0.5), reads=[b_pab], writes=[b_tht])
                kb.op("dve", lambda e: e.scalar_tensor_tensor(out=tht[:], in0=tht[:], scalar=1.0, in1=pab[:, 0:512], op0=ALU.add, op1=ALU.mult),
                      reads=[b_tht, b_pab], writes=[b_tht])
                kb.op("dve", lambda e: e.tensor_tensor(out=hid[:].rearrange("t (j p) -> t p j", j=4),
                                                       in0=tht[:].rearrange("t (p j) -> t p j", j=4),
                                                       in1=pab[:, 512:1024].rearrange("t (p j) -> t p j", j=4), op=ALU.mult),
                      reads=[b_tht, b_pab], writes=[b_hid])

            def backD_b(s):
                pr = s // 2
                (w1b, b_w1b), (w3b, b_w3b), (w2b, b_w2b) = wbufs[pr % 2]
                transposes([hid[:, k * 128:(k + 1) * 128] for k in range(4)], [b_hid], ident_b, hidT[:], b_hidT, pst=PS[3])
                py, b_py = PS[3]
                kb.group("pe", [lambda e, k=k, hf=hf: e.matmul(py[:, hf * 512:(hf + 1) * 512], lhsT=hidT[:, k * 128:(k + 1) * 128],
                                                               rhs=w2b[:, k * 1024 + hf * 512:k * 1024 + (hf + 1) * 512],
                                                               start=(k == 0), stop=(k == 3)) for hf in range(2) for k in range(4)],
                         reads=[b_hidT, b_w2b], writes=[b_py])
                ysb, b_ysb = ysbs[s % 2]
                kb.op("act", lambda e: e.activation(out=ysb[:], in_=py[:], func=AF.Copy, scale=0.5), reads=[b_py], writes=[b_ysb])
                ys_toks.append(kb.dma("sp", lambda e: e.dma_start(out=ys_d[s * 128:(s + 1) * 128, :], in_=ysb[:]), reads=[b_ysb]))

            loadW(0)
            for s0 in range(3):
                loadX(s0)
            frontD_a(0)
            frontD_b(0)
            for s in range(NSLOT):
                if s + 3 < NSLOT:
                    loadX(s + 3)
                if s + 1 < NSLOT:
                    if (s + 1) % 2 == 0:
                        loadW((s + 1) // 2)
                    frontD_a(s + 1)
                backD_a(s)
                if s + 1 < NSLOT:
                    frontD_b(s + 1)
                backD_b(s)
            kb.barrier_all()
            if stop == "D":
                return nc

        with ExitStack() as ph:
            mk = mkp(ph)
            g2t, b_g2t = mk("g2t", [128, 1024], F32)
            kb.dma("sp", lambda e: e.dma_start(out=g2t[:], in_=mod_d[:, 5120:6144]), writes=[b_g2t])
            lnE, b_lnE = mk("lnE", [128, 2, 1024], F32)
            kb.dma("sp", lambda e: e.dma_start(out=lnE[:].rearrange("p a n -> p (a n)"), in_=lnp_d[:, 2048:4096]), writes=[b_lnE])
            xts = [mk("xtE%d" % i, [128, 1024], F32) for i in range(2)]
            yg = [[mk("yg%d_%d" % (k2, i), [128, 1024], F32) for i in range(2)] for k2 in range(2)]
            z1, b_z1 = mk("z1E", [128, 1024], F32)
            ots = [mk("ot%d" % i, [128, 1024], F32) for i in range(2)]
            yst = maxtoks(ys_toks)
            x1t = maxtoks(x1_toks)
            out_toks = []
            for t in range(NT):
                xt, b_xt = xts[t % 2]
                kb.dma("pool", lambda e: e.dma_start(out=xt[:], in_=x1_d[t * 128:(t + 1) * 128, :]), writes=[b_xt], extra=x1t)
                yk = []
                for k2 in range(2):
                    ykt, b_yk = yg[k2][t % 2]
                    yk.append((ykt, b_yk))
                    kb.dma("pool", lambda e, k2=k2, ykt=ykt: e.indirect_dma_start(
                        out=ykt[:], out_offset=None, in_=ys_d, in_offset=bass.IndirectOffsetOnAxis(ap=posi[:, k2, t:t + 1], axis=0)),
                        reads=[b_posi], writes=[b_yk], extra=yst)
                kb.op("dve", lambda e: e.tensor_scalar(out=z1[:], in0=yk[0][0][:], scalar1=RK[:, 2, t:t + 1], scalar2=None, op0=ALU.mult),
                      reads=[yk[0][1], b_RK], writes=[b_z1])
                kb.op("dve", lambda e: e.scalar_tensor_tensor(out=z1[:], in0=yk[1][0][:], scalar=RK[:, 3, t:t + 1], in1=z1[:],
                                                              op0=ALU.mult, op1=ALU.add), reads=[yk[1][1], b_RK, b_z1], writes=[b_z1])
                kb.op("dve", lambda e: e.tensor_tensor(out=z1[:], in0=z1[:], in1=g2t[:], op=ALU.mult),
                      reads=[b_z1, b_g2t], writes=[b_z1])
                kb.op("dve", lambda e: e.scalar_tensor_tensor(out=z1[:], in0=xt[:], scalar=ALPHA, in1=z1[:], op0=ALU.mult, op1=ALU.add),
                      reads=[b_xt, b_z1], writes=[b_z1])
                ot, b_ot = ots[t % 2]
                layernorm(z1, b_z1, 1024, lnE[:, 0, :], lnE[:, 1, :], [b_lnE], ot[:], b_ot)
                out_toks.append(kb.dma("sp", lambda e: e.dma_start(out=out_d[t * 128:(t + 1) * 128, :], in_=ot[:]), reads=[b_ot]))
            kb.wait_tok("sp", maxtoks(out_toks))
    return nc


def _rope_table():
    n = 8192
    pos = np.arange(n)
    row = pos // 64
    col = pos % 64
    inv = (1.0 / (10000.0 ** (np.arange(16, dtype=np.float32) / 16.0))).astype(np.float32)
    tabs = []
    for p in (row, col):
        ang = p.astype(np.float32)[:, None] * inv[None, :]
        tabs.append((np.cos(ang).astype(np.float32), np.sin(ang).astype(np.float32)))
    cosr, sinr = tabs[0]
    cosc, sinc = tabs[1]
    cos64 = np.concatenate([cosr, cosr, cosc, cosc], 1)
    sin64 = np.concatenate([-sinr, sinr, -sinc, sinc], 1)
    return np.concatenate([cos64, sin64], 1).astype(np.float32)


def _prep_inputs(inputs):
    f = lambda a: np.ascontiguousarray(np.asarray(a, dtype=np.float32))
    x = f(inputs["x"]); c = f(inputs["c"]); ctx = f(inputs["ctx"]); c_ctx = f(inputs["c_ctx"])
    rope = _rope_table()
    kk = np.arange(128)[:, None]
    qq = np.arange(128)[None, :]
    mP = (kk >= qq).astype(np.float32)
    mN = (kk <= qq).astype(np.float32)
    ident = np.eye(128, dtype=np.float32)
    tri = (kk < qq).astype(np.float32)
    ones = np.ones((128, 128), np.float32)
    thr = np.tile((np.arange(65, dtype=np.float32) * 128.0)[None, :], (32, 1)).reshape(1, 2080)
    s128 = (np.arange(64, dtype=np.float32) * 256.0)[None, :]
    consts = np.concatenate([ident, tri, ones, np.zeros((128, 128), np.float32),
                             np.tile(s128, (128, 1)),
                             np.arange(128, dtype=np.float32)[:, None]], 1)
    consts = np.ascontiguousarray(consts, dtype=np.float32)
    shared = {
        "w_ada": f(inputs["w_ada"][0]),
        "b_ada2": np.ascontiguousarray(np.tile(f(inputs["b_ada"][0])[None, :], (2, 1))),
        "w_in": f(inputs["w_in"][0]),
        "sinkb": np.ascontiguousarray(np.tile(np.repeat(f(inputs["attn_sink"][0]), 128)[None, :], (64, 1))),
        "gmln": np.ascontiguousarray(np.tile(np.concatenate([f(inputs["gm_ln_g"][0]), f(inputs["gm_ln_b"][0])])[None, :], (128, 1))),
        "wsT": np.ascontiguousarray(f(inputs["gm_ws"][0]).transpose(2, 0, 1).reshape(128, 1024)),
        "bsT": np.ascontiguousarray(f(inputs["gm_bs"][0]).T),
        "w_pa": f(inputs["w_pa"][0]), "w_pb": f(inputs["w_pb"][0]), "w_o": f(inputs["w_o"][0]),
        "lnp": np.ascontiguousarray(np.tile(np.concatenate([f(inputs["ln1_g"][0]), f(inputs["ln1_b"][0]),
                                                            f(inputs["ln2_g"][0]), f(inputs["ln2_b"][0])])[None, :], (128, 1))),
        "wr": np.ascontiguousarray(np.concatenate([f(inputs["router_g_w"][0]),
                                                   f(inputs["router_e_w"][0]).transpose(1, 0, 2).reshape(1024, 32)], 1)),
        "br": np.ascontiguousarray(np.tile(np.concatenate([f(inputs["router_g_b"][0]),
                                                           f(inputs["router_e_b"][0]).reshape(32)])[None, :], (128, 1))),
        "w1": f(inputs["moe_w1"][0]).reshape(8192, 2048),
        "w3": f(inputs["moe_w3"][0]).reshape(8192, 2048),
        "w2": f(inputs["moe_w2"][0]).reshape(8192, 2048),
        "consts": consts,
    }
    in_maps = []
    for k in range(8):
        b, half = k // 2, k % 2
        lo = half * 4096
        xh = np.zeros((256, 1024), np.float32)
        rp = np.zeros((34 * 128, 128), np.float32)
        rp[128:128 + 4096] = rope[lo:lo + 4096]
        if half == 1:
            xh[0:128] = x[b, lo - 128:lo]
            rp[0:128] = rope[lo - 128:lo]
        if half == 0:
            xh[128:256] = x[b, lo + 4096:lo + 4096 + 128]
            rp[33 * 128:34 * 128] = rope[lo + 4096:lo + 4096 + 128]
        zero = np.zeros((128, 128), np.float32)
        masks = np.concatenate([mP, mN, mP if half == 1 else zero, mN if half == 0 else zero], 1)
        cT = np.stack([c[b].reshape(8, 128).T, c_ctx.reshape(8, 128).T], 2).reshape(128, 16)
        m = dict(shared)
        m.update({
            "x": np.ascontiguousarray(x[b, lo:lo + 4096]),
            "xh": xh,
            "ctx": np.ascontiguousarray(ctx[b]),
            "cT": np.ascontiguousarray(cT, dtype=np.float32),
            "rope": rp,
            "masks": np.ascontiguousarray(masks, dtype=np.float32),
        })
        in_maps.append(m)
    return in_maps


def kernel(**inputs):
    in_maps = _prep_inputs(inputs)
    nc = build_nc()
    res = run_bass_kernel_spmd(nc, in_maps, core_ids=list(range(8)))
    out = np.zeros((4, 8192, 1024), np.float32)
    for k in range(8):
        b, half = k // 2, k % 2
        out[b, half * 4096:(half + 1) * 4096] = np.asarray(res.results[k]["out"], dtype=np.float32)
    return out
```

```python
import numpy as np
from contextlib import ExitStack
import concourse.bass as bass
import concourse.mybir as mybir
from concourse.bass_utils import run_bass_kernel_spmd

F32 = mybir.dt.float32
BF16 = mybir.dt.bfloat16
I32 = mybir.dt.int32
AF = mybir.ActivationFunctionType
ALU = mybir.AluOpType
AX = mybir.AxisListType

import os
KDBG = os.environ.get('KDBG', '')
NT = 32
NSLOT = 128
NPAIR = 64
ALPHA = 2.0 ** 0.25
LN_EPS = 1e-6
GC = 0.7978845608028654


class Buf:
    def __init__(self, name):
        self.name = name
        self.w = None
        self.r = {}
        self.dsem = None
        self.dcount = 0


class EngState:
    def __init__(self, name, eng, sem, selfwait):
        self.name = name
        self.eng = eng
        self.sem = sem
        self.count = 0
        self.seen = {}
        self.selfwait = selfwait


class KB:
    def __init__(self, nc, stack):
        self.nc = nc
        self.stack = stack
        self.E = {}
        for name, eng, sw in (("pe", nc.tensor, False), ("act", nc.scalar, True),
                              ("dve", nc.vector, True), ("pool", nc.gpsimd, True),
                              ("sp", nc.sync, True)):
            sem = stack.enter_context(nc.semaphore("s_" + name))
            self.E[name] = EngState(name, eng, sem, sw)
        self.dma_toks = []

    def new_sem(self, name):
        return self.stack.enter_context(self.nc.semaphore(name))

    def sb(self, name, shape, dt):
        return self.stack.enter_context(self.nc.sbuf_tensor(name, shape, dt))

    def ps(self, name, shape, dt):
        return self.stack.enter_context(self.nc.psum_tensor(name, shape, dt))

    def _wait(self, E, toks):
        need = {}
        for t in toks:
            if t is None:
                continue
            sem, val = t
            if sem is E.sem and not E.selfwait:
                continue
            k = id(sem)
            if val > E.seen.get(k, 0):
                if k not in need or need[k][1] < val:
                    need[k] = (sem, val)
        for k, (sem, val) in need.items():
            E.eng.wait_ge(sem, val)
            E.seen[k] = val

    def _deps(self, reads, writes):
        deps = []
        for b in reads:
            deps.append(b.w)
        for b in writes:
            deps.extend(b.r.values())
            deps.append(b.w)
        return deps

    def _record(self, tok, reads, writes):
        for b in reads:
            k = id(tok[0])
            if k not in b.r or b.r[k][1] < tok[1]:
                b.r[k] = tok
        for b in writes:
            b.w = tok
            b.r = {}

    def op(self, engname, fn, reads=(), writes=(), extra=()):
        E = self.E[engname]
        self._wait(E, self._deps(reads, writes) + list(extra))
        ins = fn(E.eng)
        E.count += 1
        ins.then_inc(E.sem, 1)
        tok = (E.sem, E.count)
        self._record(tok, reads, writes)
        return tok

    def group(self, engname, fns, reads=(), writes=(), extra=()):
        E = self.E[engname]
        self._wait(E, self._deps(reads, writes) + list(extra))
        ins = None
        for fn in fns:
            ins = fn(E.eng)
        E.count += 1
        ins.then_inc(E.sem, 1)
        tok = (E.sem, E.count)
        self._record(tok, reads, writes)
        return tok

    def dma(self, engname, fn, reads=(), writes=(), owner=None, extra=()):
        E = self.E[engname]
        self._wait(E, self._deps(reads, writes) + list(extra))
        if owner is None:
            owner = (list(writes) + list(reads))[0]
        if owner.dsem is None:
            owner.dsem = self.new_sem("d_" + owner.name)
        ins = fn(E.eng)
        owner.dcount += 16
        ins.then_inc(owner.dsem, 16)
        tok = (owner.dsem, owner.dcount)
        self.dma_toks.append(tok)
        self._record(tok, reads, writes)
        return tok

    def wait_tok(self, engname, toks):
        self._wait(self.E[engname], toks)

    def barrier_all(self):
        toks = [(e.sem, e.count) for e in self.E.values() if e.count > 0] + maxtoks(self.dma_toks)
        for e in self.E.values():
            self._wait(e, [t for t in toks if t[0] is not e.sem])
        self.dma_toks = maxtoks(self.dma_toks)


def maxtoks(toks):
    best = {}
    for t in toks:
        if t is None:
            continue
        k = id(t[0])
        if k not in best or best[k][1] < t[1]:
            best[k] = t
    return list(best.values())


def build_nc(stop=None, nta=NT, dbg=False):
    nc = bass.Bass("TRN2", target_bir_lowering=False)

    def din(name, shape, dt=F32):
        return nc.dram_tensor(name, shape, dt, kind="ExternalInput").ap()

    x_d = din("x", [4096, 1024])
    xh_d = din("xh", [256, 1024])
    ctx_d = din("ctx", [256, 1024])
    cT_d = din("cT", [128, 16])
    wada_d = din("w_ada", [1024, 6144])
    bada_d = din("b_ada2", [2, 6144])
    win_d = din("w_in", [1024, 3840])
    sink_d = din("sinkb", [64, 1024])
    gmln_d = din("gmln", [128, 1024])
    wsT_d = din("wsT", [128, 1024])
    bsT_d = din("bsT", [128, 8])
    wpa_d = din("w_pa", [512, 1024])
    wpb_d = din("w_pb", [512, 1024])
    wo_d = din("w_o", [1024, 1024])
    lnp_d = din("lnp", [128, 4096])
    wr_d = din("wr", [1024, 36])
    br_d = din("br", [128, 36])
    w1_d = din("w1", [8192, 2048])
    w3_d = din("w3", [8192, 2048])
    w2_d = din("w2", [8192, 2048])
    rope_d = din("rope", [34 * 128, 128])
    masks_d = din("masks", [128, 512])
    NCST = 512 + 64 + 1
    consts_d = din("consts", [128, NCST])
    out_d = nc.dram_tensor("out", [4096, 1024], F32, kind="ExternalOutput").ap()

    dk = dict(kind="ExternalOutput") if dbg else {}
    x1_d = nc.dram_tensor("x1_scr", [4096, 1024], F32, **dk).ap()
    h2_d = nc.dram_tensor("h2_scr", [4096, 1024], BF16, **dk).ap()
    y_d = nc.dram_tensor("y_scr", [4096, 1024], BF16, **dk).ap()
    dbgA_d = nc.dram_tensor("dbgA", [4096, 1024], F32, **dk).ap() if dbg else None
    mod_d = nc.dram_tensor("mod_scr", [128, 8 * 1024], F32).ap()
    wq_d = [nc.dram_tensor("wq%d_scr" % i, [8192, 2048], BF16).ap() for i in range(3)]
    xs_d = nc.dram_tensor("xs_scr", [NSLOT * 128, 1024], BF16).ap()
    ys_d = nc.dram_tensor("ys_scr", [NSLOT * 128, 1024], F32).ap()

    with ExitStack() as st:
        kb = KB(nc, st)

        def mkp(ph):
            def mk(name, shape, dt):
                return ph.enter_context(nc.sbuf_tensor(name, shape, dt)), Buf(name)
            return mk

        mkg = mkp(st)

        PS = []
        for i in range(4):
            t = kb.ps("ps%d" % i, [128, 1024], F32)
            PS.append((t, Buf("ps%d" % i)))
        psi = [0]

        def nextps():
            r = PS[psi[0] % 4]
            psi[0] += 1
            return r

        def bfv(t):
            return t[:].bitcast(BF16)

        cst_f, b_cst_f = mkg("cst_f", [128, NCST], F32)
        kb.dma("sp", lambda e: e.dma_start(out=cst_f[:], in_=consts_d), writes=[b_cst_f])
        ident_f = cst_f[:, 0:128]
        S128 = 512
        PIDX = 512 + 64
        cst_b, b_cst_b = mkg("cst_b", [128, 384], BF16)
        kb.op("dve", lambda e: e.tensor_copy(out=cst_b[:], in_=cst_f[:, 0:384]), reads=[b_cst_f], writes=[b_cst_b])
        ident_b = cst_b[:, 0:128]
        tri_b = cst_b[:, 128:256]
        ones_b = cst_b[:, 256:384]

        st12, b_st12 = mkg("st12", [128, 12], F32)
        mv, b_mv = mkg("mv", [128, 2], F32)
        nw, b_nw = mkg("nw", [128, 4], F32)
        lnt, b_lnt = mkg("lnt", [128, 1024], F32)
        Cc, b_Cc = mkg("Cc", [128, 32], F32)
        A12all, b_A12all = mkg("A12all", [128, NT, 64], BF16)
        RK, b_RK = mkg("RK", [128, 4, NT], F32)
        posi, b_posi = mkg("posi", [128, 2, NT], I32)

        def rstd_newton(eps):
            kb.op("dve", lambda e: e.tensor_scalar(out=nw[:, 0:1], in0=mv[:, 1:2], scalar1=eps, scalar2=None, op0=ALU.add),
                  reads=[b_mv], writes=[b_nw])
            kb.op("dve", lambda e: e.tensor_scalar(out=nw[:, 2:3], in0=nw[:, 0:1], scalar1=0.5, scalar2=0.5, op0=ALU.mult, op1=ALU.add),
                  reads=[b_nw], writes=[b_nw])
            kb.op("dve", lambda e: e.reciprocal(out=nw[:, 1:2], in_=nw[:, 2:3]), reads=[b_nw], writes=[b_nw])
            for _ in range(3):
                kb.op("dve", lambda e: e.tensor_tensor(out=nw[:, 2:3], in0=nw[:, 1:2], in1=nw[:, 1:2], op=ALU.mult),
                      reads=[b_nw], writes=[b_nw])
                kb.op("dve", lambda e: e.scalar_tensor_tensor(out=nw[:, 2:3], in0=nw[:, 2:3], scalar=-0.5, in1=nw[:, 0:1],
                                                              op0=ALU.mult, op1=ALU.mult), reads=[b_nw], writes=[b_nw])
                kb.op("dve", lambda e: e.scalar_tensor_tensor(out=nw[:, 1:2], in0=nw[:, 2:3], scalar=1.5, in1=nw[:, 1:2],
                                                              op0=ALU.add, op1=ALU.mult), reads=[b_nw], writes=[b_nw])

        def layernorm(src, b_src, n, A, B, rA, dst, b_dst, eps=LN_EPS):
            for i in range(n // 512):
                kb.op("dve", lambda e, i=i: e.bn_stats(out=st12[:, 6 * i:6 * i + 6], in_=src[:, i * 512:(i + 1) * 512]),
                      reads=[b_src], writes=[b_st12])
            kb.op("dve", lambda e: e.bn_aggr(out=mv[:], in_=st12[:, 0:6 * (n // 512)]), reads=[b_st12], writes=[b_mv])
            rstd_newton(eps)
            kb.op("dve", lambda e: e.scalar_tensor_tensor(out=lnt[:, 0:n], in0=src[:, 0:n], scalar=mv[:, 0:1], in1=A,
                                                          op0=ALU.subtract, op1=ALU.mult),
                  reads=[b_src, b_mv] + rA, writes=[b_lnt])
            kb.op("dve", lambda e: e.scalar_tensor_tensor(out=dst, in0=lnt[:, 0:n], scalar=nw[:, 1:2], in1=B,
                                                          op0=ALU.mult, op1=ALU.add),
                  reads=[b_lnt, b_nw] + rA, writes=[b_dst])

        def transposes(src_aps, rsrc, identity, dst, b_dst, dt_bf=True, pst=None):
            n = len(src_aps)
            pt, b_pt = nextps() if pst is None else pst
            pv = bfv(pt) if dt_bf else pt
            kb.group("pe", [lambda e, i=i, a=a: e.transpose(pv[:, i * 128:(i + 1) * 128], a, identity)
                            for i, a in enumerate(src_aps)], reads=rsrc + [b_cst_b, b_cst_f], writes=[b_pt])
            kb.op("act", lambda e: e.copy(out=dst, in_=pv[:, 0:n * 128]), reads=[b_pt], writes=[b_dst])

        mod_toks = []
        with ExitStack() as ph:
            mk = mkp(ph)
            cT_f, b_cT = mk("cT_f", [128, 16], F32)
            kb.dma("sp", lambda e: e.dma_start(out=cT_f[:], in_=cT_d), writes=[b_cT])
            cth, b_cth = mk("cth", [128, 16], F32)
            kb.op("act", lambda e: e.activation(out=cth[:], in_=cT_f[:], func=AF.Tanh, scale=0.5), reads=[b_cT], writes=[b_cth])
            kb.op("dve", lambda e: e.scalar_tensor_tensor(out=cth[:], in0=cth[:], scalar=1.0, in1=cT_f[:], op0=ALU.add, op1=ALU.mult),
                  reads=[b_cth, b_cT], writes=[b_cth])
            sT_b, b_sT = mk("sT_b", [128, 16], F32)
            kb.op("dve", lambda e: e.tensor_scalar(out=sT_b[:], in0=cth[:], scalar1=0.5, scalar2=None, op0=ALU.mult),
                  reads=[b_cth], writes=[b_sT])
            sel, b_sel = mk("sel", [2, 256], F32)
            kb.op("pool", lambda e: e.memset(sel[:], 0.0), writes=[b_sel])
            kb.op("pool", lambda e: e.memset(sel[0:1, 0:128], 1.0), reads=[b_sel], writes=[b_sel])
            kb.dma("sp", lambda e: e.dma_start(out=sel[1:2, 128:256], in_=consts_d[0:1, 256:384]), reads=[b_sel], writes=[b_sel])
            wada_v = wada_d.rearrange("(k p) n -> p k n", p=128)
            wab = [mk("wab%d" % i, [128, 8, 512], F32) for i in range(2)]
            bad = [mk("bad%d" % i, [2, 512], F32) for i in range(2)]
            mrow = [mk("modrow%d" % i, [2, 512], F32) for i in range(2)]
            mts = [mk("mt%d" % i, [128, 1024], F32) for i in range(2)]
            for ch in range(12):
                wa, b_wa = wab[ch % 2]
                bd, b_bd = bad[ch % 2]
                modrow, b_modrow = mrow[ch % 2]
                mt, b_mt = mts[ch % 2]
                kb.dma("sp", lambda e, ch=ch, wa=wa: e.dma_start(out=wa[:], in_=wada_v[:, :, ch * 512:(ch + 1) * 512]), writes=[b_wa])
                kb.dma("sp", lambda e, ch=ch, bd=bd: e.dma_start(out=bd[:], in_=bada_d[:, ch * 512:(ch + 1) * 512]), writes=[b_bd])
                pt, b_pt = nextps()
                kb.group("pe", [lambda e, k=k, wa=wa, pt=pt: e.matmul(pt[0:2, 0:512], lhsT=sT_b[:, 2 * k:2 * k + 2], rhs=wa[:, k, :],
                                                                       start=(k == 0), stop=(k == 7)) for k in range(8)],
                         reads=[b_sT, b_wa], writes=[b_pt])
                vec = ch // 2
                addc = 1.0 if vec in (1, 4) else 0.0
                kb.op("dve", lambda e, pt=pt, bd=bd, modrow=modrow, addc=addc: e.scalar_tensor_tensor(
                    out=modrow[:], in0=pt[0:2, 0:512], scalar=addc, in1=bd[:], op0=ALU.add, op1=ALU.add),
                    reads=[b_pt, b_bd], writes=[b_modrow])
                pt2, b_pt2 = nextps()
                fns = [lambda e, pt2=pt2, modrow=modrow: e.matmul(pt2[:, 0:512], lhsT=sel[:, 0:128], rhs=modrow[:], start=True, stop=True)]
                if vec < 2:
                    fns.append(lambda e, pt2=pt2, modrow=modrow: e.matmul(pt2[:, 512:1024], lhsT=sel[:, 128:256], rhs=modrow[:],
                                                                          start=True, stop=True))
                kb.group("pe", fns, reads=[b_sel, b_modrow], writes=[b_pt2])
                sc = 0.5 if vec == 2 else 1.0
                kb.op("act", lambda e, pt2=pt2, mt=mt, sc=sc: e.activation(out=mt[:], in_=pt2[:], func=AF.Copy, scale=sc),
                      reads=[b_pt2], writes=[b_mt])
                col = vec * 1024 + (ch % 2) * 512
                mod_toks.append(kb.dma("sp", lambda e, mt=mt, col=col: e.dma_start(out=mod_d[:, col:col + 512], in_=mt[:, 0:512]), reads=[b_mt]))
                if vec < 2:
                    col2 = (6 + vec) * 1024 + (ch % 2) * 512
                    mod_toks.append(kb.dma("sp", lambda e, mt=mt, col2=col2: e.dma_start(out=mod_d[:, col2:col2 + 512], in_=mt[:, 512:1024]),
                                           reads=[b_mt]))
            kb.barrier_all()
        modt = maxtoks(mod_toks)
        if stop == "S":
            return nc

        y_toks = []
        conv_toks = []
        with ExitStack() as ph:
            mk = mkp(ph)
            msk_b, b_msk = mk("msk_b", [128, 512], BF16)
            kb.dma("pool", lambda e: e.dma_start(out=msk_b[:], in_=masks_d), writes=[b_msk])
            gmln_t, b_gmln = mk("gmln_t", [128, 1024], F32)
            kb.dma("sp", lambda e: e.dma_start(out=gmln_t[:], in_=gmln_d), writes=[b_gmln])
            bsT_t, b_bsT = mk("bsT_t", [128, 8], F32)
            kb.dma("sp", lambda e: e.dma_start(out=bsT_t[:], in_=bsT_d), writes=[b_bsT])
            esink, b_esink = mk("esink", [64, 1024], F32)
            kb.dma("sp", lambda e: e.dma_start(out=esink[:], in_=sink_d), writes=[b_esink])
            kb.op("act", lambda e: e.activation(out=esink[:], in_=esink[:], func=AF.Exp), reads=[b_esink], writes=[b_esink])
            wsT_b, b_wsT = mk("wsT_b", [128, 1024], BF16)
            kb.dma("pool", lambda e: e.dma_start(out=wsT_b[:], in_=wsT_d), writes=[b_wsT])
            win_b, b_win = mk("win_b", [128, 8, 3840], BF16)
            win_v = win_d.rearrange("(k p) n -> p k n", p=128)
            for k in range(8):
                for hf in range(2):
                    kb.dma("pool", lambda e, k=k, hf=hf: e.dma_start(out=win_b[:, k, hf * 1920:(hf + 1) * 1920],
                                                                      in_=win_v[:, k, hf * 1920:(hf + 1) * 1920]), writes=[b_win])
            wpa_b, b_wpa = mk("wpa_b", [64, 8, 1024], BF16)
            kb.dma("pool", lambda e: e.dma_start(out=wpa_b[:], in_=wpa_d.rearrange("(h p) n -> p h n", p=64)), writes=[b_wpa])
            wpb_b, b_wpb = mk("wpb_b", [128, 4, 1024], BF16)
            kb.dma("pool", lambda e: e.dma_start(out=wpb_b[:], in_=wpb_d.rearrange("(k p) n -> p k n", p=128)), writes=[b_wpb])
            modA, b_modA = mk("modA", [128, 2, 1024], F32)
            kb.dma("sp", lambda e: e.dma_start(out=modA[:].rearrange("p a n -> p (a n)"), in_=mod_d[:, 0:2048]), writes=[b_modA], extra=modt)
            KT, b_KT = mk("KT", [128, 34 * 128], BF16)
            VV, b_VV = mk("VV", [128, 34, 128], BF16)
            KTc, b_KTc = mk("KTc", [128, 256], BF16)
            VVc, b_VVc = mk("VVc", [128, 2, 128], BF16)
            xts = [mk("xt%d" % i, [128, 1024], F32) for i in range(2)]
            hb, b_hb = mk("hb", [128, 1024], BF16)
            hTs = [mk("hT%d" % i, [128, 1024], BF16) for i in range(2)]
            ropes = [mk("rope%d" % i, [128, 128], F32) for i in range(2)]
            rt1, b_rt1 = mk("rt1", [128, 512], F32)
            rt2, b_rt2 = mk("rt2", [128, 512], F32)
            rq, b_rq = mk("rq", [128, 512], BF16)
            rk, b_rk = mk("rk", [128, 128], BF16)
            QT, b_QT = mk("QT", [128, 512], BF16)
            gg, b_gg = mk("gg", [128, 1024], F32)
            gsq, b_gsq = mk("gsq", [128, 1024], F32)
            tgs = [mk("tg%d" % i, [128, 1024], F32) for i in range(2)]
            ETs = [mk("ET%d" % i, [128, 512], BF16) for i in range(4)]
            eti = [0]
            dens, b_dens = rt2[0:64, :], b_rt2
            oT, b_oT = mk("oT", [64, 8, 128], BF16)
            vgm, b_vgm = mk("vgm", [128, 512], BF16)
            spb, b_spb = rt1, b_rt1
            ygm, b_ygm = mk("ygm", [128, 512], BF16)
            ygT, b_ygT = mk("ygT", [128, 512], BF16)
            yp1, b_yp1 = tgs[0]
            y2bs = [mk("y2b%d" % i, [128, 1024], BF16) for i in range(2)]

            rsrc, b_rsrc = mk("rsrc", [128, 512], F32)

            def rope_apply(src_ps, b_srcs_ps, nh, tab, b_tab, dst, b_dst, view=None):
                n = nh * 64
                kb.op("act", lambda e: e.copy(out=rsrc[:, 0:n], in_=src_ps), reads=b_srcs_ps, writes=[b_rsrc])
                src = rsrc[:, 0:n]
                b_srcs = [b_rsrc]
                cosb = tab[:, 0:64].unsqueeze(1).to_broadcast([128, nh, 64])
                kb.op("dve", lambda e: e.tensor_tensor(out=rt1[:, 0:n].rearrange("p (h d) -> p h d", h=nh),
                                                       in0=src.rearrange("p (h d) -> p h d", h=nh), in1=cosb, op=ALU.mult),
                      reads=b_srcs + [b_tab], writes=[b_rt1])
                sv = src.rearrange("p (g a d) -> p g a d", a=2, d=16)
                tv = rt2[:, 0:n].rearrange("p (g a d) -> p g a d", a=2, d=16)
                sn = tab[:, 64:128].rearrange("p (x a d) -> p x a d", a=2, d=16)
                for a in range(2):
                    o = tv[:, :, a, :].rearrange("p (h x) d -> p h x d", x=2)
                    i0 = sv[:, :, 1 - a, :].rearrange("p (h x) d -> p h x d", x=2)
                    i1 = sn[:, :, a, :].unsqueeze(1).to_broadcast([128, nh, 2, 16])
                    kb.op("dve", lambda e, o=o, i0=i0, i1=i1: e.tensor_tensor(out=o, in0=i0, in1=i1, op=ALU.mult),
                          reads=b_srcs + [b_tab], writes=[b_rt2])
                a1, a2 = rt1[:, 0:n], rt2[:, 0:n]
                if view is not None:
                    a1, a2 = view(a1), view(a2)
                kb.op("dve", lambda e: e.tensor_tensor(out=dst, in0=a1, in1=a2, op=ALU.add),
                      reads=[b_rt1, b_rt2], writes=[b_dst])

            def ln_mod_T(xt, b_xt, A, B, rA, i2):
                hT, b_hT = hTs[i2]
                layernorm(xt, b_xt, 1024, A, B, rA, hb[:], b_hb)
                transposes([hb[:, k * 128:(k + 1) * 128] for k in range(8)], [b_hb], ident_b, hT[:], b_hT)
                return hT, b_hT

            def proj_kv(hT, b_hT):
                pt, b_pt = nextps()
                kb.group("pe", [lambda e, k=k: e.matmul(pt[:, 0:256], lhsT=hT[:, k * 128:(k + 1) * 128], rhs=win_b[:, k, 512:768],
                                                        start=(k == 0), stop=(k == 7)) for k in range(8)],
                         reads=[b_hT, b_win], writes=[b_pt])
                return pt, b_pt

            cm, b_cm = gg, b_gg
            cs_, b_cs_ = gsq, b_gsq
            kb.dma("sp", lambda e: e.dma_start(out=cm[:], in_=mod_d[:, 7 * 1024:8 * 1024]), writes=[b_cm], extra=modt)
            kb.dma("sp", lambda e: e.dma_start(out=cs_[:], in_=mod_d[:, 6 * 1024:7 * 1024]), writes=[b_cs_], extra=modt)
            for ci in range(2):
                xt, b_xt = xts[ci % 2]
                kb.dma("sp", lambda e, ci=ci, xt=xt: e.dma_start(out=xt[:], in_=ctx_d[ci * 128:(ci + 1) * 128, :]), writes=[b_xt])
                hT, b_hT = ln_mod_T(xt, b_xt, cm[:], cs_[:], [b_cm, b_cs_], ci % 2)
                pt, b_pt = proj_kv(hT, b_hT)
                kb.op("act", lambda e, pt=pt: e.copy(out=rk[:], in_=pt[:, 0:128]), reads=[b_pt], writes=[b_rk])
                kb.op("act", lambda e, pt=pt, ci=ci: e.copy(out=VVc[:, ci, :], in_=pt[:, 128:256]), reads=[b_pt], writes=[b_VVc])
                transposes([rk[:]], [b_rk], ident_b, KTc[:, ci * 128:(ci + 1) * 128], b_KTc)

            class TS:
                pass

            def stage_kv(t):
                S = TS()
                S.t = t
                slot = t + 1
                xt, b_xt = xts[slot % 2]
                if t < 0:
                    src = xh_d[0:128, :]
                elif t >= NT:
                    src = xh_d[128:256, :]
                else:
                    src = x_d[t * 128:(t + 1) * 128, :]
                kb.dma("pool", lambda e: e.dma_start(out=xt[:], in_=src), writes=[b_xt])
                tab, b_tab = ropes[slot % 2]
                kb.dma("pool", lambda e: e.dma_start(out=tab[:], in_=rope_d[slot * 128:(slot + 1) * 128, :]), writes=[b_tab])
                S.tab, S.b_tab = tab, b_tab
                S.hT, S.b_hT = ln_mod_T(xt, b_xt, modA[:, 1, :], modA[:, 0, :], [b_modA], slot % 2)
                kvp, b_kvp = proj_kv(S.hT, S.b_hT)
                if KDBG == "norope":
                    kb.op("act", lambda e: e.copy(out=rk[:], in_=kvp[:, 0:128]), reads=[b_kvp], writes=[b_rk])
                else:
                    rope_apply(kvp[:, 0:128], [b_kvp], 2, tab, b_tab, rk[:], b_rk)
                kb.op("act", lambda e: e.copy(out=VV[:, slot, :], in_=kvp[:, 128:256]), reads=[b_kvp], writes=[b_VV])
                transposes([rk[:]], [b_rk], ident_b, KT[:, slot * 128:(slot + 1) * 128], b_KT)
                return S

            def stage_pre(S):
                hT, b_hT = S.hT, S.b_hT
                for gi in range(2):
                    pg, b_pg = nextps()
                    tg, b_tg = tgs[gi]
                    kb.group("pe", [lambda e, k=k, g=g, gi=gi, pg=pg: e.matmul(
                        pg[:, g * 512:(g + 1) * 512], lhsT=hT[:, k * 128:(k + 1) * 128],
                        rhs=win_b[:, k, 1792 + gi * 1024 + g * 512:1792 + gi * 1024 + (g + 1) * 512],
                        start=(k == 0), stop=(k == 7)) for g in range(2) for k in range(8)],
                        reads=[b_hT, b_win], writes=[b_pg])
                    kb.op("act", lambda e, pg=pg, tg=tg: e.activation(out=tg[:], in_=pg[:], func=AF.Tanh, scale=0.5), reads=[b_pg], writes=[b_tg])

            def stage_main(S):
                t = S.t
                slot = t + 1
                hT, b_hT = S.hT, S.b_hT
                pq, b_pq = PS[2]
                kb.group("pe", [lambda e, k=k: e.matmul(pq[:, 0:512], lhsT=hT[:, k * 128:(k + 1) * 128], rhs=win_b[:, k, 0:512],
                                                        start=(k == 0), stop=(k == 7)) for k in range(8)],
                         reads=[b_hT, b_win], writes=[b_pq])
                rope_apply(pq[:, 0:512], [b_pq], 8, S.tab, S.b_tab, rq[:].rearrange("p (c a d) -> p a c d", a=2, d=64), b_rq,
                           view=lambda ap: ap.rearrange("p (a c d) -> p a c d", a=2, d=64))
                transposes([rq[:, c * 128:(c + 1) * 128] for c in range(4)], [b_rq], ident_b, QT[:], b_QT, pst=PS[2])
                puv, b_puv = PS[3]
                kb.group("pe", [lambda e, k=k, g=g: e.matmul(puv[:, g * 512:(g + 1) * 512], lhsT=hT[:, k * 128:(k + 1) * 128],
                                                             rhs=win_b[:, k, 768 + g * 512:768 + (g + 1) * 512],
                                                             start=(k == 0), stop=(k == 7)) for g in range(2) for k in range(8)],
                         reads=[b_hT, b_win], writes=[b_puv])
                kb.op("act", lambda e: e.activation(out=gsq[:], in_=puv[:], func=AF.Square), reads=[b_puv], writes=[b_gsq])
                kb.op("dve", lambda e: e.tensor_scalar(out=gsq[:], in0=gsq[:], scalar1=0.044715, scalar2=1.0, op0=ALU.mult, op1=ALU.add),
                      reads=[b_gsq], writes=[b_gsq])
                kb.op("dve", lambda e: e.tensor_tensor(out=gsq[:], in0=gsq[:], in1=puv[:], op=ALU.mult),
                      reads=[b_gsq, b_puv], writes=[b_gsq])
                kb.op("act", lambda e: e.activation(out=gsq[:], in_=gsq[:], func=AF.Tanh, scale=GC), reads=[b_gsq], writes=[b_gsq])
                kb.op("dve", lambda e: e.scalar_tensor_tensor(out=gg[:], in0=gsq[:], scalar=1.0, in1=puv[:], op0=ALU.add, op1=ALU.mult),
                      reads=[b_gsq, b_puv], writes=[b_gg])
                layernorm(gg[:, 512:1024], b_gg, 512, gmln_t[:, 0:512], gmln_t[:, 512:1024], [b_gmln], vgm[:], b_vgm, eps=4.0 * LN_EPS)
                po = [PS[0], PS[1]]
                blocks = [("c", 0), ("c", 1), ("l", slot - 1), ("l", slot), ("l", slot + 1)]
                steps = [(g, bi) for g in range(2) for bi in range(5)]

                def kv_of(g, bi):
                    kind, idx = blocks[bi]
                    if kind == "c":
                        return (KTc[g * 64:(g + 1) * 64, idx * 128:(idx + 1) * 128], VVc[:, idx, g * 64:(g + 1) * 64], [b_KTc, b_VVc])
                    return (KT[g * 64:(g + 1) * 64, idx * 128:(idx + 1) * 128], VV[:, idx, g * 64:(g + 1) * 64], [b_KT, b_VV])

                def issue_S(si):
                    g, bi = steps[si]
                    kt, vt, rkv = kv_of(g, bi)
                    pS, b_pS = PS[2 + (si % 2)]
                    kb.op("pe", lambda e: e.matmul(pS[:, 0:512], lhsT=kt, rhs=QT[g * 64:(g + 1) * 64, :], start=True, stop=True),
                          reads=rkv + [b_QT], writes=[b_pS])

                issue_S(0)
                for si, (g, bi) in enumerate(steps):
                    if si + 1 < len(steps):
                        issue_S(si + 1)
                    pog, b_pog = po[g]
                    pS, b_pS = PS[2 + (si % 2)]
                    kt, vt, rkv = kv_of(g, bi)
                    ET, b_ET = ETs[eti[0] % 4]
                    eti[0] += 1
                    kb.op("act", lambda e, ET=ET, pS=pS: e.activation(out=ET[:], in_=pS[:, 0:512], func=AF.Exp, scale=0.125),
                          reads=[b_pS], writes=[b_ET])
                    mi = None
                    if bi == 2:
                        mi = 2 if t == 0 else 0
                    if bi == 4:
                        mi = 3 if t == NT - 1 else 1
                    if mi is not None:
                        mb = msk_b[:, mi * 128:(mi + 1) * 128].unsqueeze(1).to_broadcast([128, 4, 128])
                        kb.op("dve", lambda e, ET=ET, mb=mb: e.tensor_tensor(out=ET[:].rearrange("p (h q) -> p h q", h=4),
                                                                           in0=ET[:].rearrange("p (h q) -> p h q", h=4), in1=mb, op=ALU.mult),
                              reads=[b_ET, b_msk], writes=[b_ET])
                    fns = [lambda e, h=h, ET=ET, vt=vt, pog=pog, bi=bi: e.matmul(pog[0:64, h * 128:(h + 1) * 128], lhsT=vt,
                                                                              rhs=ET[:, h * 128:(h + 1) * 128],
                                                                              start=(bi == 0 and h == 0), stop=(bi == 4 and h == 3),
                                                                              skip_group_check=True) for h in range(4)]
                    fns.append(lambda e, ET=ET, pog=pog, bi=bi: e.matmul(pog[0:64, 512:1024], lhsT=ones_b[:, 0:64], rhs=ET[:],
                                                                       start=(bi == 0), stop=(bi == 4), skip_group_check=True))
                    kb.group("pe", fns, reads=rkv + [b_ET, b_cst_b], writes=[b_pog])
                    if bi == 4:
                        kb.op("dve", lambda e, pog=pog, g=g: e.tensor_tensor(out=dens[:], in0=pog[0:64, 512:1024],
                                                                           in1=esink[:, g * 512:(g + 1) * 512], op=ALU.add),
                              reads=[b_pog, b_esink], writes=[b_dens])
                        kb.op("dve", lambda e: e.reciprocal(out=dens[:], in_=dens[:]), reads=[b_dens], writes=[b_dens])
                        kb.op("dve", lambda e, pog=pog, g=g: e.tensor_tensor(out=oT[:, g * 4:(g + 1) * 4, :].rearrange("p h q -> p (h q)"),
                                                                           in0=pog[0:64, 0:512], in1=dens[:], op=ALU.mult),
                              reads=[b_pog, b_dens], writes=[b_oT])
                pya, b_pya = nextps()
                kb.group("pe", [lambda e, h=h, hf=hf: e.matmul(pya[:, hf * 512:(hf + 1) * 512], lhsT=oT[:, h, :],
                                                               rhs=wpa_b[:, h, hf * 512:(hf + 1) * 512], start=(h == 0), stop=(h == 7))
                                for hf in range(2) for h in range(8)], reads=[b_oT, b_wpa], writes=[b_pya])

                kb.op("dve", lambda e: e.scalar_tensor_tensor(out=yp1[:], in0=yp1[:], scalar=1.0, in1=pya[:], op0=ALU.add, op1=ALU.mult),
                      reads=[b_yp1, b_pya], writes=[b_yp1])
                if dbg:
                    y_toks.append(kb.dma("sp", lambda e: e.dma_start(out=dbgA_d[t * 128:(t + 1) * 128, :], in_=yp1[:]), reads=[b_yp1]))
                psp, b_psp = nextps()
                kb.group("pe", [lambda e, g=g: e.matmul(psp[:, g * 64:(g + 1) * 64], lhsT=wsT_b[:, g * 128:(g + 1) * 128],
                                                        rhs=vgm[:, g * 64:(g + 1) * 64], start=True, stop=True) for g in range(8)],
                         reads=[b_wsT, b_vgm], writes=[b_psp])
                bsb = bsT_t[:, 0:8].unsqueeze(2).to_broadcast([128, 8, 64])
                kb.op("dve", lambda e: e.tensor_tensor(out=spb[:].rearrange("p (g c) -> p g c", g=8),
                                                       in0=psp[:, 0:512].rearrange("p (g c) -> p g c", g=8), in1=bsb, op=ALU.add),
                      reads=[b_psp, b_bsT], writes=[b_spb])
                kb.op("dve", lambda e: e.scalar_tensor_tensor(out=ygm[:], in0=spb[:], scalar=0.5, in1=gg[:, 0:512], op0=ALU.mult, op1=ALU.mult),
                      reads=[b_spb, b_gg], writes=[b_ygm])
                transposes([ygm[:, k * 128:(k + 1) * 128] for k in range(4)], [b_ygm], ident_b, ygT[:], b_ygT)
                pyb, b_pyb = nextps()
                kb.group("pe", [lambda e, k=k, hf=hf: e.matmul(pyb[:, hf * 512:(hf + 1) * 512], lhsT=ygT[:, k * 128:(k + 1) * 128],
                                                               rhs=wpb_b[:, k, hf * 512:(hf + 1) * 512], start=(k == 0), stop=(k == 3))
                                for hf in range(2) for k in range(4)], reads=[b_ygT, b_wpb], writes=[b_pyb])
                kb.op("dve", lambda e: e.scalar_tensor_tensor(out=tgs[1][0][:], in0=tgs[1][0][:], scalar=1.0, in1=pyb[:], op0=ALU.add, op1=ALU.mult),
                      reads=[tgs[1][1], b_pyb], writes=[tgs[1][1]])
                y2b, b_y2b = y2bs[t % 2]
                kb.op("dve", lambda e: e.tensor_tensor(out=y2b[:], in0=yp1[:], in1=tgs[1][0][:], op=ALU.add),
                      reads=[b_yp1, tgs[1][1]], writes=[b_y2b])
                y_toks.append(kb.dma("sp", lambda e: e.dma_start(out=y_d[t * 128:(t + 1) * 128, :], in_=y2b[:]), reads=[b_y2b]))

            if stop == "A0":
                kb.barrier_all()
                return nc
            stage_kv(-1)
            Scur = stage_kv(0)
            if stop == "A1":
                kb.barrier_all()
                return nc
            b_conv = Buf("wconv")
            conv_jobs = [(mi, j) for mi in range(3) for j in range(16)]
            wsrc = [w1_d, w3_d, w2_d]
            for t in range(nta):
                stage_pre(Scur)
                Snext = stage_kv(t + 1)
                for _ in range(2):
                    if conv_jobs:
                        mi, j = conv_jobs.pop(0)
                        conv_toks.append(kb.dma("pool", lambda e, mi=mi, j=j: e.dma_start(
                            out=wq_d[mi][j * 512:(j + 1) * 512, :], in_=wsrc[mi][j * 512:(j + 1) * 512, :]), owner=b_conv, reads=[b_conv]))
                stage_main(Scur)
                Scur = Snext
            kb.barrier_all()
            if stop == "A":
                return nc

        x1_toks = []
        h2_toks = []
        with ExitStack() as ph:
            mk = mkp(ph)
            zt, b_zt = mk("zt", [128, 8192], BF16)
            kb.op("pool", lambda e: e.memset(zt[:], 0.0), writes=[b_zt])
            xs_v = xs_d.rearrange("(p a) n -> p (a n)", p=128)
            b_zfill = Buf("zfill")
            zf_tok = None
            for i in range(16):
                zf_tok = kb.dma("sp", lambda e, i=i: e.dma_start(out=xs_v[:, i * 8192:(i + 1) * 8192], in_=zt[:]),
                                reads=[b_zt], owner=b_zfill)
            wo_b, b_wo = mk("wo_b", [128, 8, 1024], BF16)
            kb.dma("pool", lambda e: e.dma_start(out=wo_b[:], in_=wo_d.rearrange("(k p) n -> p k n", p=128)), writes=[b_wo])
            modB, b_modB = mk("modB", [128, 3, 1024], F32)
            kb.dma("sp", lambda e: e.dma_start(out=modB[:].rearrange("p a n -> p (a n)"), in_=mod_d[:, 2048:5120]), writes=[b_modB])
            lnB, b_lnB = mk("lnB", [128, 2, 1024], F32)
            kb.dma("sp", lambda e: e.dma_start(out=lnB[:].rearrange("p a n -> p (a n)"), in_=lnp_d[:, 0:2048]), writes=[b_lnB])
            br_t, b_br = mk("br_t", [128, 36], F32)
            kb.dma("sp", lambda e: e.dma_start(out=br_t[:], in_=br_d), writes=[b_br])
            wr_t, b_wr = mk("wr_t", [128, 8, 36], F32)
            kb.dma("sp", lambda e: e.dma_start(out=wr_t[:], in_=wr_d.rearrange("(k p) n -> p k n", p=128)), writes=[b_wr])
            kb.op("pool", lambda e: e.memset(Cc[:], 0.0), writes=[b_Cc])
            xts = [mk("xtB%d" % i, [128, 1024], F32) for i in range(3)]
            ybs = [mk("ybB%d" % i, [128, 1024], BF16) for i in range(3)]
            yT, b_yT = mk("yT", [128, 1024], BF16)
            z1, b_z1 = mk("z1", [128, 1024], F32)
            x1s = [mk("x1_%d" % i, [128, 1024], F32) for i in range(2)]
            h2f, b_h2f = mk("h2f", [128, 1024], F32)
            h2bs = [mk("h2b%d" % i, [128, 1024], BF16) for i in range(2)]
            h2T, b_h2T = mk("h2T", [128, 1024], F32)
            lg, b_lg = mk("lg", [128, 36], F32)
            rs, b_rs = mk("rs", [128, 96], F32)
            A1f, b_A1f = mk("A1f", [128, 64], F32)
            cs, b_cs = mk("cs", [128, 96], F32)
            yt_ = maxtoks(y_toks)
            def loadB(t):
                xt, b_xt = xts[t % 3]
                kb.dma("pool", lambda e: e.dma_start(out=xt[:], in_=x_d[t * 128:(t + 1) * 128, :]), writes=[b_xt])
                yb, b_yb = ybs[t % 3]
                kb.dma("pool", lambda e: e.dma_start(out=yb[:], in_=y_d[t * 128:(t + 1) * 128, :]), writes=[b_yb], extra=yt_)

            def frontB(t):
                yb, b_yb = ybs[t % 3]
                transposes([yb[:, k * 128:(k + 1) * 128] for k in range(8)], [b_yb], ident_b, yT[:], b_yT, pst=PS[2])
                pmx, b_pmx = PS[t % 2]
                kb.group("pe", [lambda e, k=k, hf=hf: e.matmul(pmx[:, hf * 512:(hf + 1) * 512], lhsT=yT[:, k * 128:(k + 1) * 128],
                                                               rhs=wo_b[:, k, hf * 512:(hf + 1) * 512], start=(k == 0), stop=(k == 7))
                                for hf in range(2) for k in range(8)], reads=[b_yT, b_wo], writes=[b_pmx])

            def backB(t):
                xt, b_xt = xts[t % 3]
                pmx, b_pmx = PS[t % 2]
                kb.op("dve", lambda e: e.tensor_tensor(out=z1[:], in0=pmx[:], in1=modB[:, 0, :], op=ALU.mult),
                      reads=[b_pmx, b_modB], writes=[b_z1])
                kb.op("dve", lambda e: e.scalar_tensor_tensor(out=z1[:], in0=xt[:], scalar=ALPHA, in1=z1[:], op0=ALU.mult, op1=ALU.add),
                      reads=[b_xt, b_z1], writes=[b_z1])
                x1, b_x1 = x1s[t % 2]
                layernorm(z1, b_z1, 1024, lnB[:, 0, :], lnB[:, 1, :], [b_lnB], x1[:], b_x1)
                x1_toks.append(kb.dma("sp", lambda e: e.dma_start(out=x1_d[t * 128:(t + 1) * 128, :], in_=x1[:]), reads=[b_x1]))
                layernorm(x1, b_x1, 1024, modB[:, 2, :], modB[:, 1, :], [b_modB], h2f[:], b_h2f)
                h2b, b_h2b = h2bs[t % 2]
                kb.op("act", lambda e: e.copy(out=h2b[:].rearrange("t (j p) -> t p j", j=8),
                                              in_=h2f[:].rearrange("t (p j) -> t p j", j=8)), reads=[b_h2f], writes=[b_h2b])
                h2_toks.append(kb.dma("sp", lambda e: e.dma_start(out=h2_d[t * 128:(t + 1) * 128, :], in_=h2b[:]), reads=[b_h2b]))
                for hf in range(2):
                    transposes([h2f[:, (hf * 4 + k) * 128:(hf * 4 + k + 1) * 128] for k in range(4)], [b_h2f], ident_f,
                               h2T[:, hf * 512:(hf + 1) * 512], b_h2T, dt_bf=False, pst=PS[3])
                plg, b_plg = PS[3]
                kb.group("pe", [lambda e, k=k: e.matmul(plg[:, 0:36], lhsT=h2T[:, k * 128:(k + 1) * 128], rhs=wr_t[:, k, :],
                                                        start=(k == 0), stop=(k == 7)) for k in range(8)],
                         reads=[b_h2T, b_wr], writes=[b_plg])
                kb.op("dve", lambda e: e.tensor_tensor(out=lg[:], in0=plg[:, 0:36], in1=br_t[:], op=ALU.add),
                      reads=[b_plg, b_br], writes=[b_lg])

                def dv(fn, reads=(), writes=()):
                    kb.op("dve", fn, reads=[b_rs, b_lg] + list(reads), writes=[b_rs] + list(writes))
                dv(lambda e: e.reduce_max(out=rs[:, 0:1], in_=lg[:, 0:4], axis=AX.X))
                dv(lambda e: e.tensor_scalar(out=rs[:, 1:2], in0=rs[:, 0:1], scalar1=-1.0, scalar2=None, op0=ALU.mult))
                kb.op("act", lambda e: e.activation(out=rs[:, 4:8], in_=lg[:, 0:4], func=AF.Exp, bias=rs[:, 1:2], scale=1.0),
                      reads=[b_rs, b_lg], writes=[b_rs])
                dv(lambda e: e.reduce_sum(out=rs[:, 2:3], in_=rs[:, 4:8], axis=AX.X))
                dv(lambda e: e.reciprocal(out=rs[:, 3:4], in_=rs[:, 2:3]))
                dv(lambda e: e.tensor_scalar(out=rs[:, 8:12], in0=lg[:, 0:4], scalar1=rs[:, 0:1], scalar2=None, op0=ALU.is_equal))
                dv(lambda e: e.tensor_tensor(out=rs[:, 12:44].rearrange("p (g x) -> p g x", g=4),
                                             in0=lg[:, 4:36].rearrange("p (g x) -> p g x", g=4),
                                             in1=rs[:, 8:12].unsqueeze(2).to_broadcast([128, 4, 8]), op=ALU.mult))
                dv(lambda e: e.tensor_reduce(out=rs[:, 44:52], in_=rs[:, 12:44].rearrange("p (g x) -> p x g", g=4), axis=AX.X, op=ALU.add))
                dv(lambda e: e.max(out=rs[:, 52:60], in_=rs[:, 44:52]))
                dv(lambda e: e.tensor_scalar(out=rs[:, 60:68], in0=rs[:, 44:52], scalar1=rs[:, 52:53], scalar2=None, op0=ALU.is_equal))
                dv(lambda e: e.tensor_scalar(out=rs[:, 68:76], in0=rs[:, 44:52], scalar1=rs[:, 53:54], scalar2=None, op0=ALU.is_equal))
                dv(lambda e: e.tensor_tensor(out=rs[:, 76:77], in0=rs[:, 53:54], in1=rs[:, 52:53], op=ALU.subtract))
                kb.op("act", lambda e: e.activation(out=rs[:, 77:78], in_=rs[:, 76:77], func=AF.Exp), reads=[b_rs], writes=[b_rs])
                dv(lambda e: e.tensor_scalar(out=rs[:, 78:79], in0=rs[:, 77:78], scalar1=1.0, scalar2=None, op0=ALU.add))
                dv(lambda e: e.reciprocal(out=rs[:, 79:80], in_=rs[:, 78:79]))
                dv(lambda e: e.tensor_tensor(out=RK[:, 2, t:t + 1], in0=rs[:, 3:4], in1=rs[:, 79:80], op=ALU.mult), writes=[b_RK])
                dv(lambda e: e.tensor_tensor(out=RK[:, 3, t:t + 1], in0=RK[:, 2, t:t + 1], in1=rs[:, 77:78], op=ALU.mult),
                   reads=[b_RK], writes=[b_RK])
                for k2 in range(2):
                    dv(lambda e, k2=k2: e.tensor_tensor(out=A1f[:, k2 * 32:(k2 + 1) * 32].rearrange("p (g x) -> p g x", g=4),
                                                      in0=rs[:, 8:12].unsqueeze(2).to_broadcast([128, 4, 8]),
                                                      in1=rs[:, 60 + 8 * k2:68 + 8 * k2].unsqueeze(1).to_broadcast([128, 4, 8]), op=ALU.mult),
                       reads=[b_A1f], writes=[b_A1f])
                kb.op("dve", lambda e: e.tensor_copy(out=A12all[:, t, :], in_=A1f[:]), reads=[b_A1f], writes=[b_A12all])
                pc, b_pc = PS[3]
                kb.group("pe", [lambda e: e.matmul(pc[:, 0:64], lhsT=tri_b, rhs=A12all[:, t, :], start=True, stop=True),
                                lambda e: e.matmul(pc[:, 64:128], lhsT=ones_b, rhs=A12all[:, t, :], start=True, stop=True)],
                         reads=[b_cst_b, b_A12all], writes=[b_pc])
                kb.op("dve", lambda e: e.tensor_tensor(out=cs[:, 0:32], in0=pc[:, 0:32], in1=Cc[:], op=ALU.add),
                      reads=[b_pc, b_Cc], writes=[b_cs])
                kb.op("dve", lambda e: e.tensor_tensor(out=cs[:, 64:96], in0=pc[:, 64:96], in1=Cc[:], op=ALU.add),
                      reads=[b_pc, b_Cc, b_cs], writes=[b_cs])
                kb.op("dve", lambda e: e.tensor_tensor(out=cs[:, 32:64], in0=pc[:, 32:64], in1=cs[:, 64:96], op=ALU.add),
                      reads=[b_pc, b_cs], writes=[b_cs])
                kb.op("dve", lambda e: e.tensor_tensor(out=cs[:, 0:64], in0=cs[:, 0:64], in1=A1f[:], op=ALU.mult),
                      reads=[b_cs, b_A1f], writes=[b_cs])
                kb.op("dve", lambda e: e.tensor_reduce(out=RK[:, 0:2, t:t + 1].rearrange("p k o -> p (k o)"),
                                                       in_=cs[:, 0:64].rearrange("p (k x) -> p k x", k=2), axis=AX.X, op=ALU.add),
                      reads=[b_cs, b_RK], writes=[b_RK])
                kb.op("dve", lambda e: e.tensor_tensor(out=Cc[:], in0=cs[:, 64:96], in1=pc[:, 96:128], op=ALU.add),
                      reads=[b_cs, b_pc, b_Cc], writes=[b_Cc])

            loadB(0)
            loadB(1)
            frontB(0)
            for t in range(NT):
                if t + 2 < NT:
                    loadB(t + 2)
                if t + 1 < NT:
                    frontB(t + 1)
                backB(t)
            kb.barrier_all()
            if stop == "B":
                return nc

        ys_toks = []
        with ExitStack() as ph:
            mk = mkp(ph)
            big, b_big = mk("big", [128, 3072], F32)
            s256 = cst_f[:, S128:S128 + 64]
            kb.op("dve", lambda e: e.tensor_tensor(out=big[:, 0:1056].rearrange("p (x j) -> p x j", j=33),
                                                   in0=Cc[:].unsqueeze(2).to_broadcast([128, 32, 33]),
                                                   in1=s256[:, 0:33].unsqueeze(1).to_broadcast([128, 32, 33]), op=ALU.is_gt),
                  reads=[b_Cc, b_cst_f], writes=[b_big])
            pe_, b_pe_ = mk("pend", [128, 4, 32], F32)
            kb.op("dve", lambda e: e.tensor_reduce(out=pe_[:, 0, :], in_=big[:, 0:1056].rearrange("p (x j) -> p x j", j=33), axis=AX.X, op=ALU.add),
                  reads=[b_big], writes=[b_pe_])
            kb.op("dve", lambda e: e.tensor_scalar(out=pe_[:, 0, :], in0=pe_[:, 0, :], scalar1=256.0, scalar2=None, op0=ALU.mult),
                  reads=[b_pe_], writes=[b_pe_])
            kb.op("dve", lambda e: e.tensor_copy(out=pe_[:, 1, :], in_=pe_[:, 0, :]), reads=[b_pe_], writes=[b_pe_])
            cur = 1
            for k in (1, 2, 4, 8, 16):
                nxt = 3 - cur
                kb.op("dve", lambda e, cur=cur, nxt=nxt, k=k: e.tensor_copy(out=pe_[:, nxt, 0:k], in_=pe_[:, cur, 0:k]),
                      reads=[b_pe_], writes=[b_pe_])
                kb.op("dve", lambda e, cur=cur, nxt=nxt, k=k: e.tensor_tensor(out=pe_[:, nxt, k:32], in0=pe_[:, cur, k:32],
                                                                           in1=pe_[:, cur, 0:32 - k], op=ALU.add),
                      reads=[b_pe_], writes=[b_pe_])
                cur = nxt
            pend = pe_[:, cur, :]
            kb.op("dve", lambda e: e.tensor_tensor(out=pe_[:, 3, :], in0=pend, in1=pe_[:, 0, :], op=ALU.subtract),
                  reads=[b_pe_], writes=[b_pe_])
            posf, b_posf = mk("posf", [128, 2, NT], F32)
            for k2 in range(2):
                kb.op("dve", lambda e, k2=k2: e.tensor_tensor(out=big[:, 0:1024].rearrange("p (t x) -> p t x", x=32),
                                                            in0=A12all[:, :, k2 * 32:(k2 + 1) * 32],
                                                            in1=pe_[:, 3, :].unsqueeze(1).to_broadcast([128, NT, 32]), op=ALU.mult),
                      reads=[b_A12all, b_pe_, b_big], writes=[b_big])
                kb.op("dve", lambda e, k2=k2: e.tensor_reduce(out=posf[:, k2, :], in_=big[:, 0:1024].rearrange("p (t x) -> p t x", x=32),
                                                            axis=AX.X, op=ALU.add), reads=[b_big, b_posf], writes=[b_posf])
                kb.op("dve", lambda e, k2=k2: e.tensor_tensor(out=posf[:, k2, :], in0=posf[:, k2, :], in1=RK[:, k2, :], op=ALU.add),
                      reads=[b_posf, b_RK], writes=[b_posf])
            kb.op("dve", lambda e: e.tensor_copy(out=posi[:], in_=posf[:]), reads=[b_posf], writes=[b_posi])
            kb.op("dve", lambda e: e.tensor_tensor(out=big[:, 0:2048].rearrange("p (s x) -> p s x", x=32),
                                                   in0=pend.unsqueeze(1).to_broadcast([128, NPAIR, 32]),
                                                   in1=s256.unsqueeze(2).to_broadcast([128, NPAIR, 32]), op=ALU.is_le),
                  reads=[b_pe_, b_cst_f, b_big], writes=[b_big])
            wif, b_wif = mk("wif", [128, NPAIR], F32)
            widx, b_widx = mk("widx", [128, 2, NPAIR], I32)
            wif2, b_wif2 = mk("wif2", [128, 2, NPAIR], F32)
            kb.op("dve", lambda e: e.tensor_reduce(out=wif[:], in_=big[:, 0:2048].rearrange("p (s x) -> p s x", x=32), axis=AX.X, op=ALU.add),
                  reads=[b_big], writes=[b_wif])
            kb.op("dve", lambda e: e.tensor_scalar(out=wif[:], in0=wif[:], scalar1=31.0, scalar2=128.0, op0=ALU.min, op1=ALU.mult),
                  reads=[b_wif], writes=[b_wif])
            kb.op("dve", lambda e: e.tensor_scalar(out=wif[:], in0=wif[:], scalar1=cst_f[:, PIDX:PIDX + 1], scalar2=None, op0=ALU.add),
                  reads=[b_wif, b_cst_f], writes=[b_wif])
            for hf in range(2):
                kb.op("dve", lambda e, hf=hf: e.tensor_scalar(out=wif2[:, hf, :], in0=wif[:], scalar1=2.0, scalar2=float(hf),
                                                            op0=ALU.mult, op1=ALU.add), reads=[b_wif, b_wif2], writes=[b_wif2])
            kb.op("dve", lambda e: e.tensor_copy(out=widx[:], in_=wif2[:]), reads=[b_wif2], writes=[b_widx])
            widx1, _b = mk("widx1", [128, NPAIR], I32)
            kb.op("dve", lambda e: e.tensor_copy(out=widx1[:], in_=wif[:]), reads=[b_wif, b_widx], writes=[b_widx])

            h2bs = [mk("h2c%d" % i, [128, 1024], BF16) for i in range(2)]
            sc_toks = []
            h2t_ = maxtoks(h2_toks)
            for t in range(NT):
                h2b, b_h2b = h2bs[t % 2]
                kb.dma("sp", lambda e: e.dma_start(out=h2b[:], in_=h2_d[t * 128:(t + 1) * 128, :]), writes=[b_h2b], extra=h2t_)
                for k2 in range(2):
                    sc_toks.append(kb.dma("pool", lambda e, k2=k2: e.indirect_dma_start(
                        out=xs_d, out_offset=bass.IndirectOffsetOnAxis(ap=posi[:, k2, t:t + 1], axis=0), in_=h2b[:], in_offset=None),
                        reads=[b_h2b, b_posi], owner=b_h2b, extra=[zf_tok]))

            wbufs = [(mk("w1b%d" % i, [128, 4096], BF16), mk("w3b%d" % i, [128, 4096], BF16), mk("w2b%d" % i, [128, 4096], BF16))
                     for i in range(2)]
            xbs = [mk("xb%d" % i, [128, 1024], BF16) for i in range(4)]
            xbT, b_xbT = mk("xbT", [128, 1024], BF16)
            hid, b_hid = mk("hid", [128, 512], BF16)
            hidT, b_hidT = mk("hidT", [128, 512], BF16)
            tht, b_tht = mk("tht", [128, 512], F32)
            ysbs = [mk("ysb%d" % i, [128, 1024], F32) for i in range(2)]
            sct = maxtoks(sc_toks)
            if stop == "C":
                kb.barrier_all()
                return nc
            convt = maxtoks(conv_toks)
            wq_v = [w.rearrange("(r h) c -> r (h c)", h=2) for w in wq_d]

            def loadW(pr):
                (w1b, b_w1b), (w3b, b_w3b), (w2b, b_w2b) = wbufs[pr % 2]
                for (wb_, bw_, wd_) in ((w1b, b_w1b, wq_v[0]), (w3b, b_w3b, wq_v[1]), (w2b, b_w2b, wq_v[2])):
                    kb.dma("pool", lambda e, wb_=wb_, wd_=wd_: e.indirect_dma_start(
                        out=wb_[:], out_offset=None, in_=wd_,
                        in_offset=bass.IndirectOffsetOnAxis(ap=widx1[:, pr:pr + 1], axis=0)),
                        reads=[b_widx], writes=[bw_], extra=convt)

            def loadX(s):
                xb, b_xb = xbs[s % 4]
                kb.dma("pool", lambda e: e.dma_start(out=xb[:], in_=xs_d[s * 128:(s + 1) * 128, :]), writes=[b_xb], extra=sct)

            def frontD_a(s):
                xb, b_xb = xbs[s % 4]
                pt, b_pt = PS[2]
                pv = bfv(pt)
                kb.group("pe", [lambda e, i=i: e.transpose(pv[:, i * 128:(i + 1) * 128], xb[:, i * 128:(i + 1) * 128], ident_b)
                                for i in range(8)], reads=[b_xb, b_cst_b], writes=[b_pt])

            def frontD_b(s):
                pr = s // 2
                (w1b, b_w1b), (w3b, b_w3b), (w2b, b_w2b) = wbufs[pr % 2]
                pt, b_pt = PS[2]
                pv = bfv(pt)
                kb.op("act", lambda e: e.copy(out=xbT[:], in_=pv[:, 0:1024]), reads=[b_pt], writes=[b_xbT])
                pab, b_pab = PS[s % 2]
                kb.group("pe", [lambda e, k=k, wq=wq, o=o: e.matmul(pab[:, o * 512:(o + 1) * 512], lhsT=xbT[:, k * 128:(k + 1) * 128],
                                                                    rhs=wq[:, k * 512:(k + 1) * 512], start=(k == 0), stop=(k == 7))
                                for o, wq in ((0, w1b), (1, w3b)) for k in range(8)],
                         reads=[b_xbT, b_w1b, b_w3b], writes=[b_pab])

            def backD_a(s):
                pab, b_pab = PS[s % 2]
                kb.op("act", lambda e: e.activation(out=tht[:], in_=pab[:, 0:512], func=AF.Tanh, scale=0.5), reads=[b_pab], writes=[b_tht])
                kb.op("dve", lambda e: e.scalar_tensor_tensor(out=tht[:], in0=tht[:], scalar=1.0, in1=pab[:, 0:512], op0=ALU.add, op1=ALU.mult),
                      reads=[b_tht, b_pab], writes=[b_tht])
                kb.op("dve", lambda e: e.tensor_tensor(out=hid[:].rearrange("t (j p) -> t p j", j=4),
                                                       in0=tht[:].rearrange("t (p j) -> t p j", j=4),
                                                       in1=pab[:, 512:1024].rearrange("t (p j) -> t p j", j=4), op=ALU.mult),
                      reads=[b_tht, b_pab], writes=[b_hid])

            def backD_b(s):
                pr = s // 2
                (w1b, b_w1b), (w3b, b_w3b), (w2b, b_w2b) = wbufs[pr % 2]
                transposes([hid[:, k * 128:(k + 1) * 128] for k in range(4)], [b_hid], ident_b, hidT[:], b_hidT, pst=PS[3])
                py, b_py = PS[3]
                kb.group("pe", [lambda e, k=k, hf=hf: e.matmul(py[:, hf * 512:(hf + 1) * 512], lhsT=hidT[:, k * 128:(k + 1) * 128],
                                                               rhs=w2b[:, k * 1024 + hf * 512:k * 1024 + (hf + 1) * 512],
                                                               start=(k == 0), stop=(k == 3)) for hf in range(2) for k in range(4)],
                         reads=[b_hidT, b_w2b], writes=[b_py])
                ysb, b_ysb = ysbs[s % 2]
                kb.op("act", lambda e: e.activation(out=ysb[:], in_=py[:], func=AF.Copy, scale=0.5), reads=[b_py], writes=[b_ysb])
                ys_toks.append(kb.dma("sp", lambda e: e.dma_start(out=ys_d[s * 128:(s + 1) * 128, :], in_=ysb[:]), reads=[b_ysb]))

            loadW(0)
            for s0 in range(3):
                loadX(s0)
            frontD_a(0)
            frontD_b(0)
            for s in range(NSLOT):
                if s + 3 < NSLOT:
                    loadX(s + 3)
                if s + 1 < NSLOT:
                    if (s + 1) % 2 == 0:
                        loadW((s + 1) // 2)
                    frontD_a(s + 1)
                backD_a(s)
                if s + 1 < NSLOT:
                    frontD_b(s + 1)
                backD_b(s)
            kb.barrier_all()
            if stop == "D":
                return nc

        with ExitStack() as ph:
            mk = mkp(ph)
            g2t, b_g2t = mk("g2t", [128, 1024], F32)
            kb.dma("sp", lambda e: e.dma_start(out=g2t[:], in_=mod_d[:, 5120:6144]), writes=[b_g2t])
            lnE, b_lnE = mk("lnE", [128, 2, 1024], F32)
            kb.dma("sp", lambda e: e.dma_start(out=lnE[:].rearrange("p a n -> p (a n)"), in_=lnp_d[:, 2048:4096]), writes=[b_lnE])
            xts = [mk("xtE%d" % i, [128, 1024], F32) for i in range(2)]
            yg = [[mk("yg%d_%d" % (k2, i), [128, 1024], F32) for i in range(2)] for k2 in range(2)]
            z1, b_z1 = mk("z1E", [128, 1024], F32)
            ots = [mk("ot%d" % i, [128, 1024], F32) for i in range(2)]
            yst = maxtoks(ys_toks)
            x1t = maxtoks(x1_toks)
            out_toks = []
            for t in range(NT):
                xt, b_xt = xts[t % 2]
                kb.dma("pool", lambda e: e.dma_start(out=xt[:], in_=x1_d[t * 128:(t + 1) * 128, :]), writes=[b_xt], extra=x1t)
                yk = []
                for k2 in range(2):
                    ykt, b_yk = yg[k2][t % 2]
                    yk.append((ykt, b_yk))
                    kb.dma("pool", lambda e, k2=k2, ykt=ykt: e.indirect_dma_start(
                        out=ykt[:], out_offset=None, in_=ys_d, in_offset=bass.IndirectOffsetOnAxis(ap=posi[:, k2, t:t + 1], axis=0)),
                        reads=[b_posi], writes=[b_yk], extra=yst)
                kb.op("act", lambda e: e.activation(out=z1[:], in_=yk[0][0][:], func=AF.Copy, scale=RK[:, 2, t:t + 1]),
                      reads=[yk[0][1], b_RK], writes=[b_z1])
                kb.op("dve", lambda e: e.scalar_tensor_tensor(out=z1[:], in0=yk[1][0][:], scalar=RK[:, 3, t:t + 1], in1=z1[:],
                                                              op0=ALU.mult, op1=ALU.add), reads=[yk[1][1], b_RK, b_z1], writes=[b_z1])
                kb.op("dve", lambda e: e.tensor_tensor(out=z1[:], in0=z1[:], in1=g2t[:], op=ALU.mult),
                      reads=[b_z1, b_g2t], writes=[b_z1])
                kb.op("dve", lambda e: e.scalar_tensor_tensor(out=z1[:], in0=xt[:], scalar=ALPHA, in1=z1[:], op0=ALU.mult, op1=ALU.add),
                      reads=[b_xt, b_z1], writes=[b_z1])
                ot, b_ot = ots[t % 2]
                layernorm(z1, b_z1, 1024, lnE[:, 0, :], lnE[:, 1, :], [b_lnE], ot[:], b_ot)
                out_toks.append(kb.dma("sp", lambda e: e.dma_start(out=out_d[t * 128:(t + 1) * 128, :], in_=ot[:]), reads=[b_ot]))
            kb.wait_tok("sp", maxtoks(out_toks))
    return nc


def _rope_table():
    n = 8192
    pos = np.arange(n)
    row = pos // 64
    col = pos % 64
    inv = (1.0 / (10000.0 ** (np.arange(16, dtype=np.float32) / 16.0))).astype(np.float32)
    tabs = []
    for p in (row, col):
        ang = p.astype(np.float32)[:, None] * inv[None, :]
        tabs.append((np.cos(ang).astype(np.float32), np.sin(ang).astype(np.float32)))
    cosr, sinr = tabs[0]
    cosc, sinc = tabs[1]
    cos64 = np.concatenate([cosr, cosr, cosc, cosc], 1)
    sin64 = np.concatenate([-sinr, sinr, -sinc, sinc], 1)
    return np.concatenate([cos64, sin64], 1).astype(np.float32)


def _prep_inputs(inputs):
    f = lambda a: np.ascontiguousarray(np.asarray(a, dtype=np.float32))
    x = f(inputs["x"]); c = f(inputs["c"]); ctx = f(inputs["ctx"]); c_ctx = f(inputs["c_ctx"])
    rope = _rope_table()
    kk = np.arange(128)[:, None]
    qq = np.arange(128)[None, :]
    mP = (kk >= qq).astype(np.float32)
    mN = (kk <= qq).astype(np.float32)
    ident = np.eye(128, dtype=np.float32)
    tri = (kk < qq).astype(np.float32)
    ones = np.ones((128, 128), np.float32)
    thr = np.tile((np.arange(65, dtype=np.float32) * 128.0)[None, :], (32, 1)).reshape(1, 2080)
    s128 = (np.arange(64, dtype=np.float32) * 256.0)[None, :]
    consts = np.concatenate([ident, tri, ones, np.zeros((128, 128), np.float32),
                             np.tile(s128, (128, 1)),
                             np.arange(128, dtype=np.float32)[:, None]], 1)
    consts = np.ascontiguousarray(consts, dtype=np.float32)
    shared = {
        "w_ada": f(inputs["w_ada"][0]),
        "b_ada2": np.ascontiguousarray(np.tile(f(inputs["b_ada"][0])[None, :], (2, 1))),
        "w_in": f(inputs["w_in"][0]),
        "sinkb": np.ascontiguousarray(np.tile(np.repeat(f(inputs["attn_sink"][0]), 128)[None, :], (64, 1))),
        "gmln": np.ascontiguousarray(np.tile(np.concatenate([f(inputs["gm_ln_g"][0]), f(inputs["gm_ln_b"][0])])[None, :], (128, 1))),
        "wsT": np.ascontiguousarray(f(inputs["gm_ws"][0]).transpose(2, 0, 1).reshape(128, 1024)),
        "bsT": np.ascontiguousarray(f(inputs["gm_bs"][0]).T),
        "w_pa": f(inputs["w_pa"][0]), "w_pb": f(inputs["w_pb"][0]), "w_o": f(inputs["w_o"][0]),
        "lnp": np.ascontiguousarray(np.tile(np.concatenate([f(inputs["ln1_g"][0]), f(inputs["ln1_b"][0]),
                                                            f(inputs["ln2_g"][0]), f(inputs["ln2_b"][0])])[None, :], (128, 1))),
        "wr": np.ascontiguousarray(np.concatenate([f(inputs["router_g_w"][0]),
                                                   f(inputs["router_e_w"][0]).transpose(1, 0, 2).reshape(1024, 32)], 1)),
        "br": np.ascontiguousarray(np.tile(np.concatenate([f(inputs["router_g_b"][0]),
                                                           f(inputs["router_e_b"][0]).reshape(32)])[None, :], (128, 1))),
        "w1": f(inputs["moe_w1"][0]).reshape(8192, 2048),
        "w3": f(inputs["moe_w3"][0]).reshape(8192, 2048),
        "w2": f(inputs["moe_w2"][0]).reshape(8192, 2048),
        "consts": consts,
    }
    in_maps = []
    for k in range(8):
        b, half = k // 2, k % 2
        lo = half * 4096
        xh = np.zeros((256, 1024), np.float32)
        rp = np.zeros((34 * 128, 128), np.float32)
        rp[128:128 + 4096] = rope[lo:lo + 4096]
        if half == 1:
            xh[0:128] = x[b, lo - 128:lo]
            rp[0:128] = rope[lo - 128:lo]
        if half == 0:
            xh[128:256] = x[b, lo + 4096:lo + 4096 + 128]
            rp[33 * 128:34 * 128] = rope[lo + 4096:lo + 4096 + 128]
        zero = np.zeros((128, 128), np.float32)
        masks = np.concatenate([mP, mN, mP if half == 1 else zero, mN if half == 0 else zero], 1)
        cT = np.stack([c[b].reshape(8, 128).T, c_ctx.reshape(8, 128).T], 2).reshape(128, 16)
        m = dict(shared)
        m.update({
            "x": np.ascontiguousarray(x[b, lo:lo + 4096]),
            "xh": xh,
            "ctx": np.ascontiguousarray(ctx[b]),
            "cT": np.ascontiguousarray(cT, dtype=np.float32),
            "rope": rp,
            "masks": np.ascontiguousarray(masks, dtype=np.float32),
        })
        in_maps.append(m)
    return in_maps


def kernel(**inputs):
    in_maps = _prep_inputs(inputs)
    nc = build_nc()
    res = run_bass_kernel_spmd(nc, in_maps, core_ids=list(range(8)))
    out = np.zeros((4, 8192, 1024), np.float32)
    for k in range(8):
        b, half = k // 2, k % 2
        out[b, half * 4096:(half + 1) * 4096] = np.asarray(res.results[k]["out"], dtype=np.float32)
    return out
```
